# Optimizing a Trainium2 kernel written in Bass

```python
import jax, jax.numpy as jnp
from jax import lax
import numpy as np

D_MODEL = 1024
BATCH = 32
SEQ = 2048
DEPTH = 4

N_A_LAYERS = DEPTH // 2
N_B_LAYERS = DEPTH - N_A_LAYERS
RMS_EPS = 1e-6
RET_HEADS = D_MODEL // 256
RET_QK_DIM = D_MODEL // RET_HEADS
RET_V_DIM = 2 * D_MODEL // RET_HEADS
RET_IN_WIDTH = 6 * D_MODEL
RET_CHUNK = 128
RET_ROT_BASE = 10000.0
GN_EPS = 1e-6
ATT_HEADS = 16
ATT_HEAD_DIM = D_MODEL // ATT_HEADS
DIL_GROUPS = ((128, 1), (512, 4), (2048, 16))
N_GROUPS = 3
ATT_BLOCK = 128
ROPE_THETA = 500000.0
ROPE_DIMS = ATT_HEAD_DIM // 4
Q_WIDTH = N_GROUPS * ATT_HEADS * ATT_HEAD_DIM
D_FF = 2816
CONV_WIDTH = 3

kernel_name = "yoco_retention_dilated_attn_convffn"


def rmsnorm(x, g):
    xf = x.astype(jnp.float32)
    y = xf * lax.rsqrt(jnp.mean(xf * xf, axis=-1, keepdims=True) + RMS_EPS)
    return (y * g).astype(x.dtype)


def apply_rotary(t, pos, inv_freq):
    n = 2 * inv_freq.shape[0]
    S = t.shape[1]
    ang = pos.astype(jnp.float32)[:, None] * inv_freq[None, :]
    bshape = (1, S) + (1,) * (t.ndim - 3) + (n // 2,)
    cos = jnp.cos(ang).reshape(bshape)
    sin = jnp.sin(ang).reshape(bshape)
    t1, t2, rest = t[..., : n // 2], t[..., n // 2: n], t[..., n:]
    rot = jnp.concatenate([t1 * cos - t2 * sin, t2 * cos + t1 * sin], axis=-1).astype(t.dtype)
    return jnp.concatenate([rot, rest], axis=-1)


def retention_chunkwise(q, k, v, log_gamma):
    B, S, H, dk = q.shape
    dv = v.shape[-1]
    C = RET_CHUNK
    nc = S // C
    qc = q.reshape(B, nc, C, H, dk)
    kc = k.reshape(B, nc, C, H, dk)
    vc = v.reshape(B, nc, C, H, dv)
    n = jnp.arange(C, dtype=jnp.float32)
    diff = n[:, None] - n[None, :]
    decay_mask = jnp.where(diff[None] >= 0,
                           jnp.exp(jnp.maximum(diff, 0.0)[None] * log_gamma[:, None, None]), 0.0)
    scores = jnp.einsum('bnqhd,bnkhd->bnhqk', qc, kc) * decay_mask
    intra = jnp.einsum('bnhqk,bnkhe->bnqhe', scores, vc)
    xi = jnp.exp((n + 1.0)[None, :] * log_gamma[:, None]).T[None, :, :, None]
    zeta = jnp.exp((C - 1.0 - n)[None, :] * log_gamma[:, None]).T[None, :, :, None]
    chunk_decay = jnp.exp(C * log_gamma)[None, :, None, None]

    def step(state, inp):
        qi, ki, vi = inp
        cross = jnp.einsum('bchd,bhde->bche', qi, state) * xi
        state = state * chunk_decay + jnp.einsum('bchd,bche->bhde', ki * zeta, vi)
        return state, cross

    state0 = jnp.zeros((B, H, dk, dv), jnp.float32)
    _, cross = lax.scan(step, state0, (jnp.moveaxis(qc, 1, 0), jnp.moveaxis(kc, 1, 0), jnp.moveaxis(vc, 1, 0)))
    out = intra + jnp.moveaxis(cross, 0, 1)
    return out.reshape(B, S, H, dv).astype(v.dtype)


def retention_mixer(h, w_in, w_out, gn_gain, pos):
    B, S, _ = h.shape
    dqk = RET_HEADS * RET_QK_DIM
    dv = RET_HEADS * RET_V_DIM
    proj = h @ w_in
    q = proj[..., :dqk].reshape(B, S, RET_HEADS, RET_QK_DIM)
    k = proj[..., dqk:2 * dqk].reshape(B, S, RET_HEADS, RET_QK_DIM) * (RET_QK_DIM ** -0.5)
    v = proj[..., 2 * dqk:2 * dqk + dv].reshape(B, S, RET_HEADS, RET_V_DIM)
    g = proj[..., 2 * dqk + dv:]
    inv_freq = 1.0 / (RET_ROT_BASE ** jnp.linspace(0.0, 1.0, RET_QK_DIM // 2, dtype=jnp.float32))
    q = apply_rotary(q, pos, inv_freq)
    k = apply_rotary(k, pos, inv_freq)
    log_gamma = jnp.log(1.0 - 2.0 ** (-5.0 - jnp.arange(RET_HEADS, dtype=jnp.float32)))
    y = retention_chunkwise(q, k, v, log_gamma).astype(jnp.float32)
    mu = jnp.mean(y, axis=-1, keepdims=True)
    var = jnp.mean(jnp.square(y - mu), axis=-1, keepdims=True)
    yn = ((y - mu) * lax.rsqrt(var + GN_EPS)).reshape(B, S, dv) * gn_gain
    return (jax.nn.silu(g) * yn.astype(g.dtype)) @ w_out


def shared_kv(x, kv_norm, w_kv, pos):
    B, S, _ = x.shape
    kv = (rmsnorm(x, kv_norm) @ w_kv).reshape(B, S, 2, N_GROUPS, ATT_HEADS, ATT_HEAD_DIM)
    inv_freq = ROPE_THETA ** (-jnp.arange(0, ROPE_DIMS, 2, dtype=jnp.float32) / ROPE_DIMS)
    k = apply_rotary(kv[:, :, 0], pos, inv_freq)
    v = kv[:, :, 1]
    return k, v


def dilated_group_attention(q, k, v, window, dilation):
    B, S, H, hd = q.shape
    blk = ATT_BLOCK
    steps = window // dilation
    span = dilation * blk
    s_pad = -(-S // span) * span
    L = s_pad // dilation
    nblk = L // blk

    def to_sub(t):
        t = jnp.pad(t, ((0, 0), (0, s_pad - S), (0, 0), (0, 0)))
        return t.reshape(B, L, dilation, H, hd).transpose(0, 2, 1, 3, 4)

    qs = to_sub(q)
    ks = jnp.pad(to_sub(k), ((0, 0), (0, 0), (blk, 0), (0, 0), (0, 0)))
    vs = jnp.pad(to_sub(v), ((0, 0), (0, 0), (blk, 0), (0, 0), (0, 0)))
    kj = jnp.arange(2 * blk)[None, :]
    dist = (jnp.arange(blk)[:, None] + blk) - kj
    band = (dist >= 0) & (dist <= steps)
    scale = hd ** -0.5

    def one_block(idx):
        r = idx // nblk
        b = idx % nblk
        qb = lax.dynamic_slice(qs, (0, r, b * blk, 0, 0), (B, 1, blk, H, hd))[:, 0]
        kb = lax.dynamic_slice(ks, (0, r, b * blk, 0, 0), (B, 1, 2 * blk, H, hd))[:, 0]
        vb = lax.dynamic_slice(vs, (0, r, b * blk, 0, 0), (B, 1, 2 * blk, H, hd))[:, 0]
        s = jnp.einsum('bqhd,bkhd->bhqk', qb, kb).astype(jnp.float32) * scale
        valid = band & (b * blk + kj - blk >= 0)
        s = jnp.where(valid, s, -jnp.inf)
        m = jnp.max(s, axis=-1, keepdims=True)
        p = jnp.exp(s - m)
        den = jnp.sum(p, axis=-1)
        o = jnp.einsum('bhqk,bkhd->bqhd', p, vb) / den.transpose(0, 2, 1)[..., None]
        lse = (m[..., 0] + jnp.log(den)).transpose(0, 2, 1)
        return o.astype(q.dtype), lse

    o, lse = lax.map(one_block, jnp.arange(dilation * nblk))
    o = o.reshape(dilation, nblk, B, blk, H, hd).transpose(2, 1, 3, 0, 4, 5).reshape(B, s_pad, H, hd)[:, :S]
    lse = lse.reshape(dilation, nblk, B, blk, H).transpose(2, 1, 3, 0, 4).reshape(B, s_pad, H)[:, :S]
    return o, lse


def dilated_mixer(h, w_q, w_o, k_sh, v_sh, pos):
    B, S, _ = h.shape
    q = (h @ w_q).reshape(B, S, N_GROUPS, ATT_HEADS, ATT_HEAD_DIM)
    inv_freq = ROPE_THETA ** (-jnp.arange(0, ROPE_DIMS, 2, dtype=jnp.float32) / ROPE_DIMS)
    q = apply_rotary(q, pos, inv_freq)
    outs, lses = [], []
    for gi, (window, dilation) in enumerate(DIL_GROUPS):
        o, l = dilated_group_attention(q[:, :, gi], k_sh[:, :, gi], v_sh[:, :, gi], window, dilation)
        outs.append(o)
        lses.append(l)
    alpha = jax.nn.softmax(jnp.stack(lses, axis=0), axis=0)
    o = jnp.einsum('gbsh,gbshd->bshd', alpha, jnp.stack(outs, axis=0).astype(jnp.float32))
    return o.astype(h.dtype).reshape(B, S, ATT_HEADS * ATT_HEAD_DIM) @ w_o


def conv_ffn(h, w_up, conv_w, conv_b, w_down):
    S = h.shape[1]
    u = h @ w_up
    gate, val = u[..., :D_FF], u[..., D_FF:]
    gp = jnp.pad(gate, ((0, 0), (CONV_WIDTH - 1, 0), (0, 0)))
    conv = conv_b
    for i in range(CONV_WIDTH):
        conv = conv + conv_w[i] * gp[:, i:i + S]
    return (jax.nn.gelu(conv, approximate=True) * val) @ w_down


def setup_inputs(seed: int = 0) -> dict:
    key = jax.random.key(seed)
    ks = jax.random.split(key, 17)
    f32 = jnp.float32

    def nrm(k, shape, fan_in):
        return jax.random.normal(k, shape, f32) * (fan_in ** -0.5)

    def gain(k, shape):
        return 1.0 + 0.02 * jax.random.normal(k, shape, f32)

    return {
        "x": jax.random.normal(ks[0], (BATCH, SEQ, D_MODEL), f32),
        "ret_w_in": nrm(ks[1], (N_A_LAYERS, D_MODEL, RET_IN_WIDTH), D_MODEL),
        "ret_w_out": nrm(ks[2], (N_A_LAYERS, RET_HEADS * RET_V_DIM, D_MODEL), RET_HEADS * RET_V_DIM),
        "ret_gn_gain": gain(ks[3], (N_A_LAYERS, RET_HEADS * RET_V_DIM)),
        "kv_norm": gain(ks[4], (D_MODEL,)),
        "att_w_kv": nrm(ks[5], (D_MODEL, 2 * Q_WIDTH), D_MODEL),
        "att_w_q": nrm(ks[6], (N_B_LAYERS, D_MODEL, Q_WIDTH), D_MODEL),
        "att_w_o": nrm(ks[7], (N_B_LAYERS, ATT_HEADS * ATT_HEAD_DIM, D_MODEL), ATT_HEADS * ATT_HEAD_DIM),
        "norm_mix_pre": gain(ks[8], (DEPTH, D_MODEL)),
        "norm_mix_post": gain(ks[9], (DEPTH, D_MODEL)),
        "norm_ffn_pre": gain(ks[10], (DEPTH, D_MODEL)),
        "norm_ffn_post": gain(ks[11], (DEPTH, D_MODEL)),
        "ffn_w_up": nrm(ks[12], (DEPTH, D_MODEL, 2 * D_FF), D_MODEL),
        "ffn_conv_w": nrm(ks[13], (DEPTH, CONV_WIDTH, D_FF), CONV_WIDTH),
        "ffn_conv_b": 0.02 * jax.random.normal(ks[14], (DEPTH, D_FF), f32),
        "ffn_w_down": nrm(ks[15], (DEPTH, D_FF, D_MODEL), D_FF),
    }


def reference(x, ret_w_in, ret_w_out, ret_gn_gain, kv_norm, att_w_kv, att_w_q, att_w_o,
              norm_mix_pre, norm_mix_post, norm_ffn_pre, norm_ffn_post,
              ffn_w_up, ffn_conv_w, ffn_conv_b, ffn_w_down):
    pos = jnp.arange(x.shape[1], dtype=jnp.int32)
    k_sh, v_sh = None, None
    for layer in range(DEPTH):
        h = rmsnorm(x, norm_mix_pre[layer])
        if layer < N_A_LAYERS:
            m = retention_mixer(h, ret_w_in[layer], ret_w_out[layer], ret_gn_gain[layer], pos)
        else:
            bi = layer - N_A_LAYERS
            m = dilated_mixer(h, att_w_q[bi], att_w_o[bi], k_sh, v_sh, pos)
        x = x + rmsnorm(m, norm_mix_post[layer])
        f = conv_ffn(rmsnorm(x, norm_ffn_pre[layer]), ffn_w_up[layer], ffn_conv_w[layer],
                     ffn_conv_b[layer], ffn_w_down[layer])
        x = x + rmsnorm(f, norm_ffn_post[layer])
        if layer == N_A_LAYERS - 1:
            k_sh, v_sh = shared_kv(x, kv_norm, att_w_kv, pos)
    return x
```

```python
import math
import numpy as np
import ml_dtypes
import concourse.bass as bass
import concourse.mybir as mybir
from concourse.bass_utils import run_bass_kernel_spmd

F32 = mybir.dt.float32
BF16 = mybir.dt.bfloat16
AF = mybir.ActivationFunctionType
ALU = mybir.AluOpType

P = 128
SEQ = 2048
DM = 1024
KC = 8
NCORES = 8
BATCH = 32
NSEQ = BATCH // NCORES
DFF = 2816
NJ = DFF // P
RMS_EPS = 1e-6
GN_EPS = 1e-6
RH = 4
DILS = (1, 4, 16)
SB_BASE = 16640
SB_TOP = 229344

V_MIXPRE = 0
V_MIXPOST = 32
V_FFNPRE = 64
V_FFNPOST = 96
V_KVN = 128
V_GN = 136
V_CW = 168
V_CB = V_CW + 4 * 3 * NJ
NV = V_CB + 4 * NJ
C_PI = 0
C_ID = 128
C_DT = 256
C_MPREV = 768
C_MCUR = 896
C_ZETA = 1024
C_EPS = 1028
NCM = 1032


class Buf:
    __slots__ = ("name", "lw", "rd")

    def __init__(self, name=""):
        self.name = name
        self.lw = None
        self.rd = {}


class Chan:
    def __init__(self, nc, name):
        self.sem = nc.alloc_semaphore(name=name)
        self.count = 0
        self.name = name


class Sched:
    def __init__(self, nc):
        self.nc = nc
        self.engs = {"pe": nc.tensor, "act": nc.scalar, "dve": nc.vector, "pool": nc.gpsimd, "sp": nc.sync}
        self.chan = {k: Chan(nc, "c_" + k) for k in self.engs}
        self.waited = {k: {} for k in self.engs}
        self.dchans = []
        self.ninst = 0

    def dchan(self, name):
        c = Chan(self.nc, name)
        self.dchans.append(c)
        return c

    def _wait(self, e, c, v):
        if v <= 0:
            return
        w = self.waited[e]
        if w.get(c, 0) >= v:
            return
        self.engs[e].wait_ge(c.sem, v)
        self.ninst += 1
        w[c] = v

    def _deps(self, e, reads, writes):
        need = {}
        for b in reads:
            if b.lw is not None:
                c, v = b.lw
                if need.get(c, 0) < v:
                    need[c] = v
        for b in writes:
            if b.lw is not None:
                c, v = b.lw
                if need.get(c, 0) < v:
                    need[c] = v
            for c, v in b.rd.items():
                if need.get(c, 0) < v:
                    need[c] = v
        for c, v in need.items():
            self._wait(e, c, v)

    def _record(self, c, v, reads, writes):
        for b in reads:
            if b.rd.get(c, 0) < v:
                b.rd[c] = v
        for b in writes:
            b.lw = (c, v)
            b.rd = {}

    def op(self, e, fn, reads=(), writes=(), inc=True):
        self._deps(e, reads, writes)
        ins = fn(self.engs[e])
        self.ninst += 1
        c = self.chan[e]
        inc = True
        if inc:
            c.count += 1
            ins.then_inc(c.sem, 1)
            v = c.count
        else:
            v = c.count + 1
        self._record(c, v, reads, writes)
        return ins

    def dma(self, ch, out, in_, reads=(), writes=(), e="sp"):
        self._deps(e, reads, writes)
        ins = self.engs[e].dma_start(out=out, in_=in_)
        self.ninst += 1
        ch.count += 16
        ins.then_inc(ch.sem, 16)
        self._record(ch, ch.count, reads, writes)
        return ins

    def barrier(self):
        chans = list(self.chan.values()) + self.dchans
        for e in self.engs:
            for c in chans:
                if c is self.chan[e]:
                    continue
                self._wait(e, c, c.count)


class SbAlloc:
    def __init__(self, nc, base, top=SB_TOP):
        self.nc = nc
        self.off = base
        self.top = top
        self.n = 0

    def __call__(self, shape, dtype, name="t"):
        nb = 1
        for s in shape[1:]:
            nb *= s
        nb *= 2 if dtype == BF16 else 4
        nb = (nb + 63) // 64 * 64
        assert self.off + nb <= self.top, f"SBUF overflow {name} {self.off}+{nb}>{self.top}"
        self.n += 1
        t = self.nc.alloc_sbuf_tensor_at(f"{name}{self.n}", list(shape), dtype, offset=self.off)
        self.off += nb
        return t.ap()

    def fork(self):
        return SbAlloc(self.nc, self.off, self.top)


class Prog:
    def __init__(self, nseq=NSEQ):
        self.nseq = nseq
        nc = self.nc = bass.Bass("TRN2", target_bir_lowering=False)
        S = self.S = Sched(nc)
        dt = nc.dram_tensor

        def ext(name, shape, dtype=F32):
            return dt(name, list(shape), dtype, kind="ExternalInput").ap()

        def scr(name, shape, dtype=BF16):
            return dt(name, list(shape), dtype, kind="Internal").ap()

        self.xin = ext("xT", [nseq, DM, SEQ])
        self.out = dt("out", [nseq, DM, SEQ], F32, kind="ExternalOutput").ap()
        self.wshapes = {
            "w_ret_in": [2, P, 4, KC, 1536],
            "w_ret_out": [2, P, KC, 16, P],
            "w_k": [P, 24, KC, P],
            "w_v": [P, 6, KC, 512],
            "w_q": [2, P, 8, KC, 384],
            "w_o": [2, P, KC, KC, P],
            "w_up": [4, P, NJ, KC, 256],
            "w_down": [4, P, KC, NJ, P],
        }
        self.wf = {k: ext(k, v) for k, v in self.wshapes.items()}
        self.wb = {k: scr(k + "_b", v) for k, v in self.wshapes.items()}
        self.vecs_d = ext("vecs", [P, NV])
        self.cmat_d = ext("cmat", [P, NCM])
        self.rcs_d = ext("rcs", [P, 2, SEQ])
        self.acs_d = ext("acs", [P, 2, SEQ])
        self.kt_d = scr("kt_s", [3, 8, P, SEQ])
        self.va_d = scr("va_s", [3, P, 8, 16 * 192])
        self.Bx = [[Buf(f"x{s}_{t}") for t in range(4)] for s in range(nseq)]
        self.Bwb = {k: Buf(k) for k in self.wshapes}
        self.Bkt = Buf("kt")
        self.Bva = Buf("va")
        self.xsrc = [self.xin[s] for s in range(nseq)]
        self.xdst = [self.out[s] for s in range(nseq)]
        self.ld = S.dchan("ld")
        self.ldw = [S.dchan(f"ldw{i}") for i in range(4)]
        self.st = S.dchan("st")
        self.ps = [nc.alloc_psum_tensor(f"ps{i}", [P, 512], F32).ap() for i in range(8)]
        self.Bps = [Buf(f"ps{i}") for i in range(8)]
        A = self.A0 = SbAlloc(nc, SB_BASE)
        self.vecs = A([P, NV], F32, "vecs")
        self.cmat = A([P, NCM], F32, "cmat")
        self.ones_b = A([P, P], BF16, "ones")
        self.ident_b = A([P, P], BF16, "ident")
        self.pi_b = A([P, P], BF16, "pi")
        self.mask_b = A([P, 2, P], BF16, "mask")
        self.Bconst = Buf("const")
        S.dma(self.ld, self.vecs, self.vecs_d, writes=[self.Bconst])
        S.dma(self.ld, self.cmat, self.cmat_d, writes=[self.Bconst])
        S.op("pool", lambda e: e.memset(self.ones_b, 1.0), writes=[self.Bconst])
        S.op("dve", lambda e: e.tensor_copy(out=self.ident_b, in_=self.cmat[:, C_ID:C_ID + P]), reads=[self.Bconst], writes=[self.Bconst])
        S.op("dve", lambda e: e.tensor_copy(out=self.pi_b, in_=self.cmat[:, C_PI:C_PI + P]), reads=[self.Bconst], writes=[self.Bconst])
        S.op("dve", lambda e: e.tensor_copy(out=self.mask_b, in_=self.cmat[:, C_MPREV:C_MPREV + 2 * P].rearrange("p (a b) -> p a b", a=2)),
             reads=[self.Bconst], writes=[self.Bconst])
        S.barrier()

    def prep(self, names=None):
        S, nc = self.S, self.nc
        A = self.A0.fork()
        NB = 3
        CH = 4096
        fin = [A([P, CH], F32, "pin") for _ in range(NB)]
        fout = [A([P, CH], BF16, "pout") for _ in range(NB)]
        Bin = [Buf() for _ in range(NB)]
        Bout = [Buf() for _ in range(NB)]
        k = 0
        engs = ["dve", "act"]
        for name in (names or list(self.wshapes)):
            shp = self.wshapes[name]
            nl = shp[0] if shp[0] != P else 1
            for l in range(nl):
                src = self.wf[name][l] if shp[0] != P else self.wf[name]
                dst = self.wb[name][l] if shp[0] != P else self.wb[name]
                nd = len(src.shape)
                letters = "abcd"[: nd - 1]
                pat = "p " + " ".join(letters) + " -> p (" + " ".join(letters) + ")"
                src2 = src.rearrange(pat)
                dst2 = dst.rearrange(pat)
                n = src2.shape[1]
                for c0 in range(0, n, CH):
                    w = min(CH, n - c0)
                    i = k % NB
                    S.dma(self.ld, fin[i][:, :w], src2[:, c0:c0 + w], writes=[Bin[i]])
                    if name == "w_ret_out":
                        per = 16 * P
                        for q0 in range(0, w, P):
                            ec = ((c0 + q0) % per) // P
                            col = V_GN + l * 16 + ec
                            S.op("dve", lambda e, i=i, q0=q0, col=col: e.tensor_scalar(
                                out=fout[i][:, q0:q0 + P], in0=fin[i][:, q0:q0 + P], scalar1=self.vecs[:, col:col + 1],
                                scalar2=None, op0=ALU.mult), reads=[Bin[i], self.Bconst], writes=[Bout[i]])
                    else:
                        en = engs[k % 2]
                        if en == "act":
                            S.op("act", lambda e, i=i, w=w: e.copy(out=fout[i][:, :w], in_=fin[i][:, :w]), reads=[Bin[i]], writes=[Bout[i]])
                        else:
                            S.op(en, lambda e, i=i, w=w: e.tensor_copy(out=fout[i][:, :w], in_=fin[i][:, :w]), reads=[Bin[i]], writes=[Bout[i]])
                    S.dma(self.st, dst2[:, c0:c0 + w], fout[i][:, :w], reads=[Bout[i]], writes=[self.Bwb[name]])
                    k += 1
        S.barrier()

    def x_tile(self, ap_seq, tt):
        return ap_seq[:, tt * 512:(tt + 1) * 512].rearrange("(kc p) t -> p kc t", p=P)

    def rstd_from(self, src, sq, psb, rstd, Bsrc, Bsq, Brstd):
        S = self.S
        lvl = int(getattr(self, "dbglvl", 9))
        if lvl < 2:
            return
        S.op("act", lambda e: e.activation(out=sq, in_=src, func=AF.Square), reads=[Bsrc], writes=[Bsq])
        if lvl < 3:
            return
        for kc in range(KC):
            S.op("pe", lambda e, kc=kc: e.matmul(self.ps[psb], lhsT=self.ones_b, rhs=sq[:, kc, :], start=(kc == 0), stop=(kc == KC - 1)),
                 reads=[Bsq, self.Bconst], writes=[self.Bps[psb]], inc=(kc == KC - 1))
        if lvl < 4:
            return
        S.op("act", lambda e: e.activation(out=rstd, in_=self.ps[psb], func=AF.Sqrt, bias=RMS_EPS, scale=1.0 / DM),
             reads=[self.Bps[psb]], writes=[Brstd])
        if lvl < 5:
            return
        S.op("dve", lambda e: e.reciprocal(out=rstd, in_=rstd), reads=[Brstd], writes=[Brstd])

    def phase_hT(self, s, A, hT, BhT, gcol):
        S = self.S
        xt = [A([P, KC, 512], F32, "xt") for _ in range(2)]
        sq = A([P, KC, 512], BF16, "sq")
        rs = [A([P, 512], F32, "rs") for _ in range(2)]
        Bxt = [Buf() for _ in range(2)]
        Bsq = Buf()
        Brs = [Buf() for _ in range(2)]
        for tt in range(4):
            i = tt % 2
            S.dma(self.ld, xt[i], self.x_tile(self.xsrc[s], tt), reads=[self.Bx[s][tt]], writes=[Bxt[i]])
            self.rstd_from(xt[i], sq, tt % 2, rs[i], Bxt[i], Bsq, Brs[i])
            if int(getattr(self, "dbglvl", 9)) < 6:
                continue
            for kc in range(KC):
                S.op("dve", lambda e, kc=kc, i=i, tt=tt: e.scalar_tensor_tensor(
                    out=hT[:, kc, tt * 512:(tt + 1) * 512], in0=xt[i][:, kc, :], scalar=self.vecs[:, gcol + kc:gcol + kc + 1],
                    in1=rs[i], op0=ALU.mult, op1=ALU.mult), reads=[Bxt[i], Brs[i], self.Bconst], writes=[BhT[tt]])

    def post_norm_residual(self, s, tt, fT, BfT, xt, Bxt, sq, Bsq, rs, Brs, gcol, psb):
        S = self.S
        self.rstd_from(fT, sq, psb, rs, BfT, Bsq, Brs)
        for kc in range(KC):
            S.op("pool", lambda e, kc=kc: e.tensor_tensor(out=fT[:, kc, :], in0=fT[:, kc, :], in1=rs, op=ALU.mult),
                 reads=[BfT, Brs], writes=[BfT])
        for kc in range(KC):
            S.op("dve", lambda e, kc=kc: e.scalar_tensor_tensor(
                out=xt[:, kc, :], in0=fT[:, kc, :], scalar=self.vecs[:, gcol + kc:gcol + kc + 1], in1=xt[:, kc, :],
                op0=ALU.mult, op1=ALU.add), reads=[BfT, Bxt, self.Bconst], writes=[Bxt])
        S.dma(self.st, self.x_tile(self.xdst[s], tt), xt, reads=[Bxt], writes=[self.Bx[s][tt]])

    def out_proj_phase(self, s, A, wname, l, nk, actT, BactT, gcol):
        S = self.S
        xt = [A([P, KC, 512], F32, "xt") for _ in range(2)]
        fT = A([P, KC, 512], F32, "fT")
        sq = A([P, KC, 512], BF16, "sq")
        rs = A([P, 512], F32, "rs")
        NW = 3
        wd = [A([P, nk, P], BF16, "wd") for _ in range(NW)]
        Bxt = [Buf() for _ in range(2)]
        BfT, Bsq, Brs = Buf(), Buf(), Buf()
        Bwd = [Buf() for _ in range(NW)]
        wsrc = self.wb[wname][l]
        seq = [(tt, dc) for tt in range(4) for dc in range(KC)]

        def loadw(n):
            tt, dc = seq[n]
            S.dma(self.ldw[n % NW], wd[n % NW], wsrc[:, dc], reads=[self.Bwb[wname]], writes=[Bwd[n % NW]])

        loadw(0)
        loadw(1)
        S.dma(self.ld, xt[0], self.x_tile(self.xsrc[s], 0), reads=[self.Bx[s][0]], writes=[Bxt[0]])
        for n, (tt, dc) in enumerate(seq):
            if n + 2 < len(seq):
                loadw(n + 2)
            if dc == 0 and tt + 1 < 4:
                S.dma(self.ld, xt[(tt + 1) % 2], self.x_tile(self.xsrc[s], tt + 1), reads=[self.Bx[s][tt + 1]], writes=[Bxt[(tt + 1) % 2]])
            pb = 2 + (n % 2)
            w = wd[n % NW]
            for j in range(nk):
                S.op("pe", lambda e, j=j, w=w, pb=pb, tt=tt: e.matmul(self.ps[pb], lhsT=w[:, j, :], rhs=actT[:, j, tt * 512:(tt + 1) * 512],
                                                               start=(j == 0), stop=(j == nk - 1)),
                     reads=[Bwd[n % NW], BactT], writes=[self.Bps[pb]], inc=(j == nk - 1))
            S.op("act", lambda e, dc=dc, pb=pb: e.copy(out=fT[:, dc, :], in_=self.ps[pb]), reads=[self.Bps[pb]], writes=[BfT])
            if dc == KC - 1:
                self.post_norm_residual(s, tt, fT, BfT, xt[tt % 2], Bxt[tt % 2], sq, Bsq, rs, Brs, gcol, tt % 2)

    def ffn(self, l, s):
        S = self.S
        A = self.A0.fork()
        aT = A([P, NJ, SEQ], BF16, "aT")
        BaT = Buf()
        R = A.fork()
        hT = R([P, KC, SEQ], BF16, "hT")
        BhT = [Buf() for _ in range(4)]
        self.phase_hT(s, self.A0.fork(), hT, BhT, V_FFNPRE + 8 * l)
        S.barrier()
        if getattr(self, "dbg", "") == "A":
            return
        NW = 3
        wu = [R([P, KC, 256], BF16, "wu") for _ in range(NW)]
        Bwu = [Buf() for _ in range(NW)]
        gb = R([P, 2 + SEQ], F32, "gb")
        Bgb = [Buf() for _ in range(4)]
        ct = [R([P, 512], F32, "ct") for _ in range(2)]
        cg = [R([P, 512], F32, "cg") for _ in range(2)]
        Bct = [Buf() for _ in range(2)]
        Bcg = [Buf() for _ in range(2)]
        S.op("pool", lambda e: e.memset(gb[:, 0:2], 0.0), writes=[Bgb[0]])
        wsrc = self.wb["w_up"][l]

        def loadw(j):
            S.dma(self.ldw[j % NW], wu[j % NW], wsrc[:, j], reads=[self.Bwb["w_up"]], writes=[Bwu[j % NW]])

        loadw(0)
        loadw(1)
        cwb = V_CW + l * 3 * NJ
        cbb = V_CB + l * NJ
        n = 0
        for j in range(NJ):
            if j + 2 < NJ:
                loadw(j + 2)
            w = wu[j % NW]
            for tt in range(4):
                pg, pv = 4 + 2 * (n % 2), 5 + 2 * (n % 2)
                i = n % 2
                cols = slice(tt * 512, (tt + 1) * 512)
                for half, pb in ((0, pg), (1, pv)):
                    for kc in range(KC):
                        S.op("pe", lambda e, kc=kc, pb=pb, half=half, w=w, cols=cols: e.matmul(
                            self.ps[pb], lhsT=w[:, kc, half * P:(half + 1) * P], rhs=hT[:, kc, cols], start=(kc == 0), stop=(kc == KC - 1)),
                            reads=[Bwu[j % NW], BhT[tt]], writes=[self.Bps[pb]], inc=(kc == KC - 1))
                S.op("act", lambda e, pg=pg, tt=tt: e.copy(out=gb[:, 2 + tt * 512:2 + (tt + 1) * 512], in_=self.ps[pg]),
                     reads=[self.Bps[pg]], writes=[Bgb[tt]])
                rb = [Bgb[tt]] + ([Bgb[tt - 1]] if tt > 0 else [])
                S.op("dve", lambda e, tt=tt, i=i, j=j: e.tensor_scalar(
                    out=ct[i], in0=gb[:, 2 + tt * 512:2 + (tt + 1) * 512], scalar1=self.vecs[:, cwb + 2 * NJ + j:cwb + 2 * NJ + j + 1],
                    scalar2=self.vecs[:, cbb + j:cbb + j + 1], op0=ALU.mult, op1=ALU.add), reads=rb + [self.Bconst], writes=[Bct[i]])
                for tap in (1, 0):
                    sh = 2 - tap
                    S.op("dve", lambda e, tt=tt, i=i, j=j, tap=tap, sh=sh: e.scalar_tensor_tensor(
                        out=ct[i], in0=gb[:, 2 - sh + tt * 512:2 - sh + (tt + 1) * 512],
                        scalar=self.vecs[:, cwb + tap * NJ + j:cwb + tap * NJ + j + 1], in1=ct[i], op0=ALU.mult, op1=ALU.add),
                        reads=rb + [Bct[i], self.Bconst], writes=[Bct[i]])
                S.op("act", lambda e, i=i: e.activation(out=cg[i], in_=ct[i], func=AF.Gelu_apprx_tanh), reads=[Bct[i]], writes=[Bcg[i]])
                S.op("dve", lambda e, i=i, j=j, cols=cols, pv=pv: e.tensor_tensor(out=aT[:, j, cols], in0=cg[i], in1=self.ps[pv], op=ALU.mult),
                     reads=[Bcg[i], self.Bps[pv]], writes=[BaT])
                n += 1
        S.barrier()
        if getattr(self, "dbg", "") == "B":
            return
        self.out_proj_phase(s, A.fork(), "w_down", l, NJ, aT, BaT, V_FFNPOST + 8 * l)
        S.barrier()


    def ret(self, l, s):
        S = self.S
        A = self.A0.fork()
        yT = A([P, 16, SEQ], BF16, "yT")
        ByT = Buf()
        R = A.fork()
        hT = R([P, KC, SEQ], BF16, "hT")
        BhT = [Buf() for _ in range(4)]
        self.phase_hT(s, self.A0.fork(), hT, BhT, V_MIXPRE + 8 * l)
        S.barrier()
        R2 = R.fork()
        cs = R([P, 2, SEQ], F32, "cs")
        Bcs = Buf()
        S.dma(self.ld, cs, self.rcs_d, writes=[Bcs])
        wsl = R([P, KC, 1536], BF16, "wsl")
        Bw = Buf()
        qk = [R([P, 4, 512], BF16, "qk") for _ in range(2)]
        vv = [R([P, 4, 512], BF16, "vv") for _ in range(2)]
        sg = [R([P, 4, 512], BF16, "sg") for _ in range(2)]
        Bqk = [[Buf(), Buf()] for _ in range(2)]
        Bvv = [[Buf() for _ in range(4)] for _ in range(2)]
        Bsg = [[Buf() for _ in range(4)] for _ in range(2)]
        tm = [R([P, 512], F32, "tm") for _ in range(4)]
        Btm = [Buf() for _ in range(4)]
        stf = R([P, 2, 512], F32, "stf")
        stb = R([P, 2, 512], BF16, "stb")
        Bstf, Bstb = Buf(), Buf()
        sT = [R([P, P], BF16, "sT") for _ in range(2)]
        kz = [R([P, 256], BF16, "kz") for _ in range(2)]
        yn = [R([P, 512], F32, "yn") for _ in range(2)]
        gt = [R([P, 512], BF16, "gt") for _ in range(2)]
        bst = [R([P, 6], F32, "bst") for _ in range(2)]
        mv = [R([P, 2], F32, "mv") for _ in range(2)]
        rg = [R([P, 2], F32, "rg") for _ in range(2)]
        BsT = [Buf() for _ in range(2)]
        Bkz = [Buf() for _ in range(2)]
        Byn = [Buf() for _ in range(2)]
        Bgt = [Buf() for _ in range(2)]
        Bsm = [Buf() for _ in range(2)]
        ps, Bps = self.ps, self.Bps
        psK = ps[3].bitcast(BF16)[:, 0:256]
        psT = ps[3].bitcast(BF16)[:, 512:1024]
        BpsK, BpsT = Buf(), Buf()
        proj_banks = [0, 1, 7]
        pbn = [0]
        wsrc = self.wb["w_ret_in"][l]
        units = [(h, st) for h in range(RH) for st in range(4)]

        def nextbank():
            b = proj_banks[pbn[0] % 3]
            pbn[0] += 1
            return b

        def load_w(h):
            S.dma(self.ldw[0], wsl, wsrc[:, h], reads=[self.Bwb["w_ret_in"]], writes=[Bw])

        def qk_pair(u, which):
            h, st = units[u]
            i = u % 2
            cols = slice(st * 512, (st + 1) * 512)
            banks = []
            for half in range(2):
                fi = which * 2 + half
                b = nextbank()
                banks.append(b)
                for kc in range(KC):
                    S.op("pe", lambda e, kc=kc, b=b, fi=fi: e.matmul(ps[b], lhsT=wsl[:, kc, fi * P:(fi + 1) * P], rhs=hT[:, kc, cols],
                                                                 start=(kc == 0), stop=(kc == KC - 1)),
                         reads=[Bw, BhT[st]], writes=[Bps[b]])
            b0, b1 = banks
            cosv, sinv = cs[:, 0, cols], cs[:, 1, cols]
            o = which * 2
            S.op("dve", lambda e: e.tensor_tensor(out=tm[0], in0=ps[b0], in1=cosv, op=ALU.mult), reads=[Bps[b0], Bcs], writes=[Btm[0]])
            S.op("dve", lambda e: e.tensor_tensor(out=tm[1], in0=ps[b1], in1=sinv, op=ALU.mult), reads=[Bps[b1], Bcs], writes=[Btm[1]])
            S.op("dve", lambda e: e.tensor_tensor(out=tm[2], in0=ps[b1], in1=cosv, op=ALU.mult), reads=[Bps[b1], Bcs], writes=[Btm[2]])
            S.op("dve", lambda e: e.tensor_tensor(out=tm[3], in0=ps[b0], in1=sinv, op=ALU.mult), reads=[Bps[b0], Bcs], writes=[Btm[3]])
            S.op("dve", lambda e: e.tensor_tensor(out=qk[i][:, o, :], in0=tm[0], in1=tm[1], op=ALU.subtract),
                 reads=[Btm[0], Btm[1]], writes=[Bqk[i][which]])
            S.op("dve", lambda e: e.tensor_tensor(out=qk[i][:, o + 1, :], in0=tm[2], in1=tm[3], op=ALU.add),
                 reads=[Btm[2], Btm[3]], writes=[Bqk[i][which]])

        def vg_part(u, c):
            h, st = units[u]
            i = u % 2
            t0 = st * 512 + c * P
            for which in range(2):
                b = nextbank()
                for kc in range(KC):
                    S.op("pe", lambda e, kc=kc, b=b, which=which: e.matmul(
                        ps[b], lhsT=hT[:, kc, t0:t0 + P], rhs=wsl[:, kc, 512 + which * 512:1024 + which * 512],
                        start=(kc == 0), stop=(kc == KC - 1)), reads=[Bw, BhT[st]], writes=[Bps[b]])
                if which == 0:
                    S.op("act", lambda e, b=b: e.copy(out=vv[i][:, c, :], in_=ps[b]), reads=[Bps[b]], writes=[Bvv[i][c]])
                else:
                    S.op("act", lambda e, b=b: e.activation(out=sg[i][:, c, :], in_=ps[b], func=AF.Silu), reads=[Bps[b]], writes=[Bsg[i][c]])

        def proj_parts(u):
            return [lambda: (qk_pair(u, 0), vg_part(u, 0)), lambda: (qk_pair(u, 1), vg_part(u, 1)),
                    lambda: vg_part(u, 2), lambda: vg_part(u, 3)]

        def front(u, c):
            h, st = units[u]
            i = u % 2
            k = c % 2
            cc = slice(c * P, (c + 1) * P)
            for j in range(2):
                S.op("pe", lambda e, j=j: e.matmul(ps[2][:, 0:P], lhsT=qk[i][:, 2 + j, cc], rhs=qk[i][:, j, cc], start=(j == 0), stop=(j == 1)),
                     reads=[Bqk[i][0], Bqk[i][1]], writes=[Bps[2]])
            for j in range(2):
                S.op("pe", lambda e, j=j: e.transpose(psK[:, j * P:(j + 1) * P], qk[i][:, 2 + j, cc], self.ident_b),
                     reads=[Bqk[i][1], self.Bconst], writes=[BpsK])
            S.op("dve", lambda e: e.tensor_tensor(out=sT[k], in0=ps[2][:, 0:P], in1=self.cmat[:, C_DT + h * P:C_DT + (h + 1) * P], op=ALU.mult),
                 reads=[Bps[2], self.Bconst], writes=[BsT[k]])
            S.op("act", lambda e: e.activation(out=kz[k], in_=psK, func=AF.Identity, scale=self.cmat[:, C_ZETA + h:C_ZETA + h + 1]),
                 reads=[BpsK, self.Bconst], writes=[Bkz[k]])

        def back(u, c):
            h, st = units[u]
            i = u % 2
            k = c % 2
            cg = st * 4 + c
            cc = slice(c * P, (c + 1) * P)
            lg = math.log(1.0 - 2.0 ** (-5.0 - h))
            gC = math.exp(128.0 * lg)
            S.op("pe", lambda e: e.matmul(ps[4], lhsT=sT[k], rhs=vv[i][:, c, :], start=True, stop=(cg == 0)),
                 reads=[BsT[k], Bvv[i][c]], writes=[Bps[4]])
            if cg > 0:
                for j in range(2):
                    S.op("pe", lambda e, j=j: e.matmul(ps[4], lhsT=qk[i][:, j, cc], rhs=stb[:, j, :], start=False, stop=(j == 1)),
                         reads=[Bqk[i][0], Bstb], writes=[Bps[4]])
            if cg < 15:
                for j in range(2):
                    S.op("pe", lambda e, j=j: e.matmul(ps[5 + j], lhsT=kz[k][:, j * P:(j + 1) * P], rhs=vv[i][:, c, :], start=True, stop=True),
                         reads=[Bkz[k], Bvv[i][c]], writes=[Bps[5 + j]])
                for j in range(2):
                    if cg == 0:
                        S.op("dve", lambda e, j=j: e.tensor_copy(out=stf[:, j, :], in_=ps[5 + j]), reads=[Bps[5 + j]], writes=[Bstf])
                    else:
                        S.op("dve", lambda e, j=j: e.scalar_tensor_tensor(out=stf[:, j, :], in0=stf[:, j, :], scalar=gC, in1=ps[5 + j],
                                                                         op0=ALU.mult, op1=ALU.add), reads=[Bps[5 + j], Bstf], writes=[Bstf])
                S.op("act", lambda e: e.copy(out=stb, in_=stf), reads=[Bstf], writes=[Bstb])
            S.op("dve", lambda e: e.bn_stats(out=bst[k], in_=ps[4]), reads=[Bps[4]], writes=[Bsm[k]])
            S.op("dve", lambda e: e.bn_aggr(out=mv[k], in_=bst[k]), reads=[Bsm[k]], writes=[Bsm[k]])
            S.op("act", lambda e: e.activation(out=rg[k][:, 0:1], in_=mv[k][:, 1:2], func=AF.Sqrt, bias=self.cmat[:, C_EPS + h:C_EPS + h + 1], scale=1.0),
                 reads=[Bsm[k], self.Bconst], writes=[Bsm[k]])
            S.op("dve", lambda e: e.reciprocal(out=rg[k][:, 0:1], in_=rg[k][:, 0:1]), reads=[Bsm[k]], writes=[Bsm[k]])
            S.op("dve", lambda e: e.scalar_tensor_tensor(out=rg[k][:, 1:2], in0=mv[k][:, 0:1], scalar=-1.0, in1=rg[k][:, 0:1], op0=ALU.mult, op1=ALU.mult),
                 reads=[Bsm[k]], writes=[Bsm[k]])
            S.op("act", lambda e: e.activation(out=yn[k], in_=ps[4], func=AF.Identity, bias=rg[k][:, 1:2], scale=rg[k][:, 0:1]),
                 reads=[Bps[4], Bsm[k]], writes=[Byn[k]])
            S.op("dve", lambda e: e.tensor_tensor(out=gt[k], in0=yn[k], in1=sg[i][:, c, :], op=ALU.mult),
                 reads=[Byn[k], Bsg[i][c]], writes=[Bgt[k]])

        def ytr(u, c):
            h, st = units[u]
            k = c % 2
            cg = st * 4 + c
            for jj in range(4):
                S.op("pe", lambda e, jj=jj: e.transpose(psT[:, jj * P:(jj + 1) * P], gt[k][:, jj * P:(jj + 1) * P], self.ident_b),
                     reads=[Bgt[k], self.Bconst], writes=[BpsT])
            S.op("act", lambda e: e.copy(out=yT[:, 4 * h:4 * h + 4, cg * P:(cg + 1) * P], in_=psT.rearrange("p (a b) -> p a b", a=4)),
                 reads=[BpsT], writes=[ByT])

        load_w(0)
        for f in proj_parts(0):
            f()
        pending = None
        for u in range(len(units)):
            nxt = [None] * 4
            if u + 1 < len(units):
                if units[u + 1][0] != units[u][0]:
                    load_w(units[u + 1][0])
                nxt = proj_parts(u + 1)
            for c in range(4):
                front(u, c)
                if nxt[c] is not None:
                    nxt[c]()
                back(u, c)
                if pending is not None:
                    ytr(*pending)
                pending = (u, c)
        ytr(*pending)
        S.barrier()
        self.out_proj_phase(s, R2, "w_ret_out", l, 16, yT, ByT, V_MIXPOST + 8 * l)
        S.barrier()


    @staticmethod
    def gcols(ap2d, g, u):
        if g == 0:
            return ap2d[:, u * 512:(u + 1) * 512]
        if g == 1:
            return ap2d[:, u::4]
        return ap2d.rearrange("p (i r) -> p r i", r=16)[:, 4 * u:4 * u + 4, :]

    @staticmethod
    def gview(ap512, g):
        return ap512 if g < 2 else ap512.rearrange("p (r i) -> p r i", r=4)

    @staticmethod
    def chunk_tokens(ap2d, g, c):
        if g == 0:
            return ap2d[..., c * P:(c + 1) * P] if False else ap2d[:, c * P:(c + 1) * P]
        if g == 1:
            st = 512 * (c % 4) + c // 4
            return ap2d[:, st:st + 509:4]
        return ap2d[:, c::16]

    def rot_proj(self, g, u, w_lhsT, hT, BhT, Bw, cs, Bcs, qraw, Bqraw, t1, t2, Bt, out512, Bout, pb, pb2):
        S, ps, Bps = self.S, self.ps, self.Bps
        for kc in range(KC):
            S.op("pe", lambda e, kc=kc: e.matmul(self.gview(ps[pb], g), lhsT=w_lhsT[:, kc, :], rhs=self.gcols(hT[:, kc, :], g, u),
                                               start=(kc == 0), stop=(kc == KC - 1)), reads=[Bw] + BhT, writes=[Bps[pb]])
        S.op("act", lambda e: e.copy(out=qraw, in_=ps[pb]), reads=[Bps[pb]], writes=[Bqraw])
        S.op("pe", lambda e: e.matmul(ps[pb2], lhsT=self.pi_b, rhs=qraw, start=True, stop=True), reads=[Bqraw, self.Bconst], writes=[Bps[pb2]])
        S.op("dve", lambda e: e.tensor_tensor(out=self.gview(t1, g), in0=self.gview(ps[pb2], g), in1=self.gcols(cs[:, 1, :], g, u), op=ALU.mult),
             reads=[Bps[pb2], Bcs], writes=[Bt[0]])
        S.op("dve", lambda e: e.tensor_tensor(out=self.gview(t2, g), in0=self.gview(ps[pb], g), in1=self.gcols(cs[:, 0, :], g, u), op=ALU.mult),
             reads=[Bps[pb], Bcs], writes=[Bt[1]])
        S.op("dve", lambda e: e.tensor_tensor(out=out512, in0=t1, in1=t2, op=ALU.add), reads=[Bt[0], Bt[1]], writes=[Bout])

    def kv(self, s):
        S = self.S
        A = self.A0.fork()
        hT = A([P, KC, SEQ], BF16, "hT")
        BhT = [Buf() for _ in range(4)]
        self.phase_hT(s, A.fork(), hT, BhT, V_KVN)
        S.barrier()
        R = A.fork()
        cs = R([P, 2, SEQ], F32, "cs")
        Bcs = Buf()
        S.dma(self.ld, cs, self.acs_d, writes=[Bcs])
        wk = [R([P, KC, P], BF16, "wk") for _ in range(3)]
        Bwk = [Buf() for _ in range(3)]
        qraw = [R([P, 512], BF16, "qraw") for _ in range(2)]
        t1 = [R([P, 512], F32, "t1") for _ in range(2)]
        t2 = [R([P, 512], F32, "t2") for _ in range(2)]
        Bqraw = [Buf() for _ in range(2)]
        Bt = [[Buf(), Buf()] for _ in range(2)]
        kst = [R([P, SEQ], BF16, "kst") for _ in range(2)]
        Bkst = [Buf() for _ in range(2)]
        wv = R([P, 2, KC, 512], BF16, "wv")
        Bwv = Buf()
        stg = R([P, 8, 16, 192], BF16, "stg")
        Bstg = Buf()
        S.op("dve", lambda e: e.memset(stg.rearrange("p a c x -> p (a c x)"), 1.0), writes=[Bstg])
        wsrc = self.wb["w_k"]

        def loadk(gp):
            S.dma(self.ldw[gp % 3], wk[gp % 3], wsrc[:, gp], reads=[self.Bwb["w_k"]], writes=[Bwk[gp % 3]])

        loadk(0)
        loadk(1)
        n = 0
        for gp in range(24):
            g, hp = gp // 8, gp % 8
            if gp + 2 < 24:
                loadk(gp + 2)
            ks = kst[gp % 2]
            for u in range(4):
                i = n % 2
                self.rot_proj(g, u, wk[gp % 3], hT, BhT, Bwk[gp % 3], cs, Bcs, qraw[i], Bqraw[i], t1[i], t2[i], Bt[i],
                              ks[:, u * 512:(u + 1) * 512], Bkst[gp % 2], 0 + i, 2 + i)
                n += 1
            S.dma(self.st, self.kt_d[g, hp], ks, reads=[Bkst[gp % 2]], writes=[self.Bkt])
        ps, Bps = self.ps, self.Bps
        n = 0
        for g in range(3):
            S.dma(self.ldw[3], wv, self.wb["w_v"][:, 2 * g:2 * g + 2], reads=[self.Bwb["w_v"]], writes=[Bwv])
            for c in range(16):
                for half in range(2):
                    pb = 4 + (n % 4)
                    n += 1
                    for kc in range(KC):
                        S.op("pe", lambda e, kc=kc, pb=pb, half=half: e.matmul(ps[pb], lhsT=self.chunk_tokens(hT[:, kc, :], g, c), rhs=wv[:, half, kc, :],
                                                                         start=(kc == 0), stop=(kc == KC - 1)), reads=[Bwv] + BhT, writes=[Bps[pb]])
                    pv = ps[pb].rearrange("p (a h d) -> p a h d", a=4, h=2)
                    for hh in range(2):
                        o = stg[:, half * 4:half * 4 + 4, c, hh * 128:hh * 128 + 64]
                        if hh == 0:
                            S.op("act", lambda e, o=o, pv=pv: e.copy(out=o, in_=pv[:, :, 0, :]), reads=[Bps[pb]], writes=[Bstg])
                        else:
                            S.op("dve", lambda e, o=o, pv=pv: e.tensor_copy(out=o, in_=pv[:, :, 1, :]), reads=[Bps[pb]], writes=[Bstg])
            S.dma(self.st, self.va_d[g], stg.rearrange("p a c x -> p a (c x)"), reads=[Bstg], writes=[self.Bva])
        S.barrier()

    def att(self, l, s):
        S = self.S
        li = l - 2
        A = self.A0.fork()
        oT = A([P, 8, SEQ], BF16, "oT")
        BoT = Buf()
        R = A.fork()
        R0 = R.fork()
        hT = R([P, KC, SEQ], BF16, "hT")
        BhT = [Buf() for _ in range(4)]
        self.phase_hT(s, R.fork(), hT, BhT, V_MIXPRE + 8 * l)
        S.barrier()
        cs = R([P, 2, SEQ], F32, "cs")
        Bcs = Buf()
        S.dma(self.ld, cs, self.acs_d, writes=[Bcs])
        wq = [R([P, KC, 384], BF16, "wq") for _ in range(2)]
        Bwq = [Buf() for _ in range(2)]
        kt = R([P, 3, SEQ], BF16, "kt")
        vt = R([P, 3, 16 * 192], BF16, "vt")
        Bkt, Bvt = Buf(), Buf()
        qT = R([P, 3, SEQ], BF16, "qT")
        BqT = [Buf() for _ in range(3)]
        acc = R([P, 2, SEQ], F32, "acc")
        Bacc = Buf()
        qraw = [R([P, 512], BF16, "qraw") for _ in range(2)]
        t1 = [R([P, 512], F32, "t1") for _ in range(2)]
        t2 = [R([P, 512], F32, "t2") for _ in range(2)]
        Bqraw = [Buf() for _ in range(2)]
        Bt = [[Buf(), Buf()] for _ in range(2)]
        pP = [R([P, 2, 256], BF16, "pP") for _ in range(2)]
        PT = [R([P, 2, 2, P], BF16, "PT") for _ in range(2)]
        BpP = [Buf() for _ in range(2)]
        BPT = [Buf() for _ in range(2)]
        tden = R([P, SEQ], F32, "tden")
        Btden = Buf()
        ps, Bps = self.ps, self.Bps
        psTb = ps[6].bitcast(BF16)
        psT = [psTb[:, k * 512:(k + 1) * 512].rearrange("p (h c q) -> p h c q", h=2, c=2) for k in range(2)]
        psU = [ps[7][:, k * 256:(k + 1) * 256].rearrange("p (h q) -> p h q", h=2) for k in range(2)]
        BpsT = [Buf() for _ in range(2)]
        BpsU = [Buf() for _ in range(2)]
        wsrc = self.wb["w_q"][li]

        def loadq(hp):
            S.dma(self.ldw[hp % 2], wq[hp % 2], wsrc[:, hp], reads=[self.Bwb["w_q"]], writes=[Bwq[hp % 2]])

        loadq(0)
        n = 0
        m = 0
        for hp in range(8):
            if hp + 1 < 8:
                loadq(hp + 1)
            S.dma(self.ldw[2], kt, self.kt_d[:, hp].rearrange("g p t -> p g t"), reads=[self.Bkt], writes=[Bkt])
            S.dma(self.ldw[3], vt, self.va_d[:, :, hp].rearrange("g p x -> p g x"), reads=[self.Bva], writes=[Bvt])
            w = wq[hp % 2]
            for g in range(3):
                for u in range(4):
                    i = n % 2
                    self.rot_proj(g, u, w[:, :, g * P:(g + 1) * P], hT, BhT, Bwq[hp % 2], cs, Bcs, qraw[i], Bqraw[i], t1[i], t2[i], Bt[i],
                                  qT[:, g, u * 512:(u + 1) * 512], BqT[g], 0 + i, 2 + i)
                    n += 1
            for g in range(3):
                for c in range(16):
                    has_prev = (c > 0) if g == 0 else ((c % 4) > 0 if g == 1 else False)
                    nkc = 2 if has_prev else 1
                    nk = nkc * P
                    k0 = (c - 1) * P if has_prev else c * P
                    k = m % 2
                    m += 1
                    pS = 4 + k
                    pSv = ps[pS].rearrange("p (h x) -> p h x", h=2)
                    for hh in range(2):
                        rows = slice(hh * 64, (hh + 1) * 64)
                        S.op("pe", lambda e, hh=hh, rows=rows: e.matmul(pSv[:, hh, 0:nk], lhsT=qT[rows, g, c * P:(c + 1) * P], rhs=kt[rows, g, k0:k0 + nk],
                                                                     start=True, stop=True), reads=[BqT[g], Bkt], writes=[Bps[pS]])
                    S.op("act", lambda e: e.activation(out=pP[k][:, :, 0:nk], in_=pSv[:, :, 0:nk], func=AF.Exp, scale=0.125),
                         reads=[Bps[pS]], writes=[BpP[k]])
                    for hh in range(2):
                        for kc in range(nkc):
                            S.op("pe", lambda e, hh=hh, kc=kc: e.transpose(psT[k][:, hh, kc, :], pP[k][:, hh, kc * P:(kc + 1) * P], self.ident_b),
                                 reads=[BpP[k], self.Bconst], writes=[BpsT[k]])
                    msk = self.mask_b[:, 2 - nkc:2, :].unsqueeze(1).broadcast_to([P, 2, nkc, P])
                    S.op("dve", lambda e, msk=msk: e.tensor_tensor(out=PT[k][:, :, 0:nkc, :], in0=psT[k][:, :, 0:nkc, :], in1=msk, op=ALU.mult),
                         reads=[BpsT[k], self.Bconst], writes=[BPT[k]])
                    for hh in range(2):
                        for kc in range(nkc):
                            ch = (c - 1 + kc) if has_prev else c
                            S.op("pe", lambda e, hh=hh, kc=kc, ch=ch: e.matmul(psU[k][:, hh, :], lhsT=vt[:, g, ch * 192 + hh * 64:ch * 192 + hh * 64 + P],
                                                                            rhs=PT[k][:, hh, kc, :], start=(kc == 0), stop=(kc == nkc - 1)),
                                 reads=[Bvt, BPT[k]], writes=[BpsU[k]])
                    if g == 0:
                        S.op("act", lambda e: e.copy(out=acc[:, :, c * P:(c + 1) * P], in_=psU[k]), reads=[BpsU[k]], writes=[Bacc])
                    else:
                        if g == 1:
                            st = 512 * (c % 4) + c // 4
                            dst = acc[:, :, st:st + 509:4]
                        else:
                            dst = acc[:, :, c::16]
                        S.op("dve", lambda e, dst=dst: e.tensor_tensor(out=dst, in0=dst, in1=psU[k], op=ALU.add), reads=[BpsU[k], Bacc], writes=[Bacc])
            S.op("act", lambda e: e.copy(out=tden[0:64, :], in_=acc[64:128, 0, :]), reads=[Bacc], writes=[Btden])
            S.op("act", lambda e: e.copy(out=tden[64:128, :], in_=acc[0:64, 1, :]), reads=[Bacc], writes=[Btden])
            S.op("dve", lambda e: e.reciprocal(out=tden, in_=tden), reads=[Btden], writes=[Btden])
            S.op("dve", lambda e, hp=hp: e.tensor_tensor(out=oT[0:64, hp, :], in0=acc[0:64, 0, :], in1=tden[0:64, :], op=ALU.mult),
                 reads=[Bacc, Btden], writes=[BoT])
            S.op("dve", lambda e, hp=hp: e.tensor_tensor(out=oT[64:128, hp, :], in0=acc[64:128, 1, :], in1=tden[64:128, :], op=ALU.mult),
                 reads=[Bacc, Btden], writes=[BoT])
        S.barrier()
        self.out_proj_phase(s, R0, "w_o", li, 8, oT, BoT, V_MIXPOST + 8 * l)
        S.barrier()

    def run_stages(self, stages=None):
        if stages is None:
            stages = [("prep", None)]
            for s in range(self.nseq):
                for l in range(4):
                    stages.append(("ret" if l < 2 else "att", l, s))
                    stages.append(("ffn", l, s))
                    if l == 1:
                        stages.append(("kv", s))
        for st in stages:
            kind = st[0]
            if kind == "prep":
                self.prep(st[1])
            elif kind == "ffn":
                self.ffn(st[1], st[2])
                self.xsrc[st[2]] = self.xdst[st[2]]
            elif kind == "ret":
                self.ret(st[1], st[2])
                self.xsrc[st[2]] = self.xdst[st[2]]
            elif kind == "att":
                self.att(st[1], st[2])
                self.xsrc[st[2]] = self.xdst[st[2]]
            elif kind == "kv":
                self.kv(st[1])

    def finish(self):
        S = self.S
        S.barrier()
        return self.nc


def _kxm(w):
    K, F = w.shape
    return np.ascontiguousarray(w.reshape(K // P, P, F).transpose(1, 0, 2))


def host_weights(inp):
    out = {}
    wi = np.asarray(inp["ret_w_in"], np.float32)
    cols = []
    for h in range(RH):
        cols += list(range(h * 256, (h + 1) * 256))
        cols += list(range(1024 + h * 256, 1024 + (h + 1) * 256))
        cols += list(range(2048 + h * 512, 2048 + (h + 1) * 512))
        cols += list(range(4096 + h * 512, 4096 + (h + 1) * 512))
    cols = np.array(cols)
    out["w_ret_in"] = np.stack([_kxm(wi[l][:, cols]).reshape(P, KC, 4, 1536).transpose(0, 2, 1, 3) for l in range(2)])
    wo = np.asarray(inp["ret_w_out"], np.float32)
    out["w_ret_out"] = np.stack([_kxm(wo[l]).reshape(P, 16, KC, P).transpose(0, 2, 1, 3) for l in range(2)])
    wkv = np.asarray(inp["att_w_kv"], np.float32)
    out["w_k"] = _kxm(wkv[:, :3072]).reshape(P, KC, 24, P).transpose(0, 2, 1, 3)
    out["w_v"] = _kxm(wkv[:, 3072:]).reshape(P, KC, 6, 512).transpose(0, 2, 1, 3)
    wq = np.asarray(inp["att_w_q"], np.float32)
    out["w_q"] = np.stack([_kxm(wq[l]).reshape(P, KC, 3, 8, P).transpose(0, 3, 1, 2, 4).reshape(P, 8, KC, 384) for l in range(2)])
    wao = np.asarray(inp["att_w_o"], np.float32)
    out["w_o"] = np.stack([_kxm(wao[l]).reshape(P, KC, KC, P).transpose(0, 2, 1, 3) for l in range(2)])
    wu = np.asarray(inp["ffn_w_up"], np.float32)
    out["w_up"] = np.stack([np.concatenate([_kxm(wu[l][:, :DFF]).reshape(P, KC, NJ, P), _kxm(wu[l][:, DFF:]).reshape(P, KC, NJ, P)], axis=3)
                            .transpose(0, 2, 1, 3) for l in range(4)])
    wd = np.asarray(inp["ffn_w_down"], np.float32)
    out["w_down"] = np.stack([_kxm(wd[l]).reshape(P, NJ, KC, P).transpose(0, 2, 1, 3) for l in range(4)])
    out = {k: np.ascontiguousarray(v, dtype=np.float32) for k, v in out.items()}

    def pv(v):
        v = np.asarray(v, np.float32)
        return v.reshape(-1, P).T

    vecs = np.zeros((P, NV), np.float32)
    for l in range(4):
        vecs[:, V_MIXPRE + 8 * l:V_MIXPRE + 8 * l + 8] = pv(inp["norm_mix_pre"][l])
        vecs[:, V_MIXPOST + 8 * l:V_MIXPOST + 8 * l + 8] = pv(inp["norm_mix_post"][l])
        vecs[:, V_FFNPRE + 8 * l:V_FFNPRE + 8 * l + 8] = pv(inp["norm_ffn_pre"][l])
        vecs[:, V_FFNPOST + 8 * l:V_FFNPOST + 8 * l + 8] = pv(inp["norm_ffn_post"][l])
        for tap in range(3):
            vecs[:, V_CW + (l * 3 + tap) * NJ:V_CW + (l * 3 + tap + 1) * NJ] = pv(inp["ffn_conv_w"][l][tap])
        vecs[:, V_CB + l * NJ:V_CB + (l + 1) * NJ] = pv(inp["ffn_conv_b"][l])
    vecs[:, V_KVN:V_KVN + 8] = pv(inp["kv_norm"])
    for l in range(2):
        vecs[:, V_GN + 16 * l:V_GN + 16 * l + 16] = pv(inp["ret_gn_gain"][l])
    out["vecs"] = vecs
    return out


def host_consts():
    c = {}
    pos = np.arange(SEQ, dtype=np.float32)
    inv = (1.0 / (np.float32(10000.0) ** np.linspace(0.0, 1.0, 128, dtype=np.float32))).astype(np.float32)
    ang = (pos[None, :] * inv[:, None]).astype(np.float32)
    c["rcs"] = np.stack([np.cos(ang), np.sin(ang)], axis=1).astype(np.float32)
    invf = (np.float32(500000.0) ** (-np.arange(0, 16, 2, dtype=np.float32) / np.float32(16))).astype(np.float32)
    acs = np.zeros((P, 2, SEQ), np.float32)
    pi = np.zeros((P, P), np.float32)
    for p in range(P):
        hd = p % 64
        if hd < 8:
            a = (pos * invf[hd]).astype(np.float32)
            acs[p, 0] = np.cos(a)
            acs[p, 1] = -np.sin(a)
            pi[p + 8, p] = 1.0
        elif hd < 16:
            a = (pos * invf[hd - 8]).astype(np.float32)
            acs[p, 0] = np.cos(a)
            acs[p, 1] = np.sin(a)
            pi[p - 8, p] = 1.0
        else:
            acs[p, 0] = 1.0
    c["acs"] = acs
    cm = np.zeros((P, NCM), np.float32)
    cm[:, C_PI:C_PI + P] = pi
    cm[:, C_ID:C_ID + P] = np.eye(P, dtype=np.float32)
    n = np.arange(P, dtype=np.float64)
    for h in range(RH):
        lg = math.log(1.0 - 2.0 ** (-5.0 - h))
        dtm = np.where(n[None, :] >= n[:, None], np.exp(-(n[:, None] + 1.0) * lg) / 16.0, 0.0)
        cm[:, C_DT + h * P:C_DT + (h + 1) * P] = dtm
        cm[:, C_ZETA + h] = np.exp((127.0 - n) * lg) / 16.0
        cm[:, C_EPS + h] = GN_EPS * np.exp(-2.0 * (n + 1.0) * lg)
    kj = np.arange(P)
    cm[:, C_MPREV:C_MPREV + P] = (kj[:, None] >= kj[None, :]).astype(np.float32)
    cm[:, C_MCUR:C_MCUR + P] = (kj[:, None] <= kj[None, :]).astype(np.float32)
    c["cmat"] = cm
    return c


def build_program(nseq=NSEQ, stages=None):
    pg = Prog(nseq)
    pg.run_stages(stages)
    return pg.finish()


def kernel(**inputs):
    x = np.asarray(inputs["x"], np.float32)
    hw = host_weights(inputs)
    hc = host_consts()
    nc = build_program(NSEQ)
    in_maps = []
    for c in range(NCORES):
        m = dict(hw)
        m.update(hc)
        m["xT"] = np.ascontiguousarray(x[c * NSEQ:(c + 1) * NSEQ].transpose(0, 2, 1))
        in_maps.append(m)
    res = run_bass_kernel_spmd(nc, in_maps, core_ids=list(range(NCORES)))
    outs = [np.asarray(r["out"], np.float32).transpose(0, 2, 1) for r in res.results]
    return np.ascontiguousarray(np.concatenate(outs, axis=0))
```

```python
import math
import numpy as np
import ml_dtypes
import concourse.bass as bass
import concourse.mybir as mybir
from concourse.bass_utils import run_bass_kernel_spmd

F32 = mybir.dt.float32
BF16 = mybir.dt.bfloat16
AF = mybir.ActivationFunctionType
ALU = mybir.AluOpType

P = 128
SEQ = 2048
DM = 1024
KC = 8
NCORES = 8
BATCH = 32
NSEQ = BATCH // NCORES
DFF = 2816
NJ = DFF // P
RMS_EPS = 1e-6
GN_EPS = 1e-6
RH = 4
DILS = (1, 4, 16)
SB_BASE = 16640
FORCE_INC = False
SB_TOP = 229344

V_MIXPRE = 0
V_MIXPOST = 32
V_FFNPRE = 64
V_FFNPOST = 96
V_KVN = 128
V_GN = 136
V_CW = 168
V_CB = V_CW + 4 * 3 * NJ
NV = V_CB + 4 * NJ
C_PI = 0
C_ID = 128
C_DT = 256
C_MPREV = 768
C_MCUR = 896
C_ZETA = 1024
C_EPS = 1028
NCM = 1032


class Buf:
    __slots__ = ("name", "lw", "rd")

    def __init__(self, name=""):
        self.name = name
        self.lw = None
        self.rd = {}


class Chan:
    def __init__(self, nc, name):
        self.sem = nc.alloc_semaphore(name=name)
        self.count = 0
        self.name = name


class Sched:
    def __init__(self, nc):
        self.nc = nc
        self.engs = {"pe": nc.tensor, "act": nc.scalar, "dve": nc.vector, "pool": nc.gpsimd, "sp": nc.sync}
        self.chan = {k: Chan(nc, "c_" + k) for k in self.engs}
        self.waited = {k: {} for k in self.engs}
        self.dchans = []
        self.ninst = 0

    def dchan(self, name):
        c = Chan(self.nc, name)
        self.dchans.append(c)
        return c

    def _wait(self, e, c, v):
        if v <= 0:
            return
        w = self.waited[e]
        if w.get(c, 0) >= v:
            return
        self.engs[e].wait_ge(c.sem, v)
        self.ninst += 1
        w[c] = v

    def _deps(self, e, reads, writes):
        need = {}
        for b in reads:
            if b.lw is not None:
                c, v = b.lw
                if need.get(c, 0) < v:
                    need[c] = v
        for b in writes:
            if b.lw is not None:
                c, v = b.lw
                if need.get(c, 0) < v:
                    need[c] = v
            for c, v in b.rd.items():
                if need.get(c, 0) < v:
                    need[c] = v
        own = self.chan.get(e)
        for c, v in need.items():
            if c is own and v > c.count:
                continue
            self._wait(e, c, v)

    def _record(self, c, v, reads, writes):
        for b in reads:
            if b.rd.get(c, 0) < v:
                b.rd[c] = v
        for b in writes:
            b.lw = (c, v)
            b.rd = {}

    def op(self, e, fn, reads=(), writes=(), inc=True):
        self._deps(e, reads, writes)
        ins = fn(self.engs[e])
        self.ninst += 1
        c = self.chan[e]
        if FORCE_INC:
            inc = True
        if inc:
            c.count += 1
            ins.then_inc(c.sem, 1)
            v = c.count
        else:
            v = c.count + 1
        self._record(c, v, reads, writes)
        return ins

    def dma(self, ch, out, in_, reads=(), writes=(), e="sp"):
        self._deps(e, reads, writes)
        ins = self.engs[e].dma_start(out=out, in_=in_)
        self.ninst += 1
        ch.count += 16
        ins.then_inc(ch.sem, 16)
        self._record(ch, ch.count, reads, writes)
        return ins

    def barrier(self):
        chans = list(self.chan.values()) + self.dchans
        for e in self.engs:
            for c in chans:
                if c is self.chan[e]:
                    continue
                self._wait(e, c, c.count)


class SbAlloc:
    def __init__(self, nc, base, top=SB_TOP):
        self.nc = nc
        self.off = base
        self.top = top
        self.n = 0

    def __call__(self, shape, dtype, name="t"):
        nb = 1
        for s in shape[1:]:
            nb *= s
        nb *= 2 if dtype == BF16 else 4
        nb = (nb + 63) // 64 * 64
        assert self.off + nb <= self.top, f"SBUF overflow {name} {self.off}+{nb}>{self.top}"
        self.n += 1
        t = self.nc.alloc_sbuf_tensor_at(f"{name}{self.n}", list(shape), dtype, offset=self.off)
        self.off += nb
        return t.ap()

    def fork(self):
        return SbAlloc(self.nc, self.off, self.top)


class Prog:
    def __init__(self, nseq=NSEQ):
        self.nseq = nseq
        nc = self.nc = bass.Bass("TRN2", target_bir_lowering=False)
        S = self.S = Sched(nc)
        dt = nc.dram_tensor

        def ext(name, shape, dtype=F32):
            return dt(name, list(shape), dtype, kind="ExternalInput").ap()

        def scr(name, shape, dtype=BF16):
            return dt(name, list(shape), dtype, kind="Internal").ap()

        self.xin = ext("xT", [nseq, DM, SEQ])
        self.out = dt("out", [nseq, DM, SEQ], F32, kind="ExternalOutput").ap()
        self.wshapes = {
            "w_ret_in": [2, P, 4, KC, 1536],
            "w_ret_out": [2, P, KC, 16, P],
            "w_k": [P, 24, KC, P],
            "w_v": [P, 6, KC, 512],
            "w_q": [2, P, 8, KC, 384],
            "w_o": [2, P, KC, KC, P],
            "w_up": [4, P, NJ, KC, 256],
            "w_down": [4, P, KC, NJ, P],
        }
        self.wf = {k: ext(k, v) for k, v in self.wshapes.items()}
        self.wb = {k: scr(k + "_b", v) for k, v in self.wshapes.items()}
        self.vecs_d = ext("vecs", [P, NV])
        self.cmat_d = ext("cmat", [P, NCM])
        self.rcs_d = ext("rcs", [P, 2, SEQ])
        self.acs_d = ext("acs", [P, 2, SEQ])
        self.kt_d = scr("kt_s", [3, 8, P, SEQ])
        self.va_d = scr("va_s", [3, P, 8, 16 * 192])
        self.Bx = [[Buf(f"x{s}_{t}") for t in range(4)] for s in range(nseq)]
        self.Bwb = {k: Buf(k) for k in self.wshapes}
        self.Bkt = Buf("kt")
        self.Bva = Buf("va")
        self.xsrc = [self.xin[s] for s in range(nseq)]
        self.xdst = [self.out[s] for s in range(nseq)]
        self.ld = S.dchan("ld")
        self.ldw = [S.dchan(f"ldw{i}") for i in range(4)]
        self.st = S.dchan("st")
        self.ps = [nc.alloc_psum_tensor(f"ps{i}", [P, 512], F32).ap() for i in range(8)]
        self.Bps = [Buf(f"ps{i}") for i in range(8)]
        A = self.A0 = SbAlloc(nc, SB_BASE)
        self.vecs = A([P, NV], F32, "vecs")
        self.cmat = A([P, NCM], F32, "cmat")
        self.ones_b = A([P, P], BF16, "ones")
        self.ident_b = A([P, P], BF16, "ident")
        self.pi_b = A([P, P], BF16, "pi")
        self.mask_b = A([P, 2, P], BF16, "mask")
        self.Bconst = Buf("const")
        S.dma(self.ld, self.vecs, self.vecs_d, writes=[self.Bconst])
        S.dma(self.ld, self.cmat, self.cmat_d, writes=[self.Bconst])
        S.op("pool", lambda e: e.memset(self.ones_b, 1.0), writes=[self.Bconst])
        S.op("dve", lambda e: e.tensor_copy(out=self.ident_b, in_=self.cmat[:, C_ID:C_ID + P]), reads=[self.Bconst], writes=[self.Bconst])
        S.op("dve", lambda e: e.tensor_copy(out=self.pi_b, in_=self.cmat[:, C_PI:C_PI + P]), reads=[self.Bconst], writes=[self.Bconst])
        S.op("dve", lambda e: e.tensor_copy(out=self.mask_b, in_=self.cmat[:, C_MPREV:C_MPREV + 2 * P].rearrange("p (a b) -> p a b", a=2)),
             reads=[self.Bconst], writes=[self.Bconst])
        S.barrier()

    def prep(self, names=None):
        S, nc = self.S, self.nc
        A = self.A0.fork()
        NB = 3
        CH = 4096
        fin = [A([P, CH], F32, "pin") for _ in range(NB)]
        fout = [A([P, CH], BF16, "pout") for _ in range(NB)]
        Bin = [Buf() for _ in range(NB)]
        Bout = [Buf() for _ in range(NB)]
        k = 0
        engs = ["dve", "act"]
        for name in (names or list(self.wshapes)):
            shp = self.wshapes[name]
            nl = shp[0] if shp[0] != P else 1
            for l in range(nl):
                src = self.wf[name][l] if shp[0] != P else self.wf[name]
                dst = self.wb[name][l] if shp[0] != P else self.wb[name]
                nd = len(src.shape)
                letters = "abcd"[: nd - 1]
                pat = "p " + " ".join(letters) + " -> p (" + " ".join(letters) + ")"
                src2 = src.rearrange(pat)
                dst2 = dst.rearrange(pat)
                n = src2.shape[1]
                for c0 in range(0, n, CH):
                    w = min(CH, n - c0)
                    i = k % NB
                    S.dma(self.ld, fin[i][:, :w], src2[:, c0:c0 + w], writes=[Bin[i]])
                    if name == "w_ret_out":
                        per = 16 * P
                        for q0 in range(0, w, P):
                            ec = ((c0 + q0) % per) // P
                            col = V_GN + l * 16 + ec
                            S.op("dve", lambda e, i=i, q0=q0, col=col: e.tensor_scalar(
                                out=fout[i][:, q0:q0 + P], in0=fin[i][:, q0:q0 + P], scalar1=self.vecs[:, col:col + 1],
                                scalar2=None, op0=ALU.mult), reads=[Bin[i], self.Bconst], writes=[Bout[i]])
                    else:
                        en = engs[k % 2]
                        if en == "act":
                            S.op("act", lambda e, i=i, w=w: e.copy(out=fout[i][:, :w], in_=fin[i][:, :w]), reads=[Bin[i]], writes=[Bout[i]])
                        else:
                            S.op(en, lambda e, i=i, w=w: e.tensor_copy(out=fout[i][:, :w], in_=fin[i][:, :w]), reads=[Bin[i]], writes=[Bout[i]])
                    S.dma(self.st, dst2[:, c0:c0 + w], fout[i][:, :w], reads=[Bout[i]], writes=[self.Bwb[name]])
                    k += 1
        S.barrier()

    def x_tile(self, ap_seq, tt):
        return ap_seq[:, tt * 512:(tt + 1) * 512].rearrange("(kc p) t -> p kc t", p=P)

    def rstd_from(self, src, sq, psb, rstd, Bsrc, Bsq, Brstd):
        S = self.S
        lvl = int(getattr(self, "dbglvl", 9))
        if lvl < 2:
            return
        S.op("act", lambda e: e.activation(out=sq, in_=src, func=AF.Square), reads=[Bsrc], writes=[Bsq])
        if lvl < 3:
            return
        for kc in range(KC):
            S.op("pe", lambda e, kc=kc: e.matmul(self.ps[psb], lhsT=self.ones_b, rhs=sq[:, kc, :], start=(kc == 0), stop=(kc == KC - 1)),
                 reads=[Bsq, self.Bconst], writes=[self.Bps[psb]], inc=(kc == KC - 1))
        if lvl < 4:
            return
        S.op("act", lambda e: e.activation(out=rstd, in_=self.ps[psb], func=AF.Sqrt, bias=RMS_EPS, scale=1.0 / DM),
             reads=[self.Bps[psb]], writes=[Brstd])
        if lvl < 5:
            return
        S.op("dve", lambda e: e.reciprocal(out=rstd, in_=rstd), reads=[Brstd], writes=[Brstd])

    def phase_hT(self, s, A, hT, BhT, gcol):
        S = self.S
        xt = [A([P, KC, 512], F32, "xt") for _ in range(2)]
        sq = A([P, KC, 512], BF16, "sq")
        rs = [A([P, 512], F32, "rs") for _ in range(2)]
        Bxt = [Buf() for _ in range(2)]
        Bsq = Buf()
        Brs = [Buf() for _ in range(2)]
        for tt in range(4):
            i = tt % 2
            S.dma(self.ld, xt[i], self.x_tile(self.xsrc[s], tt), reads=[self.Bx[s][tt]], writes=[Bxt[i]])
            self.rstd_from(xt[i], sq, tt % 2, rs[i], Bxt[i], Bsq, Brs[i])
            if int(getattr(self, "dbglvl", 9)) < 6:
                continue
            for kc in range(KC):
                S.op("dve", lambda e, kc=kc, i=i, tt=tt: e.scalar_tensor_tensor(
                    out=hT[:, kc, tt * 512:(tt + 1) * 512], in0=xt[i][:, kc, :], scalar=self.vecs[:, gcol + kc:gcol + kc + 1],
                    in1=rs[i], op0=ALU.mult, op1=ALU.mult), reads=[Bxt[i], Brs[i], self.Bconst], writes=[BhT[tt]])

    def post_norm_residual(self, s, tt, fT, BfT, xt, Bxt, sq, Bsq, rs, Brs, gcol, psb):
        S = self.S
        self.rstd_from(fT, sq, psb, rs, BfT, Bsq, Brs)
        for kc in range(KC):
            S.op("pool", lambda e, kc=kc: e.tensor_tensor(out=fT[:, kc, :], in0=fT[:, kc, :], in1=rs, op=ALU.mult),
                 reads=[BfT, Brs], writes=[BfT])
        for kc in range(KC):
            S.op("dve", lambda e, kc=kc: e.scalar_tensor_tensor(
                out=xt[:, kc, :], in0=fT[:, kc, :], scalar=self.vecs[:, gcol + kc:gcol + kc + 1], in1=xt[:, kc, :],
                op0=ALU.mult, op1=ALU.add), reads=[BfT, Bxt, self.Bconst], writes=[Bxt])
        S.dma(self.st, self.x_tile(self.xdst[s], tt), xt, reads=[Bxt], writes=[self.Bx[s][tt]])

    def out_proj_phase(self, s, A, wname, l, nk, actT, BactT, gcol):
        S = self.S
        xt = [A([P, KC, 512], F32, "xt") for _ in range(2)]
        fT = A([P, KC, 512], F32, "fT")
        sq = A([P, KC, 512], BF16, "sq")
        rs = A([P, 512], F32, "rs")
        NW = 3
        wd = [A([P, nk, P], BF16, "wd") for _ in range(NW)]
        Bxt = [Buf() for _ in range(2)]
        BfT, Bsq, Brs = Buf(), Buf(), Buf()
        Bwd = [Buf() for _ in range(NW)]
        wsrc = self.wb[wname][l]
        seq = [(tt, dc) for tt in range(4) for dc in range(KC)]

        def loadw(n):
            tt, dc = seq[n]
            S.dma(self.ldw[n % NW], wd[n % NW], wsrc[:, dc], reads=[self.Bwb[wname]], writes=[Bwd[n % NW]])

        loadw(0)
        loadw(1)
        S.dma(self.ld, xt[0], self.x_tile(self.xsrc[s], 0), reads=[self.Bx[s][0]], writes=[Bxt[0]])
        for n, (tt, dc) in enumerate(seq):
            if n + 2 < len(seq):
                loadw(n + 2)
            if dc == 0 and tt + 1 < 4:
                S.dma(self.ld, xt[(tt + 1) % 2], self.x_tile(self.xsrc[s], tt + 1), reads=[self.Bx[s][tt + 1]], writes=[Bxt[(tt + 1) % 2]])
            pb = 2 + (n % 2)
            w = wd[n % NW]
            for j in range(nk):
                S.op("pe", lambda e, j=j, w=w, pb=pb, tt=tt: e.matmul(self.ps[pb], lhsT=w[:, j, :], rhs=actT[:, j, tt * 512:(tt + 1) * 512],
                                                               start=(j == 0), stop=(j == nk - 1)),
                     reads=[Bwd[n % NW], BactT], writes=[self.Bps[pb]], inc=(j == nk - 1))
            S.op("act", lambda e, dc=dc, pb=pb: e.copy(out=fT[:, dc, :], in_=self.ps[pb]), reads=[self.Bps[pb]], writes=[BfT])
            if dc == KC - 1:
                self.post_norm_residual(s, tt, fT, BfT, xt[tt % 2], Bxt[tt % 2], sq, Bsq, rs, Brs, gcol, tt % 2)

    def ffn(self, l, s):
        S = self.S
        A = self.A0.fork()
        aT = A([P, NJ, SEQ], BF16, "aT")
        BaT = Buf()
        R = A.fork()
        hT = R([P, KC, SEQ], BF16, "hT")
        BhT = [Buf() for _ in range(4)]
        self.phase_hT(s, self.A0.fork(), hT, BhT, V_FFNPRE + 8 * l)
        S.barrier()
        if getattr(self, "dbg", "") == "A":
            return
        NW = 3
        wu = [R([P, KC, 256], BF16, "wu") for _ in range(NW)]
        Bwu = [Buf() for _ in range(NW)]
        gb = R([P, 2 + SEQ], F32, "gb")
        Bgb = [Buf() for _ in range(4)]
        ct = [R([P, 512], F32, "ct") for _ in range(2)]
        cg = [R([P, 512], F32, "cg") for _ in range(2)]
        Bct = [Buf() for _ in range(2)]
        Bcg = [Buf() for _ in range(2)]
        S.op("pool", lambda e: e.memset(gb[:, 0:2], 0.0), writes=[Bgb[0]])
        wsrc = self.wb["w_up"][l]

        def loadw(j):
            S.dma(self.ldw[j % NW], wu[j % NW], wsrc[:, j], reads=[self.Bwb["w_up"]], writes=[Bwu[j % NW]])

        loadw(0)
        loadw(1)
        cwb = V_CW + l * 3 * NJ
        cbb = V_CB + l * NJ
        n = 0
        for j in range(NJ):
            if j + 2 < NJ:
                loadw(j + 2)
            w = wu[j % NW]
            for tt in range(4):
                pg, pv = 4 + 2 * (n % 2), 5 + 2 * (n % 2)
                i = n % 2
                cols = slice(tt * 512, (tt + 1) * 512)
                for half, pb in ((0, pg), (1, pv)):
                    for kc in range(KC):
                        S.op("pe", lambda e, kc=kc, pb=pb, half=half, w=w, cols=cols: e.matmul(
                            self.ps[pb], lhsT=w[:, kc, half * P:(half + 1) * P], rhs=hT[:, kc, cols], start=(kc == 0), stop=(kc == KC - 1)),
                            reads=[Bwu[j % NW], BhT[tt]], writes=[self.Bps[pb]], inc=(kc == KC - 1))
                S.op("act", lambda e, pg=pg, tt=tt: e.copy(out=gb[:, 2 + tt * 512:2 + (tt + 1) * 512], in_=self.ps[pg]),
                     reads=[self.Bps[pg]], writes=[Bgb[tt]])
                rb = [Bgb[tt]] + ([Bgb[tt - 1]] if tt > 0 else [])
                S.op("dve", lambda e, tt=tt, i=i, j=j: e.tensor_scalar(
                    out=ct[i], in0=gb[:, 2 + tt * 512:2 + (tt + 1) * 512], scalar1=self.vecs[:, cwb + 2 * NJ + j:cwb + 2 * NJ + j + 1],
                    scalar2=self.vecs[:, cbb + j:cbb + j + 1], op0=ALU.mult, op1=ALU.add), reads=rb + [self.Bconst], writes=[Bct[i]])
                for tap in (1, 0):
                    sh = 2 - tap
                    S.op("dve", lambda e, tt=tt, i=i, j=j, tap=tap, sh=sh: e.scalar_tensor_tensor(
                        out=ct[i], in0=gb[:, 2 - sh + tt * 512:2 - sh + (tt + 1) * 512],
                        scalar=self.vecs[:, cwb + tap * NJ + j:cwb + tap * NJ + j + 1], in1=ct[i], op0=ALU.mult, op1=ALU.add),
                        reads=rb + [Bct[i], self.Bconst], writes=[Bct[i]])
                S.op("act", lambda e, i=i: e.activation(out=cg[i], in_=ct[i], func=AF.Gelu_apprx_tanh), reads=[Bct[i]], writes=[Bcg[i]])
                S.op("dve", lambda e, i=i, j=j, cols=cols, pv=pv: e.tensor_tensor(out=aT[:, j, cols], in0=cg[i], in1=self.ps[pv], op=ALU.mult),
                     reads=[Bcg[i], self.Bps[pv]], writes=[BaT])
                n += 1
        S.barrier()
        if getattr(self, "dbg", "") == "B":
            return
        self.out_proj_phase(s, A.fork(), "w_down", l, NJ, aT, BaT, V_FFNPOST + 8 * l)
        S.barrier()


    def ret(self, l, s):
        S = self.S
        A = self.A0.fork()
        yT = A([P, 16, SEQ], BF16, "yT")
        ByT = Buf()
        R = A.fork()
        hT = R([P, KC, SEQ], BF16, "hT")
        BhT = [Buf() for _ in range(4)]
        self.phase_hT(s, self.A0.fork(), hT, BhT, V_MIXPRE + 8 * l)
        S.barrier()
        R2 = R.fork()
        cs = R([P, 2, SEQ], F32, "cs")
        Bcs = Buf()
        S.dma(self.ld, cs, self.rcs_d, writes=[Bcs])
        wsl = R([P, KC, 1536], BF16, "wsl")
        Bw = Buf()
        qk = [R([P, 4, 512], BF16, "qk") for _ in range(2)]
        vv = [R([P, 4, 512], BF16, "vv") for _ in range(2)]
        sg = [R([P, 4, 512], BF16, "sg") for _ in range(2)]
        Bqk = [[Buf(), Buf()] for _ in range(2)]
        Bvv = [[Buf() for _ in range(4)] for _ in range(2)]
        Bsg = [[Buf() for _ in range(4)] for _ in range(2)]
        tm = [R([P, 512], F32, "tm") for _ in range(4)]
        Btm = [Buf() for _ in range(4)]
        stf = R([P, 2, 512], F32, "stf")
        stb = R([P, 2, 512], BF16, "stb")
        Bstf, Bstb = Buf(), Buf()
        sT = [R([P, P], BF16, "sT") for _ in range(2)]
        kz = [R([P, 256], BF16, "kz") for _ in range(2)]
        yn = [R([P, 512], F32, "yn") for _ in range(2)]
        gt = [R([P, 512], BF16, "gt") for _ in range(2)]
        bst = [R([P, 6], F32, "bst") for _ in range(2)]
        mv = [R([P, 2], F32, "mv") for _ in range(2)]
        rg = [R([P, 2], F32, "rg") for _ in range(2)]
        BsT = [Buf() for _ in range(2)]
        Bkz = [Buf() for _ in range(2)]
        Byn = [Buf() for _ in range(2)]
        Bgt = [Buf() for _ in range(2)]
        Bsm = [Buf() for _ in range(2)]
        ps, Bps = self.ps, self.Bps
        psK = ps[3].bitcast(BF16)[:, 0:256]
        psT = ps[3].bitcast(BF16)[:, 512:1024]
        BpsK, BpsT = Buf(), Buf()
        proj_banks = [0, 1, 7]
        pbn = [0]
        wsrc = self.wb["w_ret_in"][l]
        units = [(h, st) for h in range(RH) for st in range(4)]

        def nextbank():
            b = proj_banks[pbn[0] % 3]
            pbn[0] += 1
            return b

        def load_w(h):
            S.dma(self.ldw[0], wsl, wsrc[:, h], reads=[self.Bwb["w_ret_in"]], writes=[Bw])

        def qk_pair(u, which):
            h, st = units[u]
            i = u % 2
            cols = slice(st * 512, (st + 1) * 512)
            banks = []
            for half in range(2):
                fi = which * 2 + half
                b = nextbank()
                banks.append(b)
                for kc in range(KC):
                    S.op("pe", lambda e, kc=kc, b=b, fi=fi: e.matmul(ps[b], lhsT=wsl[:, kc, fi * P:(fi + 1) * P], rhs=hT[:, kc, cols],
                                                                 start=(kc == 0), stop=(kc == KC - 1)),
                         reads=[Bw, BhT[st]], writes=[Bps[b]], inc=(kc == KC - 1))
            b0, b1 = banks
            cosv, sinv = cs[:, 0, cols], cs[:, 1, cols]
            o = which * 2
            S.op("dve", lambda e: e.tensor_tensor(out=tm[0], in0=ps[b0], in1=cosv, op=ALU.mult), reads=[Bps[b0], Bcs], writes=[Btm[0]])
            S.op("dve", lambda e: e.tensor_tensor(out=tm[1], in0=ps[b1], in1=sinv, op=ALU.mult), reads=[Bps[b1], Bcs], writes=[Btm[1]])
            S.op("dve", lambda e: e.tensor_tensor(out=tm[2], in0=ps[b1], in1=cosv, op=ALU.mult), reads=[Bps[b1], Bcs], writes=[Btm[2]])
            S.op("dve", lambda e: e.tensor_tensor(out=tm[3], in0=ps[b0], in1=sinv, op=ALU.mult), reads=[Bps[b0], Bcs], writes=[Btm[3]])
            S.op("dve", lambda e: e.tensor_tensor(out=qk[i][:, o, :], in0=tm[0], in1=tm[1], op=ALU.subtract),
                 reads=[Btm[0], Btm[1]], writes=[Bqk[i][which]])
            S.op("dve", lambda e: e.tensor_tensor(out=qk[i][:, o + 1, :], in0=tm[2], in1=tm[3], op=ALU.add),
                 reads=[Btm[2], Btm[3]], writes=[Bqk[i][which]])

        def vg_part(u, c):
            h, st = units[u]
            i = u % 2
            t0 = st * 512 + c * P
            for which in range(2):
                b = nextbank()
                for kc in range(KC):
                    S.op("pe", lambda e, kc=kc, b=b, which=which: e.matmul(
                        ps[b], lhsT=hT[:, kc, t0:t0 + P], rhs=wsl[:, kc, 512 + which * 512:1024 + which * 512],
                        start=(kc == 0), stop=(kc == KC - 1)), reads=[Bw, BhT[st]], writes=[Bps[b]], inc=(kc == KC - 1))
                if which == 0:
                    S.op("act", lambda e, b=b: e.copy(out=vv[i][:, c, :], in_=ps[b]), reads=[Bps[b]], writes=[Bvv[i][c]])
                else:
                    S.op("act", lambda e, b=b: e.activation(out=sg[i][:, c, :], in_=ps[b], func=AF.Silu), reads=[Bps[b]], writes=[Bsg[i][c]])

        def proj_parts(u):
            return [lambda: (qk_pair(u, 0), vg_part(u, 0)), lambda: (qk_pair(u, 1), vg_part(u, 1)),
                    lambda: vg_part(u, 2), lambda: vg_part(u, 3)]

        def front(u, c):
            h, st = units[u]
            i = u % 2
            k = c % 2
            cc = slice(c * P, (c + 1) * P)
            for j in range(2):
                S.op("pe", lambda e, j=j: e.matmul(ps[2][:, 0:P], lhsT=qk[i][:, 2 + j, cc], rhs=qk[i][:, j, cc], start=(j == 0), stop=(j == 1)),
                     reads=[Bqk[i][0], Bqk[i][1]], writes=[Bps[2]])
            for j in range(2):
                S.op("pe", lambda e, j=j: e.transpose(psK[:, j * P:(j + 1) * P], qk[i][:, 2 + j, cc], self.ident_b),
                     reads=[Bqk[i][1], self.Bconst], writes=[BpsK])
            S.op("dve", lambda e: e.tensor_tensor(out=sT[k], in0=ps[2][:, 0:P], in1=self.cmat[:, C_DT + h * P:C_DT + (h + 1) * P], op=ALU.mult),
                 reads=[Bps[2], self.Bconst], writes=[BsT[k]])
            S.op("act", lambda e: e.activation(out=kz[k], in_=psK, func=AF.Identity, scale=self.cmat[:, C_ZETA + h:C_ZETA + h + 1]),
                 reads=[BpsK, self.Bconst], writes=[Bkz[k]])

        def back(u, c):
            h, st = units[u]
            i = u % 2
            k = c % 2
            cg = st * 4 + c
            cc = slice(c * P, (c + 1) * P)
            lg = math.log(1.0 - 2.0 ** (-5.0 - h))
            gC = math.exp(128.0 * lg)
            S.op("pe", lambda e: e.matmul(ps[4], lhsT=sT[k], rhs=vv[i][:, c, :], start=True, stop=(cg == 0)),
                 reads=[BsT[k], Bvv[i][c]], writes=[Bps[4]])
            if cg > 0:
                for j in range(2):
                    S.op("pe", lambda e, j=j: e.matmul(ps[4], lhsT=qk[i][:, j, cc], rhs=stb[:, j, :], start=False, stop=(j == 1)),
                         reads=[Bqk[i][0], Bstb], writes=[Bps[4]])
            if cg < 15:
                for j in range(2):
                    S.op("pe", lambda e, j=j: e.matmul(ps[5 + j], lhsT=kz[k][:, j * P:(j + 1) * P], rhs=vv[i][:, c, :], start=True, stop=True),
                         reads=[Bkz[k], Bvv[i][c]], writes=[Bps[5 + j]])
                for j in range(2):
                    if cg == 0:
                        S.op("dve", lambda e, j=j: e.tensor_copy(out=stf[:, j, :], in_=ps[5 + j]), reads=[Bps[5 + j]], writes=[Bstf])
                    else:
                        S.op("dve", lambda e, j=j: e.scalar_tensor_tensor(out=stf[:, j, :], in0=stf[:, j, :], scalar=gC, in1=ps[5 + j],
                                                                         op0=ALU.mult, op1=ALU.add), reads=[Bps[5 + j], Bstf], writes=[Bstf])
                S.op("act", lambda e: e.copy(out=stb, in_=stf), reads=[Bstf], writes=[Bstb])
            S.op("dve", lambda e: e.bn_stats(out=bst[k], in_=ps[4]), reads=[Bps[4]], writes=[Bsm[k]])
            S.op("dve", lambda e: e.bn_aggr(out=mv[k], in_=bst[k]), reads=[Bsm[k]], writes=[Bsm[k]])
            S.op("act", lambda e: e.activation(out=rg[k][:, 0:1], in_=mv[k][:, 1:2], func=AF.Sqrt, bias=self.cmat[:, C_EPS + h:C_EPS + h + 1], scale=1.0),
                 reads=[Bsm[k], self.Bconst], writes=[Bsm[k]])
            S.op("dve", lambda e: e.reciprocal(out=rg[k][:, 0:1], in_=rg[k][:, 0:1]), reads=[Bsm[k]], writes=[Bsm[k]])
            S.op("dve", lambda e: e.scalar_tensor_tensor(out=rg[k][:, 1:2], in0=mv[k][:, 0:1], scalar=-1.0, in1=rg[k][:, 0:1], op0=ALU.mult, op1=ALU.mult),
                 reads=[Bsm[k]], writes=[Bsm[k]])
            S.op("act", lambda e: e.activation(out=yn[k], in_=ps[4], func=AF.Identity, bias=rg[k][:, 1:2], scale=rg[k][:, 0:1]),
                 reads=[Bps[4], Bsm[k]], writes=[Byn[k]])
            S.op("dve", lambda e: e.tensor_tensor(out=gt[k], in0=yn[k], in1=sg[i][:, c, :], op=ALU.mult),
                 reads=[Byn[k], Bsg[i][c]], writes=[Bgt[k]])

        def ytr(u, c):
            h, st = units[u]
            k = c % 2
            cg = st * 4 + c
            for jj in range(4):
                S.op("pe", lambda e, jj=jj: e.transpose(psT[:, jj * P:(jj + 1) * P], gt[k][:, jj * P:(jj + 1) * P], self.ident_b),
                     reads=[Bgt[k], self.Bconst], writes=[BpsT])
            S.op("act", lambda e: e.copy(out=yT[:, 4 * h:4 * h + 4, cg * P:(cg + 1) * P], in_=psT.rearrange("p (a b) -> p a b", a=4)),
                 reads=[BpsT], writes=[ByT])

        load_w(0)
        for f in proj_parts(0):
            f()
        pending = None
        for u in range(len(units)):
            nxt = [None] * 4
            if u + 1 < len(units):
                if units[u + 1][0] != units[u][0]:
                    load_w(units[u + 1][0])
                nxt = proj_parts(u + 1)
            for c in range(4):
                front(u, c)
                if nxt[c] is not None:
                    nxt[c]()
                back(u, c)
                if pending is not None:
                    ytr(*pending)
                pending = (u, c)
        ytr(*pending)
        S.barrier()
        self.out_proj_phase(s, R2, "w_ret_out", l, 16, yT, ByT, V_MIXPOST + 8 * l)
        S.barrier()


    @staticmethod
    def gcols(ap2d, g, u):
        if g == 0:
            return ap2d[:, u * 512:(u + 1) * 512]
        if g == 1:
            return ap2d[:, u::4]
        return ap2d.rearrange("p (i r) -> p r i", r=16)[:, 4 * u:4 * u + 4, :]

    @staticmethod
    def gview(ap512, g):
        return ap512 if g < 2 else ap512.rearrange("p (r i) -> p r i", r=4)

    @staticmethod
    def chunk_tokens(ap2d, g, c):
        if g == 0:
            return ap2d[..., c * P:(c + 1) * P] if False else ap2d[:, c * P:(c + 1) * P]
        if g == 1:
            st = 512 * (c % 4) + c // 4
            return ap2d[:, st:st + 509:4]
        return ap2d[:, c::16]

    def rot_proj(self, g, u, w_lhsT, hT, BhT, Bw, cs, Bcs, qraw, Bqraw, t1, t2, Bt, out512, Bout, pb, pb2):
        S, ps, Bps = self.S, self.ps, self.Bps
        for kc in range(KC):
            S.op("pe", lambda e, kc=kc: e.matmul(self.gview(ps[pb], g), lhsT=w_lhsT[:, kc, :], rhs=self.gcols(hT[:, kc, :], g, u),
                                               start=(kc == 0), stop=(kc == KC - 1)), reads=[Bw] + BhT, writes=[Bps[pb]], inc=(kc == KC - 1))
        S.op("act", lambda e: e.copy(out=qraw, in_=ps[pb]), reads=[Bps[pb]], writes=[Bqraw])
        S.op("pe", lambda e: e.matmul(ps[pb2], lhsT=self.pi_b, rhs=qraw, start=True, stop=True), reads=[Bqraw, self.Bconst], writes=[Bps[pb2]])
        S.op("dve", lambda e: e.tensor_tensor(out=self.gview(t1, g), in0=self.gview(ps[pb2], g), in1=self.gcols(cs[:, 1, :], g, u), op=ALU.mult),
             reads=[Bps[pb2], Bcs], writes=[Bt[0]])
        S.op("dve", lambda e: e.tensor_tensor(out=self.gview(t2, g), in0=self.gview(ps[pb], g), in1=self.gcols(cs[:, 0, :], g, u), op=ALU.mult),
             reads=[Bps[pb], Bcs], writes=[Bt[1]])
        S.op("dve", lambda e: e.tensor_tensor(out=out512, in0=t1, in1=t2, op=ALU.add), reads=[Bt[0], Bt[1]], writes=[Bout])

    def kv(self, s):
        S = self.S
        A = self.A0.fork()
        hT = A([P, KC, SEQ], BF16, "hT")
        BhT = [Buf() for _ in range(4)]
        self.phase_hT(s, A.fork(), hT, BhT, V_KVN)
        S.barrier()
        R = A.fork()
        cs = R([P, 2, SEQ], F32, "cs")
        Bcs = Buf()
        S.dma(self.ld, cs, self.acs_d, writes=[Bcs])
        wk = [R([P, KC, P], BF16, "wk") for _ in range(3)]
        Bwk = [Buf() for _ in range(3)]
        qraw = [R([P, 512], BF16, "qraw") for _ in range(2)]
        t1 = [R([P, 512], F32, "t1") for _ in range(2)]
        t2 = [R([P, 512], F32, "t2") for _ in range(2)]
        Bqraw = [Buf() for _ in range(2)]
        Bt = [[Buf(), Buf()] for _ in range(2)]
        kst = [R([P, SEQ], BF16, "kst") for _ in range(2)]
        Bkst = [Buf() for _ in range(2)]
        wv = R([P, 2, KC, 512], BF16, "wv")
        Bwv = Buf()
        stg = R([P, 8, 16, 192], BF16, "stg")
        Bstg = Buf()
        S.op("dve", lambda e: e.memset(stg.rearrange("p a c x -> p (a c x)"), 1.0), writes=[Bstg])
        wsrc = self.wb["w_k"]

        def loadk(gp):
            S.dma(self.ldw[gp % 3], wk[gp % 3], wsrc[:, gp], reads=[self.Bwb["w_k"]], writes=[Bwk[gp % 3]])

        loadk(0)
        loadk(1)
        n = 0
        for gp in range(24):
            g, hp = gp // 8, gp % 8
            if gp + 2 < 24:
                loadk(gp + 2)
            ks = kst[gp % 2]
            for u in range(4):
                i = n % 2
                self.rot_proj(g, u, wk[gp % 3], hT, BhT, Bwk[gp % 3], cs, Bcs, qraw[i], Bqraw[i], t1[i], t2[i], Bt[i],
                              ks[:, u * 512:(u + 1) * 512], Bkst[gp % 2], 0 + i, 2 + i)
                n += 1
            S.dma(self.st, self.kt_d[g, hp], ks, reads=[Bkst[gp % 2]], writes=[self.Bkt])
        ps, Bps = self.ps, self.Bps
        n = 0
        for g in range(3):
            S.dma(self.ldw[3], wv, self.wb["w_v"][:, 2 * g:2 * g + 2], reads=[self.Bwb["w_v"]], writes=[Bwv])
            for c in range(16):
                for half in range(2):
                    pb = 4 + (n % 4)
                    n += 1
                    for kc in range(KC):
                        S.op("pe", lambda e, kc=kc, pb=pb, half=half: e.matmul(ps[pb], lhsT=self.chunk_tokens(hT[:, kc, :], g, c), rhs=wv[:, half, kc, :],
                                                                         start=(kc == 0), stop=(kc == KC - 1)), reads=[Bwv] + BhT, writes=[Bps[pb]], inc=(kc == KC - 1))
                    pv = ps[pb].rearrange("p (a h d) -> p a h d", a=4, h=2)
                    for hh in range(2):
                        o = stg[:, half * 4:half * 4 + 4, c, hh * 128:hh * 128 + 64]
                        if hh == 0:
                            S.op("act", lambda e, o=o, pv=pv: e.copy(out=o, in_=pv[:, :, 0, :]), reads=[Bps[pb]], writes=[Bstg])
                        else:
                            S.op("dve", lambda e, o=o, pv=pv: e.tensor_copy(out=o, in_=pv[:, :, 1, :]), reads=[Bps[pb]], writes=[Bstg])
            S.dma(self.st, self.va_d[g], stg.rearrange("p a c x -> p a (c x)"), reads=[Bstg], writes=[self.Bva])
        S.barrier()

    def att(self, l, s):
        S = self.S
        li = l - 2
        A = self.A0.fork()
        oT = A([P, 8, SEQ], BF16, "oT")
        BoT = Buf()
        R = A.fork()
        R0 = R.fork()
        hT = R([P, KC, SEQ], BF16, "hT")
        BhT = [Buf() for _ in range(4)]
        self.phase_hT(s, R.fork(), hT, BhT, V_MIXPRE + 8 * l)
        S.barrier()
        cs = R([P, 2, SEQ], F32, "cs")
        Bcs = Buf()
        S.dma(self.ld, cs, self.acs_d, writes=[Bcs])
        wq = [R([P, KC, 384], BF16, "wq") for _ in range(2)]
        Bwq = [Buf() for _ in range(2)]
        kt = R([P, 3, SEQ], BF16, "kt")
        vt = R([P, 3, 16 * 192], BF16, "vt")
        Bkt, Bvt = Buf(), Buf()
        qT = R([P, 3, SEQ], BF16, "qT")
        BqT = [Buf() for _ in range(3)]
        acc = R([P, 2, SEQ], F32, "acc")
        Bacc = Buf()
        qraw = [R([P, 512], BF16, "qraw") for _ in range(2)]
        t1 = [R([P, 512], F32, "t1") for _ in range(2)]
        t2 = [R([P, 512], F32, "t2") for _ in range(2)]
        Bqraw = [Buf() for _ in range(2)]
        Bt = [[Buf(), Buf()] for _ in range(2)]
        pP = [R([P, 2, 256], BF16, "pP") for _ in range(2)]
        PT = [R([P, 2, 2, P], BF16, "PT") for _ in range(2)]
        BpP = [Buf() for _ in range(2)]
        BPT = [Buf() for _ in range(2)]
        tden = R([P, SEQ], F32, "tden")
        Btden = Buf()
        ps, Bps = self.ps, self.Bps
        psTb = ps[6].bitcast(BF16)
        psT = [psTb[:, k * 512:(k + 1) * 512].rearrange("p (h c q) -> p h c q", h=2, c=2) for k in range(2)]
        psU = [ps[7][:, k * 256:(k + 1) * 256].rearrange("p (h q) -> p h q", h=2) for k in range(2)]
        BpsT = [Buf() for _ in range(2)]
        BpsU = [Buf() for _ in range(2)]
        wsrc = self.wb["w_q"][li]

        def loadq(hp):
            S.dma(self.ldw[hp % 2], wq[hp % 2], wsrc[:, hp], reads=[self.Bwb["w_q"]], writes=[Bwq[hp % 2]])

        loadq(0)
        n = 0
        m = 0
        for hp in range(8):
            if hp + 1 < 8:
                loadq(hp + 1)
            S.dma(self.ldw[2], kt, self.kt_d[:, hp].rearrange("g p t -> p g t"), reads=[self.Bkt], writes=[Bkt])
            S.dma(self.ldw[3], vt, self.va_d[:, :, hp].rearrange("g p x -> p g x"), reads=[self.Bva], writes=[Bvt])
            w = wq[hp % 2]
            for g in range(3):
                for u in range(4):
                    i = n % 2
                    self.rot_proj(g, u, w[:, :, g * P:(g + 1) * P], hT, BhT, Bwq[hp % 2], cs, Bcs, qraw[i], Bqraw[i], t1[i], t2[i], Bt[i],
                                  qT[:, g, u * 512:(u + 1) * 512], BqT[g], 0 + i, 2 + i)
                    n += 1
            for g in range(3):
                for c in range(16):
                    has_prev = (c > 0) if g == 0 else ((c % 4) > 0 if g == 1 else False)
                    nkc = 2 if has_prev else 1
                    nk = nkc * P
                    k0 = (c - 1) * P if has_prev else c * P
                    k = m % 2
                    m += 1
                    pS = 4 + k
                    pSv = ps[pS].rearrange("p (h x) -> p h x", h=2)
                    for hh in range(2):
                        rows = slice(hh * 64, (hh + 1) * 64)
                        S.op("pe", lambda e, hh=hh, rows=rows: e.matmul(pSv[:, hh, 0:nk], lhsT=qT[rows, g, c * P:(c + 1) * P], rhs=kt[rows, g, k0:k0 + nk],
                                                                     start=True, stop=True), reads=[BqT[g], Bkt], writes=[Bps[pS]])
                    S.op("act", lambda e: e.activation(out=pP[k][:, :, 0:nk], in_=pSv[:, :, 0:nk], func=AF.Exp, scale=0.125),
                         reads=[Bps[pS]], writes=[BpP[k]])
                    for hh in range(2):
                        for kc in range(nkc):
                            S.op("pe", lambda e, hh=hh, kc=kc: e.transpose(psT[k][:, hh, kc, :], pP[k][:, hh, kc * P:(kc + 1) * P], self.ident_b),
                                 reads=[BpP[k], self.Bconst], writes=[BpsT[k]])
                    msk = self.mask_b[:, 2 - nkc:2, :].unsqueeze(1).broadcast_to([P, 2, nkc, P])
                    S.op("dve", lambda e, msk=msk: e.tensor_tensor(out=PT[k][:, :, 0:nkc, :], in0=psT[k][:, :, 0:nkc, :], in1=msk, op=ALU.mult),
                         reads=[BpsT[k], self.Bconst], writes=[BPT[k]])
                    for hh in range(2):
                        for kc in range(nkc):
                            ch = (c - 1 + kc) if has_prev else c
                            S.op("pe", lambda e, hh=hh, kc=kc, ch=ch: e.matmul(psU[k][:, hh, :], lhsT=vt[:, g, ch * 192 + hh * 64:ch * 192 + hh * 64 + P],
                                                                            rhs=PT[k][:, hh, kc, :], start=(kc == 0), stop=(kc == nkc - 1)),
                                 reads=[Bvt, BPT[k]], writes=[BpsU[k]])
                    if g == 0:
                        S.op("act", lambda e: e.copy(out=acc[:, :, c * P:(c + 1) * P], in_=psU[k]), reads=[BpsU[k]], writes=[Bacc])
                    else:
                        if g == 1:
                            st = 512 * (c % 4) + c // 4
                            dst = acc[:, :, st:st + 509:4]
                        else:
                            dst = acc[:, :, c::16]
                        S.op("dve", lambda e, dst=dst: e.tensor_tensor(out=dst, in0=dst, in1=psU[k], op=ALU.add), reads=[BpsU[k], Bacc], writes=[Bacc])
            S.op("act", lambda e: e.copy(out=tden[0:64, :], in_=acc[64:128, 0, :]), reads=[Bacc], writes=[Btden])
            S.op("act", lambda e: e.copy(out=tden[64:128, :], in_=acc[0:64, 1, :]), reads=[Bacc], writes=[Btden])
            S.op("dve", lambda e: e.reciprocal(out=tden, in_=tden), reads=[Btden], writes=[Btden])
            S.op("dve", lambda e, hp=hp: e.tensor_tensor(out=oT[0:64, hp, :], in0=acc[0:64, 0, :], in1=tden[0:64, :], op=ALU.mult),
                 reads=[Bacc, Btden], writes=[BoT])
            S.op("dve", lambda e, hp=hp: e.tensor_tensor(out=oT[64:128, hp, :], in0=acc[64:128, 1, :], in1=tden[64:128, :], op=ALU.mult),
                 reads=[Bacc, Btden], writes=[BoT])
        S.barrier()
        self.out_proj_phase(s, R0, "w_o", li, 8, oT, BoT, V_MIXPOST + 8 * l)
        S.barrier()

    def run_stages(self, stages=None):
        if stages is None:
            stages = [("prep", None)]
            for s in range(self.nseq):
                for l in range(4):
                    stages.append(("ret" if l < 2 else "att", l, s))
                    stages.append(("ffn", l, s))
                    if l == 1:
                        stages.append(("kv", s))
        for st in stages:
            kind = st[0]
            if kind == "prep":
                self.prep(st[1])
            elif kind == "ffn":
                self.ffn(st[1], st[2])
                self.xsrc[st[2]] = self.xdst[st[2]]
            elif kind == "ret":
                self.ret(st[1], st[2])
                self.xsrc[st[2]] = self.xdst[st[2]]
            elif kind == "att":
                self.att(st[1], st[2])
                self.xsrc[st[2]] = self.xdst[st[2]]
            elif kind == "kv":
                self.kv(st[1])

    def finish(self):
        S = self.S
        S.barrier()
        return self.nc


def _kxm(w):
    K, F = w.shape
    return np.ascontiguousarray(w.reshape(K // P, P, F).transpose(1, 0, 2))


def host_weights(inp):
    out = {}
    wi = np.asarray(inp["ret_w_in"], np.float32)
    cols = []
    for h in range(RH):
        cols += list(range(h * 256, (h + 1) * 256))
        cols += list(range(1024 + h * 256, 1024 + (h + 1) * 256))
        cols += list(range(2048 + h * 512, 2048 + (h + 1) * 512))
        cols += list(range(4096 + h * 512, 4096 + (h + 1) * 512))
    cols = np.array(cols)
    out["w_ret_in"] = np.stack([_kxm(wi[l][:, cols]).reshape(P, KC, 4, 1536).transpose(0, 2, 1, 3) for l in range(2)])
    wo = np.asarray(inp["ret_w_out"], np.float32)
    out["w_ret_out"] = np.stack([_kxm(wo[l]).reshape(P, 16, KC, P).transpose(0, 2, 1, 3) for l in range(2)])
    wkv = np.asarray(inp["att_w_kv"], np.float32)
    out["w_k"] = _kxm(wkv[:, :3072]).reshape(P, KC, 24, P).transpose(0, 2, 1, 3)
    out["w_v"] = _kxm(wkv[:, 3072:]).reshape(P, KC, 6, 512).transpose(0, 2, 1, 3)
    wq = np.asarray(inp["att_w_q"], np.float32)
    out["w_q"] = np.stack([_kxm(wq[l]).reshape(P, KC, 3, 8, P).transpose(0, 3, 1, 2, 4).reshape(P, 8, KC, 384) for l in range(2)])
    wao = np.asarray(inp["att_w_o"], np.float32)
    out["w_o"] = np.stack([_kxm(wao[l]).reshape(P, KC, KC, P).transpose(0, 2, 1, 3) for l in range(2)])
    wu = np.asarray(inp["ffn_w_up"], np.float32)
    out["w_up"] = np.stack([np.concatenate([_kxm(wu[l][:, :DFF]).reshape(P, KC, NJ, P), _kxm(wu[l][:, DFF:]).reshape(P, KC, NJ, P)], axis=3)
                            .transpose(0, 2, 1, 3) for l in range(4)])
    wd = np.asarray(inp["ffn_w_down"], np.float32)
    out["w_down"] = np.stack([_kxm(wd[l]).reshape(P, NJ, KC, P).transpose(0, 2, 1, 3) for l in range(4)])
    out = {k: np.ascontiguousarray(v, dtype=np.float32) for k, v in out.items()}

    def pv(v):
        v = np.asarray(v, np.float32)
        return v.reshape(-1, P).T

    vecs = np.zeros((P, NV), np.float32)
    for l in range(4):
        vecs[:, V_MIXPRE + 8 * l:V_MIXPRE + 8 * l + 8] = pv(inp["norm_mix_pre"][l])
        vecs[:, V_MIXPOST + 8 * l:V_MIXPOST + 8 * l + 8] = pv(inp["norm_mix_post"][l])
        vecs[:, V_FFNPRE + 8 * l:V_FFNPRE + 8 * l + 8] = pv(inp["norm_ffn_pre"][l])
        vecs[:, V_FFNPOST + 8 * l:V_FFNPOST + 8 * l + 8] = pv(inp["norm_ffn_post"][l])
        for tap in range(3):
            vecs[:, V_CW + (l * 3 + tap) * NJ:V_CW + (l * 3 + tap + 1) * NJ] = pv(inp["ffn_conv_w"][l][tap])
        vecs[:, V_CB + l * NJ:V_CB + (l + 1) * NJ] = pv(inp["ffn_conv_b"][l])
    vecs[:, V_KVN:V_KVN + 8] = pv(inp["kv_norm"])
    for l in range(2):
        vecs[:, V_GN + 16 * l:V_GN + 16 * l + 16] = pv(inp["ret_gn_gain"][l])
    out["vecs"] = vecs
    return out


def host_consts():
    c = {}
    pos = np.arange(SEQ, dtype=np.float32)
    inv = (1.0 / (np.float32(10000.0) ** np.linspace(0.0, 1.0, 128, dtype=np.float32))).astype(np.float32)
    ang = (pos[None, :] * inv[:, None]).astype(np.float32)
    c["rcs"] = np.stack([np.cos(ang), np.sin(ang)], axis=1).astype(np.float32)
    invf = (np.float32(500000.0) ** (-np.arange(0, 16, 2, dtype=np.float32) / np.float32(16))).astype(np.float32)
    acs = np.zeros((P, 2, SEQ), np.float32)
    pi = np.zeros((P, P), np.float32)
    for p in range(P):
        hd = p % 64
        if hd < 8:
            a = (pos * invf[hd]).astype(np.float32)
            acs[p, 0] = np.cos(a)
            acs[p, 1] = -np.sin(a)
            pi[p + 8, p] = 1.0
        elif hd < 16:
            a = (pos * invf[hd - 8]).astype(np.float32)
            acs[p, 0] = np.cos(a)
            acs[p, 1] = np.sin(a)
            pi[p - 8, p] = 1.0
        else:
            acs[p, 0] = 1.0
    c["acs"] = acs
    cm = np.zeros((P, NCM), np.float32)
    cm[:, C_PI:C_PI + P] = pi
    cm[:, C_ID:C_ID + P] = np.eye(P, dtype=np.float32)
    n = np.arange(P, dtype=np.float64)
    for h in range(RH):
        lg = math.log(1.0 - 2.0 ** (-5.0 - h))
        dtm = np.where(n[None, :] >= n[:, None], np.exp(-(n[:, None] + 1.0) * lg) / 16.0, 0.0)
        cm[:, C_DT + h * P:C_DT + (h + 1) * P] = dtm
        cm[:, C_ZETA + h] = np.exp((127.0 - n) * lg) / 16.0
        cm[:, C_EPS + h] = GN_EPS * np.exp(-2.0 * (n + 1.0) * lg)
    kj = np.arange(P)
    cm[:, C_MPREV:C_MPREV + P] = (kj[:, None] >= kj[None, :]).astype(np.float32)
    cm[:, C_MCUR:C_MCUR + P] = (kj[:, None] <= kj[None, :]).astype(np.float32)
    c["cmat"] = cm
    return c


def build_program(nseq=NSEQ, stages=None):
    pg = Prog(nseq)
    pg.run_stages(stages)
    return pg.finish()


def kernel(**inputs):
    x = np.asarray(inputs["x"], np.float32)
    hw = host_weights(inputs)
    hc = host_consts()
    nc = build_program(NSEQ)
    in_maps = []
    for c in range(NCORES):
        m = dict(hw)
        m.update(hc)
        m["xT"] = np.ascontiguousarray(x[c * NSEQ:(c + 1) * NSEQ].transpose(0, 2, 1))
        in_maps.append(m)
    res = run_bass_kernel_spmd(nc, in_maps, core_ids=list(range(NCORES)))
    outs = [np.asarray(r["out"], np.float32).transpose(0, 2, 1) for r in res.results]
    return np.ascontiguousarray(np.concatenate(outs, axis=0))
```

```python
import math
import numpy as np
import ml_dtypes
import concourse.bass as bass
import concourse.mybir as mybir
from concourse.bass_utils import run_bass_kernel_spmd

F32 = mybir.dt.float32
BF16 = mybir.dt.bfloat16
AF = mybir.ActivationFunctionType
ALU = mybir.AluOpType

P = 128
SEQ = 2048
DM = 1024
KC = 8
NCORES = 8
BATCH = 32
NSEQ = BATCH // NCORES
DFF = 2816
NJ = DFF // P
RMS_EPS = 1e-6
GN_EPS = 1e-6
RH = 4
DILS = (1, 4, 16)
SB_BASE = 16640
FORCE_INC = False
SB_TOP = 229344

V_MIXPRE = 0
V_MIXPOST = 32
V_FFNPRE = 64
V_FFNPOST = 96
V_KVN = 128
V_GN = 136
V_CW = 168
V_CB = V_CW + 4 * 3 * NJ
NV = V_CB + 4 * NJ
C_PI = 0
C_ID = 128
C_DT = 256
C_MPREV = 768
C_MCUR = 896
C_ZETA = 1024
C_EPS = 1028
NCM = 1032


class Buf:
    __slots__ = ("name", "lw", "rd")

    def __init__(self, name=""):
        self.name = name
        self.lw = None
        self.rd = {}


class Chan:
    def __init__(self, nc, name):
        self.sem = nc.alloc_semaphore(name=name)
        self.count = 0
        self.name = name


class Sched:
    def __init__(self, nc):
        self.nc = nc
        self.engs = {"pe": nc.tensor, "act": nc.scalar, "dve": nc.vector, "pool": nc.gpsimd, "sp": nc.sync}
        self.chan = {k: Chan(nc, "c_" + k) for k in self.engs}
        self.waited = {k: {} for k in self.engs}
        self.dchans = []
        self.ninst = 0

    def dchan(self, name):
        c = Chan(self.nc, name)
        self.dchans.append(c)
        return c

    def _wait(self, e, c, v):
        if v <= 0:
            return
        w = self.waited[e]
        if w.get(c, 0) >= v:
            return
        self.engs[e].wait_ge(c.sem, v)
        self.ninst += 1
        w[c] = v

    def _deps(self, e, reads, writes):
        need = {}
        for b in reads:
            if b.lw is not None:
                c, v = b.lw
                if need.get(c, 0) < v:
                    need[c] = v
        for b in writes:
            if b.lw is not None:
                c, v = b.lw
                if need.get(c, 0) < v:
                    need[c] = v
            for c, v in b.rd.items():
                if need.get(c, 0) < v:
                    need[c] = v
        own = self.chan.get(e)
        for c, v in need.items():
            if c is own and v > c.count:
                continue
            self._wait(e, c, v)

    def _record(self, c, v, reads, writes):
        for b in reads:
            if b.rd.get(c, 0) < v:
                b.rd[c] = v
        for b in writes:
            b.lw = (c, v)
            b.rd = {}

    def op(self, e, fn, reads=(), writes=(), inc=True):
        self._deps(e, reads, writes)
        ins = fn(self.engs[e])
        self.ninst += 1
        c = self.chan[e]
        if FORCE_INC:
            inc = True
        if inc:
            c.count += 1
            ins.then_inc(c.sem, 1)
            v = c.count
        else:
            v = c.count + 1
        self._record(c, v, reads, writes)
        return ins

    def dma(self, ch, out, in_, reads=(), writes=(), e="sp"):
        self._deps(e, reads, writes)
        ins = self.engs[e].dma_start(out=out, in_=in_)
        self.ninst += 1
        ch.count += 16
        ins.then_inc(ch.sem, 16)
        self._record(ch, ch.count, reads, writes)
        return ins

    def barrier(self):
        chans = list(self.chan.values()) + self.dchans
        for e in self.engs:
            for c in chans:
                if c is self.chan[e]:
                    continue
                self._wait(e, c, c.count)


class SbAlloc:
    def __init__(self, nc, base, top=SB_TOP):
        self.nc = nc
        self.off = base
        self.top = top
        self.n = 0

    def __call__(self, shape, dtype, name="t"):
        nb = 1
        for s in shape[1:]:
            nb *= s
        nb *= 2 if dtype == BF16 else 4
        nb = (nb + 63) // 64 * 64
        assert self.off + nb <= self.top, f"SBUF overflow {name} {self.off}+{nb}>{self.top}"
        self.n += 1
        t = self.nc.alloc_sbuf_tensor_at(f"{name}{self.n}", list(shape), dtype, offset=self.off)
        self.off += nb
        return t.ap()

    def fork(self):
        return SbAlloc(self.nc, self.off, self.top)


class Prog:
    def __init__(self, nseq=NSEQ):
        self.nseq = nseq
        nc = self.nc = bass.Bass("TRN2", target_bir_lowering=False)
        S = self.S = Sched(nc)
        dt = nc.dram_tensor

        def ext(name, shape, dtype=F32):
            return dt(name, list(shape), dtype, kind="ExternalInput").ap()

        def scr(name, shape, dtype=BF16):
            return dt(name, list(shape), dtype, kind="Internal").ap()

        self.xin = ext("xT", [nseq, DM, SEQ])
        self.out = dt("out", [nseq, DM, SEQ], F32, kind="ExternalOutput").ap()
        self.wshapes = {
            "w_ret_in": [2, P, 4, KC, 1536],
            "w_ret_out": [2, P, KC, 16, P],
            "w_k": [P, 24, KC, P],
            "w_v": [P, 6, KC, 512],
            "w_q": [2, P, 8, KC, 384],
            "w_o": [2, P, KC, KC, P],
            "w_up": [4, P, NJ, KC, 256],
            "w_down": [4, P, KC, NJ, P],
        }
        self.wf = {k: ext(k, v) for k, v in self.wshapes.items()}
        self.wb = {k: scr(k + "_b", v) for k, v in self.wshapes.items()}
        self.vecs_d = ext("vecs", [P, NV])
        self.cmat_d = ext("cmat", [P, NCM])
        self.rcs_d = ext("rcs", [P, 2, SEQ])
        self.acs_d = ext("acs", [P, 2, SEQ])
        self.kt_d = scr("kt_s", [3, 8, P, SEQ])
        self.va_d = scr("va_s", [3, P, 8, 16 * 192])
        self.Bx = [[Buf(f"x{s}_{t}") for t in range(4)] for s in range(nseq)]
        self.Bwb = {k: Buf(k) for k in self.wshapes}
        self.Bkt = Buf("kt")
        self.Bva = Buf("va")
        self.xsrc = [self.xin[s] for s in range(nseq)]
        self.xdst = [self.out[s] for s in range(nseq)]
        self.ld = S.dchan("ld")
        self.ldw = [S.dchan(f"ldw{i}") for i in range(4)]
        self.st = S.dchan("st")
        self.ps = [nc.alloc_psum_tensor(f"ps{i}", [P, 512], F32).ap() for i in range(8)]
        self.Bps = [Buf(f"ps{i}") for i in range(8)]
        A = self.A0 = SbAlloc(nc, SB_BASE)
        self.vecs = A([P, NV], F32, "vecs")
        self.cmat = A([P, NCM], F32, "cmat")
        self.ones_b = A([P, P], BF16, "ones")
        self.ident_b = A([P, P], BF16, "ident")
        self.pi_b = A([P, P], BF16, "pi")
        self.mask_b = A([P, 2, P], BF16, "mask")
        self.Bconst = Buf("const")
        S.dma(self.ld, self.vecs, self.vecs_d, writes=[self.Bconst])
        S.dma(self.ld, self.cmat, self.cmat_d, writes=[self.Bconst])
        S.op("pool", lambda e: e.memset(self.ones_b, 1.0), writes=[self.Bconst])
        S.op("dve", lambda e: e.tensor_copy(out=self.ident_b, in_=self.cmat[:, C_ID:C_ID + P]), reads=[self.Bconst], writes=[self.Bconst])
        S.op("dve", lambda e: e.tensor_copy(out=self.pi_b, in_=self.cmat[:, C_PI:C_PI + P]), reads=[self.Bconst], writes=[self.Bconst])
        S.op("dve", lambda e: e.tensor_copy(out=self.mask_b, in_=self.cmat[:, C_MPREV:C_MPREV + 2 * P].rearrange("p (a b) -> p a b", a=2)),
             reads=[self.Bconst], writes=[self.Bconst])
        S.barrier()

    def prep(self, names=None):
        S, nc = self.S, self.nc
        A = self.A0.fork()
        NB = 3
        CH = 4096
        fin = [A([P, CH], F32, "pin") for _ in range(NB)]
        fout = [A([P, CH], BF16, "pout") for _ in range(NB)]
        Bin = [Buf() for _ in range(NB)]
        Bout = [Buf() for _ in range(NB)]
        k = 0
        engs = ["dve", "act"]
        for name in (names or list(self.wshapes)):
            shp = self.wshapes[name]
            nl = shp[0] if shp[0] != P else 1
            for l in range(nl):
                src = self.wf[name][l] if shp[0] != P else self.wf[name]
                dst = self.wb[name][l] if shp[0] != P else self.wb[name]
                nd = len(src.shape)
                letters = "abcd"[: nd - 1]
                pat = "p " + " ".join(letters) + " -> p (" + " ".join(letters) + ")"
                src2 = src.rearrange(pat)
                dst2 = dst.rearrange(pat)
                n = src2.shape[1]
                for c0 in range(0, n, CH):
                    w = min(CH, n - c0)
                    i = k % NB
                    S.dma(self.ld, fin[i][:, :w], src2[:, c0:c0 + w], writes=[Bin[i]])
                    if name == "w_ret_out":
                        per = 16 * P
                        for q0 in range(0, w, P):
                            ec = ((c0 + q0) % per) // P
                            col = V_GN + l * 16 + ec
                            S.op("dve", lambda e, i=i, q0=q0, col=col: e.tensor_scalar(
                                out=fout[i][:, q0:q0 + P], in0=fin[i][:, q0:q0 + P], scalar1=self.vecs[:, col:col + 1],
                                scalar2=None, op0=ALU.mult), reads=[Bin[i], self.Bconst], writes=[Bout[i]])
                    else:
                        en = engs[k % 2]
                        if en == "act":
                            S.op("act", lambda e, i=i, w=w: e.copy(out=fout[i][:, :w], in_=fin[i][:, :w]), reads=[Bin[i]], writes=[Bout[i]])
                        else:
                            S.op(en, lambda e, i=i, w=w: e.tensor_copy(out=fout[i][:, :w], in_=fin[i][:, :w]), reads=[Bin[i]], writes=[Bout[i]])
                    S.dma(self.st, dst2[:, c0:c0 + w], fout[i][:, :w], reads=[Bout[i]], writes=[self.Bwb[name]])
                    k += 1
        S.barrier()

    def x_tile(self, ap_seq, tt):
        return ap_seq[:, tt * 512:(tt + 1) * 512].rearrange("(kc p) t -> p kc t", p=P)

    def rstd_from(self, src, sq, psb, rstd, Bsrc, Bsq, Brstd):
        S = self.S
        lvl = int(getattr(self, "dbglvl", 9))
        if lvl < 2:
            return
        S.op("act", lambda e: e.activation(out=sq, in_=src, func=AF.Square), reads=[Bsrc], writes=[Bsq])
        if lvl < 3:
            return
        for kc in range(KC):
            S.op("pe", lambda e, kc=kc: e.matmul(self.ps[psb], lhsT=self.ones_b, rhs=sq[:, kc, :], start=(kc == 0), stop=(kc == KC - 1)),
                 reads=[Bsq, self.Bconst], writes=[self.Bps[psb]], inc=(kc == KC - 1))
        if lvl < 4:
            return
        S.op("act", lambda e: e.activation(out=rstd, in_=self.ps[psb], func=AF.Sqrt, bias=RMS_EPS, scale=1.0 / DM),
             reads=[self.Bps[psb]], writes=[Brstd])
        if lvl < 5:
            return
        S.op("dve", lambda e: e.reciprocal(out=rstd, in_=rstd), reads=[Brstd], writes=[Brstd])

    def phase_hT(self, s, A, hT, BhT, gcol):
        S = self.S
        xt = [A([P, KC, 512], F32, "xt") for _ in range(2)]
        sq = A([P, KC, 512], BF16, "sq")
        rs = [A([P, 512], F32, "rs") for _ in range(2)]
        Bxt = [Buf() for _ in range(2)]
        Bsq = Buf()
        Brs = [Buf() for _ in range(2)]
        for tt in range(4):
            i = tt % 2
            S.dma(self.ld, xt[i], self.x_tile(self.xsrc[s], tt), reads=[self.Bx[s][tt]], writes=[Bxt[i]])
            self.rstd_from(xt[i], sq, tt % 2, rs[i], Bxt[i], Bsq, Brs[i])
            if int(getattr(self, "dbglvl", 9)) < 6:
                continue
            for kc in range(KC):
                S.op("dve", lambda e, kc=kc, i=i, tt=tt: e.scalar_tensor_tensor(
                    out=hT[:, kc, tt * 512:(tt + 1) * 512], in0=xt[i][:, kc, :], scalar=self.vecs[:, gcol + kc:gcol + kc + 1],
                    in1=rs[i], op0=ALU.mult, op1=ALU.mult), reads=[Bxt[i], Brs[i], self.Bconst], writes=[BhT[tt]])

    def post_norm_residual(self, s, tt, fT, BfT, xt, Bxt, sq, Bsq, rs, Brs, gcol, psb):
        S = self.S
        self.rstd_from(fT, sq, psb, rs, BfT, Bsq, Brs)
        for kc in range(KC):
            S.op("pool", lambda e, kc=kc: e.tensor_tensor(out=fT[:, kc, :], in0=fT[:, kc, :], in1=rs, op=ALU.mult),
                 reads=[BfT, Brs], writes=[BfT])
        for kc in range(KC):
            S.op("dve", lambda e, kc=kc: e.scalar_tensor_tensor(
                out=xt[:, kc, :], in0=fT[:, kc, :], scalar=self.vecs[:, gcol + kc:gcol + kc + 1], in1=xt[:, kc, :],
                op0=ALU.mult, op1=ALU.add), reads=[BfT, Bxt, self.Bconst], writes=[Bxt])
        S.dma(self.st, self.x_tile(self.xdst[s], tt), xt, reads=[Bxt], writes=[self.Bx[s][tt]])

    def out_proj_phase(self, s, A, wname, l, nk, actT, BactT, gcol):
        S = self.S
        xt = [A([P, KC, 512], F32, "xt") for _ in range(2)]
        fT = A([P, KC, 512], F32, "fT")
        sq = A([P, KC, 512], BF16, "sq")
        rs = A([P, 512], F32, "rs")
        NW = 3
        wd = [A([P, nk, P], BF16, "wd") for _ in range(NW)]
        Bxt = [Buf() for _ in range(2)]
        BfT, Bsq, Brs = Buf(), Buf(), Buf()
        Bwd = [Buf() for _ in range(NW)]
        wsrc = self.wb[wname][l]
        seq = [(tt, dc) for tt in range(4) for dc in range(KC)]

        def loadw(n):
            tt, dc = seq[n]
            S.dma(self.ldw[n % NW], wd[n % NW], wsrc[:, dc], reads=[self.Bwb[wname]], writes=[Bwd[n % NW]])

        loadw(0)
        loadw(1)
        S.dma(self.ld, xt[0], self.x_tile(self.xsrc[s], 0), reads=[self.Bx[s][0]], writes=[Bxt[0]])
        for n, (tt, dc) in enumerate(seq):
            if n + 2 < len(seq):
                loadw(n + 2)
            if dc == 0 and tt + 1 < 4:
                S.dma(self.ld, xt[(tt + 1) % 2], self.x_tile(self.xsrc[s], tt + 1), reads=[self.Bx[s][tt + 1]], writes=[Bxt[(tt + 1) % 2]])
            pb = 2 + (n % 2)
            w = wd[n % NW]
            for j in range(nk):
                S.op("pe", lambda e, j=j, w=w, pb=pb, tt=tt: e.matmul(self.ps[pb], lhsT=w[:, j, :], rhs=actT[:, j, tt * 512:(tt + 1) * 512],
                                                               start=(j == 0), stop=(j == nk - 1)),
                     reads=[Bwd[n % NW], BactT], writes=[self.Bps[pb]], inc=(j == nk - 1))
            S.op("act", lambda e, dc=dc, pb=pb: e.copy(out=fT[:, dc, :], in_=self.ps[pb]), reads=[self.Bps[pb]], writes=[BfT])
            if dc == KC - 1:
                self.post_norm_residual(s, tt, fT, BfT, xt[tt % 2], Bxt[tt % 2], sq, Bsq, rs, Brs, gcol, tt % 2)

    def ffn(self, l, s):
        S = self.S
        A = self.A0.fork()
        aT = A([P, NJ, SEQ], BF16, "aT")
        BaT = Buf()
        R = A.fork()
        hT = R([P, KC, SEQ], BF16, "hT")
        BhT = [Buf() for _ in range(4)]
        self.phase_hT(s, self.A0.fork(), hT, BhT, V_FFNPRE + 8 * l)
        S.barrier()
        if getattr(self, "dbg", "") == "A":
            return
        NW = 3
        wu = [R([P, KC, 256], BF16, "wu") for _ in range(NW)]
        Bwu = [Buf() for _ in range(NW)]
        gb = R([P, 2 + SEQ], F32, "gb")
        Bgb = [Buf() for _ in range(4)]
        ct = [R([P, 512], F32, "ct") for _ in range(2)]
        cg = [R([P, 512], F32, "cg") for _ in range(2)]
        Bct = [Buf() for _ in range(2)]
        Bcg = [Buf() for _ in range(2)]
        S.op("pool", lambda e: e.memset(gb[:, 0:2], 0.0), writes=[Bgb[0]])
        wsrc = self.wb["w_up"][l]

        def loadw(j):
            S.dma(self.ldw[j % NW], wu[j % NW], wsrc[:, j], reads=[self.Bwb["w_up"]], writes=[Bwu[j % NW]])

        loadw(0)
        loadw(1)
        cwb = V_CW + l * 3 * NJ
        cbb = V_CB + l * NJ
        n = 0
        for j in range(NJ):
            if j + 2 < NJ:
                loadw(j + 2)
            w = wu[j % NW]
            for tt in range(4):
                pg, pv = 4 + 2 * (n % 2), 5 + 2 * (n % 2)
                i = n % 2
                cols = slice(tt * 512, (tt + 1) * 512)
                for half, pb in ((0, pg), (1, pv)):
                    for kc in range(KC):
                        S.op("pe", lambda e, kc=kc, pb=pb, half=half, w=w, cols=cols: e.matmul(
                            self.ps[pb], lhsT=w[:, kc, half * P:(half + 1) * P], rhs=hT[:, kc, cols], start=(kc == 0), stop=(kc == KC - 1)),
                            reads=[Bwu[j % NW], BhT[tt]], writes=[self.Bps[pb]], inc=(kc == KC - 1))
                S.op("act", lambda e, pg=pg, tt=tt: e.copy(out=gb[:, 2 + tt * 512:2 + (tt + 1) * 512], in_=self.ps[pg]),
                     reads=[self.Bps[pg]], writes=[Bgb[tt]])
                rb = [Bgb[tt]] + ([Bgb[tt - 1]] if tt > 0 else [])
                S.op("dve", lambda e, tt=tt, i=i, j=j: e.tensor_scalar(
                    out=ct[i], in0=gb[:, 2 + tt * 512:2 + (tt + 1) * 512], scalar1=self.vecs[:, cwb + 2 * NJ + j:cwb + 2 * NJ + j + 1],
                    scalar2=self.vecs[:, cbb + j:cbb + j + 1], op0=ALU.mult, op1=ALU.add), reads=rb + [self.Bconst], writes=[Bct[i]])
                for tap in (1, 0):
                    sh = 2 - tap
                    S.op("dve", lambda e, tt=tt, i=i, j=j, tap=tap, sh=sh: e.scalar_tensor_tensor(
                        out=ct[i], in0=gb[:, 2 - sh + tt * 512:2 - sh + (tt + 1) * 512],
                        scalar=self.vecs[:, cwb + tap * NJ + j:cwb + tap * NJ + j + 1], in1=ct[i], op0=ALU.mult, op1=ALU.add),
                        reads=rb + [Bct[i], self.Bconst], writes=[Bct[i]])
                S.op("act", lambda e, i=i: e.activation(out=cg[i], in_=ct[i], func=AF.Gelu_apprx_tanh), reads=[Bct[i]], writes=[Bcg[i]])
                S.op("dve", lambda e, i=i, j=j, cols=cols, pv=pv: e.tensor_tensor(out=aT[:, j, cols], in0=cg[i], in1=self.ps[pv], op=ALU.mult),
                     reads=[Bcg[i], self.Bps[pv]], writes=[BaT])
                n += 1
        S.barrier()
        if getattr(self, "dbg", "") == "B":
            return
        self.out_proj_phase(s, A.fork(), "w_down", l, NJ, aT, BaT, V_FFNPOST + 8 * l)
        S.barrier()


    def ret(self, l, s):
        S = self.S
        A = self.A0.fork()
        yT = A([P, 16, SEQ], BF16, "yT")
        ByT = Buf()
        R = A.fork()
        hT = R([P, KC, SEQ], BF16, "hT")
        BhT = [Buf() for _ in range(4)]
        self.phase_hT(s, self.A0.fork(), hT, BhT, V_MIXPRE + 8 * l)
        S.barrier()
        R2 = R.fork()
        cs = R([P, 2, SEQ], F32, "cs")
        Bcs = Buf()
        S.dma(self.ld, cs, self.rcs_d, writes=[Bcs])
        wsl = R([P, KC, 1536], BF16, "wsl")
        Bw = Buf()
        qk = [R([P, 4, 512], BF16, "qk") for _ in range(2)]
        vv = [R([P, 4, 512], BF16, "vv") for _ in range(2)]
        sg = [R([P, 4, 512], BF16, "sg") for _ in range(2)]
        Bqk = [[Buf(), Buf()] for _ in range(2)]
        Bvv = [[Buf() for _ in range(4)] for _ in range(2)]
        Bsg = [[Buf() for _ in range(4)] for _ in range(2)]
        tm = [R([P, 512], F32, "tm") for _ in range(4)]
        Btm = [Buf() for _ in range(4)]
        stf = R([P, 2, 512], F32, "stf")
        stb = R([P, 2, 512], BF16, "stb")
        Bstf, Bstb = Buf(), Buf()
        sT = [R([P, P], BF16, "sT") for _ in range(2)]
        kz = [R([P, 256], BF16, "kz") for _ in range(2)]
        yn = [R([P, 512], F32, "yn") for _ in range(2)]
        gt = [R([P, 512], BF16, "gt") for _ in range(2)]
        bst = [R([P, 6], F32, "bst") for _ in range(2)]
        mv = [R([P, 2], F32, "mv") for _ in range(2)]
        rg = [R([P, 2], F32, "rg") for _ in range(2)]
        BsT = [Buf() for _ in range(2)]
        Bkz = [Buf() for _ in range(2)]
        Byn = [Buf() for _ in range(2)]
        Bgt = [Buf() for _ in range(2)]
        Bsm = [Buf() for _ in range(2)]
        ps, Bps = self.ps, self.Bps
        psK = ps[3].bitcast(BF16)[:, 0:256]
        psT = ps[3].bitcast(BF16)[:, 512:1024]
        BpsK, BpsT = Buf(), Buf()
        proj_banks = [0, 1, 7]
        pbn = [0]
        wsrc = self.wb["w_ret_in"][l]
        units = [(h, st) for h in range(RH) for st in range(4)]

        def nextbank():
            b = proj_banks[pbn[0] % 3]
            pbn[0] += 1
            return b

        def load_w(h):
            S.dma(self.ldw[0], wsl, wsrc[:, h], reads=[self.Bwb["w_ret_in"]], writes=[Bw])

        def qk_pair(u, which):
            h, st = units[u]
            i = u % 2
            cols = slice(st * 512, (st + 1) * 512)
            banks = []
            for half in range(2):
                fi = which * 2 + half
                b = nextbank()
                banks.append(b)
                for kc in range(KC):
                    S.op("pe", lambda e, kc=kc, b=b, fi=fi: e.matmul(ps[b], lhsT=wsl[:, kc, fi * P:(fi + 1) * P], rhs=hT[:, kc, cols],
                                                                 start=(kc == 0), stop=(kc == KC - 1)),
                         reads=[Bw, BhT[st]], writes=[Bps[b]], inc=(kc == KC - 1))
            b0, b1 = banks
            cosv, sinv = cs[:, 0, cols], cs[:, 1, cols]
            o = which * 2
            S.op("dve", lambda e: e.tensor_tensor(out=tm[0], in0=ps[b0], in1=cosv, op=ALU.mult), reads=[Bps[b0], Bcs], writes=[Btm[0]])
            S.op("dve", lambda e: e.tensor_tensor(out=tm[1], in0=ps[b1], in1=sinv, op=ALU.mult), reads=[Bps[b1], Bcs], writes=[Btm[1]])
            S.op("dve", lambda e: e.tensor_tensor(out=tm[2], in0=ps[b1], in1=cosv, op=ALU.mult), reads=[Bps[b1], Bcs], writes=[Btm[2]])
            S.op("dve", lambda e: e.tensor_tensor(out=tm[3], in0=ps[b0], in1=sinv, op=ALU.mult), reads=[Bps[b0], Bcs], writes=[Btm[3]])
            S.op("dve", lambda e: e.tensor_tensor(out=qk[i][:, o, :], in0=tm[0], in1=tm[1], op=ALU.subtract),
                 reads=[Btm[0], Btm[1]], writes=[Bqk[i][which]])
            S.op("dve", lambda e: e.tensor_tensor(out=qk[i][:, o + 1, :], in0=tm[2], in1=tm[3], op=ALU.add),
                 reads=[Btm[2], Btm[3]], writes=[Bqk[i][which]])

        def vg_part(u, c):
            h, st = units[u]
            i = u % 2
            t0 = st * 512 + c * P
            for which in range(2):
                b = nextbank()
                for kc in range(KC):
                    S.op("pe", lambda e, kc=kc, b=b, which=which: e.matmul(
                        ps[b], lhsT=hT[:, kc, t0:t0 + P], rhs=wsl[:, kc, 512 + which * 512:1024 + which * 512],
                        start=(kc == 0), stop=(kc == KC - 1)), reads=[Bw, BhT[st]], writes=[Bps[b]], inc=(kc == KC - 1))
                if which == 0:
                    S.op("act", lambda e, b=b: e.copy(out=vv[i][:, c, :], in_=ps[b]), reads=[Bps[b]], writes=[Bvv[i][c]])
                else:
                    S.op("act", lambda e, b=b: e.activation(out=sg[i][:, c, :], in_=ps[b], func=AF.Silu), reads=[Bps[b]], writes=[Bsg[i][c]])

        def proj_parts(u):
            return [lambda: (qk_pair(u, 0), vg_part(u, 0)), lambda: (qk_pair(u, 1), vg_part(u, 1)),
                    lambda: vg_part(u, 2), lambda: vg_part(u, 3)]

        def front(u, c):
            h, st = units[u]
            i = u % 2
            k = c % 2
            cc = slice(c * P, (c + 1) * P)
            for j in range(2):
                S.op("pe", lambda e, j=j: e.matmul(ps[2][:, 0:P], lhsT=qk[i][:, 2 + j, cc], rhs=qk[i][:, j, cc], start=(j == 0), stop=(j == 1)),
                     reads=[Bqk[i][0], Bqk[i][1]], writes=[Bps[2]])
            for j in range(2):
                S.op("pe", lambda e, j=j: e.transpose(psK[:, j * P:(j + 1) * P], qk[i][:, 2 + j, cc], self.ident_b),
                     reads=[Bqk[i][1], self.Bconst], writes=[BpsK])
            S.op("dve", lambda e: e.tensor_tensor(out=sT[k], in0=ps[2][:, 0:P], in1=self.cmat[:, C_DT + h * P:C_DT + (h + 1) * P], op=ALU.mult),
                 reads=[Bps[2], self.Bconst], writes=[BsT[k]])
            S.op("act", lambda e: e.activation(out=kz[k], in_=psK, func=AF.Identity, scale=self.cmat[:, C_ZETA + h:C_ZETA + h + 1]),
                 reads=[BpsK, self.Bconst], writes=[Bkz[k]])

        def back(u, c):
            h, st = units[u]
            i = u % 2
            k = c % 2
            cg = st * 4 + c
            cc = slice(c * P, (c + 1) * P)
            lg = math.log(1.0 - 2.0 ** (-5.0 - h))
            gC = math.exp(128.0 * lg)
            S.op("pe", lambda e: e.matmul(ps[4], lhsT=sT[k], rhs=vv[i][:, c, :], start=True, stop=(cg == 0)),
                 reads=[BsT[k], Bvv[i][c]], writes=[Bps[4]])
            if cg > 0:
                for j in range(2):
                    S.op("pe", lambda e, j=j: e.matmul(ps[4], lhsT=qk[i][:, j, cc], rhs=stb[:, j, :], start=False, stop=(j == 1)),
                         reads=[Bqk[i][0], Bstb], writes=[Bps[4]])
            if cg < 15:
                for j in range(2):
                    S.op("pe", lambda e, j=j: e.matmul(ps[5 + j], lhsT=kz[k][:, j * P:(j + 1) * P], rhs=vv[i][:, c, :], start=True, stop=True),
                         reads=[Bkz[k], Bvv[i][c]], writes=[Bps[5 + j]])
                for j in range(2):
                    if cg == 0:
                        S.op("dve", lambda e, j=j: e.tensor_copy(out=stf[:, j, :], in_=ps[5 + j]), reads=[Bps[5 + j]], writes=[Bstf])
                    else:
                        S.op("dve", lambda e, j=j: e.scalar_tensor_tensor(out=stf[:, j, :], in0=stf[:, j, :], scalar=gC, in1=ps[5 + j],
                                                                         op0=ALU.mult, op1=ALU.add), reads=[Bps[5 + j], Bstf], writes=[Bstf])
                S.op("act", lambda e: e.copy(out=stb, in_=stf), reads=[Bstf], writes=[Bstb])
            S.op("dve", lambda e: e.bn_stats(out=bst[k], in_=ps[4]), reads=[Bps[4]], writes=[Bsm[k]])
            S.op("dve", lambda e: e.bn_aggr(out=mv[k], in_=bst[k]), reads=[Bsm[k]], writes=[Bsm[k]])
            S.op("act", lambda e: e.activation(out=rg[k][:, 0:1], in_=mv[k][:, 1:2], func=AF.Sqrt, bias=self.cmat[:, C_EPS + h:C_EPS + h + 1], scale=1.0),
                 reads=[Bsm[k], self.Bconst], writes=[Bsm[k]])
            S.op("dve", lambda e: e.reciprocal(out=rg[k][:, 0:1], in_=rg[k][:, 0:1]), reads=[Bsm[k]], writes=[Bsm[k]])
            S.op("dve", lambda e: e.scalar_tensor_tensor(out=rg[k][:, 1:2], in0=mv[k][:, 0:1], scalar=-1.0, in1=rg[k][:, 0:1], op0=ALU.mult, op1=ALU.mult),
                 reads=[Bsm[k]], writes=[Bsm[k]])
            S.op("act", lambda e: e.activation(out=yn[k], in_=ps[4], func=AF.Identity, bias=rg[k][:, 1:2], scale=rg[k][:, 0:1]),
                 reads=[Bps[4], Bsm[k]], writes=[Byn[k]])
            S.op("dve", lambda e: e.tensor_tensor(out=gt[k], in0=yn[k], in1=sg[i][:, c, :], op=ALU.mult),
                 reads=[Byn[k], Bsg[i][c]], writes=[Bgt[k]])

        def ytr(u, c):
            h, st = units[u]
            k = c % 2
            cg = st * 4 + c
            for jj in range(4):
                S.op("pe", lambda e, jj=jj: e.transpose(psT[:, jj * P:(jj + 1) * P], gt[k][:, jj * P:(jj + 1) * P], self.ident_b),
                     reads=[Bgt[k], self.Bconst], writes=[BpsT])
            S.op("act", lambda e: e.copy(out=yT[:, 4 * h:4 * h + 4, cg * P:(cg + 1) * P], in_=psT.rearrange("p (a b) -> p a b", a=4)),
                 reads=[BpsT], writes=[ByT])

        load_w(0)
        for f in proj_parts(0):
            f()
        pending = None
        for u in range(len(units)):
            nxt = [None] * 4
            if u + 1 < len(units):
                if units[u + 1][0] != units[u][0]:
                    load_w(units[u + 1][0])
                nxt = proj_parts(u + 1)
            for c in range(4):
                front(u, c)
                if nxt[c] is not None:
                    nxt[c]()
                back(u, c)
                if pending is not None:
                    ytr(*pending)
                pending = (u, c)
        ytr(*pending)
        S.barrier()
        self.out_proj_phase(s, R2, "w_ret_out", l, 16, yT, ByT, V_MIXPOST + 8 * l)
        S.barrier()


    @staticmethod
    def gcols(ap2d, g, u):
        if g == 0:
            return ap2d[:, u * 512:(u + 1) * 512]
        if g == 1:
            return ap2d[:, u::4]
        return ap2d.rearrange("p (i r) -> p r i", r=16)[:, 4 * u:4 * u + 4, :]

    @staticmethod
    def gview(ap512, g):
        return ap512 if g < 2 else ap512.rearrange("p (r i) -> p r i", r=4)

    @staticmethod
    def chunk_tokens(ap2d, g, c):
        if g == 0:
            return ap2d[..., c * P:(c + 1) * P] if False else ap2d[:, c * P:(c + 1) * P]
        if g == 1:
            st = 512 * (c % 4) + c // 4
            return ap2d[:, st:st + 509:4]
        return ap2d[:, c::16]

    def rot_proj(self, g, u, w_lhsT, hT, BhT, Bw, cs, Bcs, qraw, Bqraw, t1, t2, Bt, out512, Bout, pb, pb2):
        S, ps, Bps = self.S, self.ps, self.Bps
        for kc in range(KC):
            S.op("pe", lambda e, kc=kc: e.matmul(self.gview(ps[pb], g), lhsT=w_lhsT[:, kc, :], rhs=self.gcols(hT[:, kc, :], g, u),
                                               start=(kc == 0), stop=(kc == KC - 1)), reads=[Bw] + BhT, writes=[Bps[pb]], inc=(kc == KC - 1))
        S.op("act", lambda e: e.copy(out=qraw, in_=ps[pb]), reads=[Bps[pb]], writes=[Bqraw])
        S.op("pe", lambda e: e.matmul(ps[pb2], lhsT=self.pi_b, rhs=qraw, start=True, stop=True), reads=[Bqraw, self.Bconst], writes=[Bps[pb2]])
        S.op("dve", lambda e: e.tensor_tensor(out=self.gview(t1, g), in0=self.gview(ps[pb2], g), in1=self.gcols(cs[:, 1, :], g, u), op=ALU.mult),
             reads=[Bps[pb2], Bcs], writes=[Bt[0]])
        S.op("dve", lambda e: e.tensor_tensor(out=self.gview(t2, g), in0=self.gview(ps[pb], g), in1=self.gcols(cs[:, 0, :], g, u), op=ALU.mult),
             reads=[Bps[pb], Bcs], writes=[Bt[1]])
        if isinstance(out512, tuple):
            for rows, o in out512:
                S.op("dve", lambda e, rows=rows, o=o: e.tensor_tensor(out=o, in0=t1[rows, :], in1=t2[rows, :], op=ALU.add),
                     reads=[Bt[0], Bt[1]], writes=[Bout])
        else:
            S.op("dve", lambda e: e.tensor_tensor(out=out512, in0=t1, in1=t2, op=ALU.add), reads=[Bt[0], Bt[1]], writes=[Bout])

    def kv(self, s):
        S = self.S
        A = self.A0.fork()
        hT = A([P, KC, SEQ], BF16, "hT")
        BhT = [Buf() for _ in range(4)]
        self.phase_hT(s, A.fork(), hT, BhT, V_KVN)
        S.barrier()
        R = A.fork()
        cs = R([P, 2, SEQ], F32, "cs")
        Bcs = Buf()
        S.dma(self.ld, cs, self.acs_d, writes=[Bcs])
        wk = [R([P, KC, P], BF16, "wk") for _ in range(3)]
        Bwk = [Buf() for _ in range(3)]
        qraw = [R([P, 512], BF16, "qraw") for _ in range(2)]
        t1 = [R([P, 512], F32, "t1") for _ in range(2)]
        t2 = [R([P, 512], F32, "t2") for _ in range(2)]
        Bqraw = [Buf() for _ in range(2)]
        Bt = [[Buf(), Buf()] for _ in range(2)]
        kst = [R([P, SEQ], BF16, "kst") for _ in range(2)]
        Bkst = [Buf() for _ in range(2)]
        wv = R([P, 2, KC, 512], BF16, "wv")
        Bwv = Buf()
        stg = R([P, 8, 16, 192], BF16, "stg")
        Bstg = Buf()
        S.op("dve", lambda e: e.memset(stg.rearrange("p a c x -> p (a c x)"), 1.0), writes=[Bstg])
        wsrc = self.wb["w_k"]

        def loadk(gp):
            S.dma(self.ldw[gp % 3], wk[gp % 3], wsrc[:, gp], reads=[self.Bwb["w_k"]], writes=[Bwk[gp % 3]])

        loadk(0)
        loadk(1)
        n = 0
        for gp in range(24):
            g, hp = gp // 8, gp % 8
            if gp + 2 < 24:
                loadk(gp + 2)
            ks = kst[gp % 2]
            for u in range(4):
                i = n % 2
                self.rot_proj(g, u, wk[gp % 3], hT, BhT, Bwk[gp % 3], cs, Bcs, qraw[i], Bqraw[i], t1[i], t2[i], Bt[i],
                              ks[:, u * 512:(u + 1) * 512], Bkst[gp % 2], 0 + i, 2 + i)
                n += 1
            S.dma(self.st, self.kt_d[g, hp], ks, reads=[Bkst[gp % 2]], writes=[self.Bkt])
        ps, Bps = self.ps, self.Bps
        n = 0
        for g in range(3):
            S.dma(self.ldw[3], wv, self.wb["w_v"][:, 2 * g:2 * g + 2], reads=[self.Bwb["w_v"]], writes=[Bwv])
            for c in range(16):
                for half in range(2):
                    pb = 4 + (n % 4)
                    n += 1
                    for kc in range(KC):
                        S.op("pe", lambda e, kc=kc, pb=pb, half=half: e.matmul(ps[pb], lhsT=self.chunk_tokens(hT[:, kc, :], g, c), rhs=wv[:, half, kc, :],
                                                                         start=(kc == 0), stop=(kc == KC - 1)), reads=[Bwv] + BhT, writes=[Bps[pb]], inc=(kc == KC - 1))
                    pv = ps[pb].rearrange("p (a h d) -> p a h d", a=4, h=2)
                    for hh in range(2):
                        o = stg[:, half * 4:half * 4 + 4, c, hh * 128:hh * 128 + 64]
                        if hh == 0:
                            S.op("act", lambda e, o=o, pv=pv: e.copy(out=o, in_=pv[:, :, 0, :]), reads=[Bps[pb]], writes=[Bstg])
                        else:
                            S.op("dve", lambda e, o=o, pv=pv: e.tensor_copy(out=o, in_=pv[:, :, 1, :]), reads=[Bps[pb]], writes=[Bstg])
            S.dma(self.st, self.va_d[g], stg.rearrange("p a c x -> p a (c x)"), reads=[Bstg], writes=[self.Bva])
        S.barrier()

    def att(self, l, s):
        S = self.S
        li = l - 2
        A = self.A0.fork()
        oT = A([P, 8, SEQ], BF16, "oT")
        BoT = Buf()
        R = A.fork()
        R0 = R.fork()
        hT = R([P, KC, SEQ], BF16, "hT")
        BhT = [Buf() for _ in range(4)]
        self.phase_hT(s, R.fork(), hT, BhT, V_MIXPRE + 8 * l)
        S.barrier()
        cs = R([P, 2, SEQ], F32, "cs")
        Bcs = Buf()
        S.dma(self.ld, cs, self.acs_d, writes=[Bcs])
        wq = [R([P, KC, 384], BF16, "wq") for _ in range(2)]
        Bwq = [Buf() for _ in range(2)]
        kt = R([P, 3, SEQ], BF16, "kt")
        vt = R([P, 3, 16 * 192], BF16, "vt")
        Bkt, Bvt = Buf(), Buf()
        qT = R([P, 3, 2, SEQ], BF16, "qT")
        BqT = [Buf() for _ in range(3)]
        S.op("dve", lambda e: e.memset(qT.rearrange("p g h t -> p (g h t)"), 0.0), writes=BqT)
        acc = R([P, 2, SEQ], F32, "acc")
        Bacc = Buf()
        qraw = [R([P, 512], BF16, "qraw") for _ in range(2)]
        t1 = [R([P, 512], F32, "t1") for _ in range(2)]
        t2 = [R([P, 512], F32, "t2") for _ in range(2)]
        Bqraw = [Buf() for _ in range(2)]
        Bt = [[Buf(), Buf()] for _ in range(2)]
        PT = [R([P, 2, 2, P], BF16, "PT") for _ in range(2)]
        BpP = [Buf() for _ in range(2)]
        BPT = [Buf() for _ in range(2)]
        tden = R([P, SEQ], F32, "tden")
        Btden = Buf()
        ps, Bps = self.ps, self.Bps
        psTb = ps[6].bitcast(BF16)
        psT = [psTb[:, k * 512:(k + 1) * 512].rearrange("p (h c q) -> p h c q", h=2, c=2) for k in range(2)]
        psU = [ps[7][:, k * 256:(k + 1) * 256].rearrange("p (h q) -> p h q", h=2) for k in range(2)]
        BpsT = [Buf() for _ in range(2)]
        BpsU = [Buf() for _ in range(2)]
        wsrc = self.wb["w_q"][li]

        def loadq(hp):
            S.dma(self.ldw[hp % 2], wq[hp % 2], wsrc[:, hp], reads=[self.Bwb["w_q"]], writes=[Bwq[hp % 2]])

        loadq(0)
        n = 0
        m = 0
        for hp in range(8):
            if hp + 1 < 8:
                loadq(hp + 1)
            S.dma(self.ldw[2], kt, self.kt_d[:, hp].rearrange("g p t -> p g t"), reads=[self.Bkt], writes=[Bkt])
            S.dma(self.ldw[3], vt, self.va_d[:, :, hp].rearrange("g p x -> p g x"), reads=[self.Bva], writes=[Bvt])
            w = wq[hp % 2]
            for g in range(3):
                for u in range(4):
                    i = n % 2
                    self.rot_proj(g, u, w[:, :, g * P:(g + 1) * P], hT, BhT, Bwq[hp % 2], cs, Bcs, qraw[i], Bqraw[i], t1[i], t2[i], Bt[i],
                                  ((slice(0, 64), qT[0:64, g, 0, u * 512:(u + 1) * 512]), (slice(64, 128), qT[64:128, g, 1, u * 512:(u + 1) * 512])),
                                  BqT[g], 0 + i, 2 + i)
                    n += 1
            for g in range(3):
                for c in range(16):
                    has_prev = (c > 0) if g == 0 else ((c % 4) > 0 if g == 1 else False)
                    nkc = 2 if has_prev else 1
                    nk = nkc * P
                    k0 = (c - 1) * P if has_prev else c * P
                    k = m % 2
                    m += 1
                    pS = 4 + k
                    pSv = ps[pS].rearrange("p (c h q) -> p c h q", c=2, h=2)
                    for kc in range(nkc):
                        S.op("pe", lambda e, kc=kc: e.matmul(pSv[:, kc], lhsT=kt[:, g, k0 + kc * P:k0 + (kc + 1) * P], rhs=qT[:, g, :, c * P:(c + 1) * P],
                                                          start=True, stop=True), reads=[BqT[g], Bkt], writes=[Bps[pS]])
                    S.op("act", lambda e: e.activation(out=PT[k][:, 0:nkc], in_=pSv[:, 0:nkc], func=AF.Exp, scale=0.125),
                         reads=[Bps[pS]], writes=[BPT[k]])
                    msk = self.mask_b[:, 2 - nkc:2, :].unsqueeze(2).broadcast_to([P, nkc, 2, P])
                    S.op("dve", lambda e, msk=msk: e.tensor_tensor(out=PT[k][:, 0:nkc], in0=PT[k][:, 0:nkc], in1=msk, op=ALU.mult),
                         reads=[BPT[k], self.Bconst], writes=[BPT[k]])
                    for hh in range(2):
                        for kc in range(nkc):
                            ch = (c - 1 + kc) if has_prev else c
                            S.op("pe", lambda e, hh=hh, kc=kc, ch=ch: e.matmul(psU[k][:, hh, :], lhsT=vt[:, g, ch * 192 + hh * 64:ch * 192 + hh * 64 + P],
                                                                            rhs=PT[k][:, kc, hh, :], start=(kc == 0), stop=(kc == nkc - 1)),
                                 reads=[Bvt, BPT[k]], writes=[BpsU[k]], inc=(kc == nkc - 1))
                    if g == 0:
                        S.op("act", lambda e: e.copy(out=acc[:, :, c * P:(c + 1) * P], in_=psU[k]), reads=[BpsU[k]], writes=[Bacc])
                    else:
                        if g == 1:
                            st = 512 * (c % 4) + c // 4
                            dst = acc[:, :, st:st + 509:4]
                        else:
                            dst = acc[:, :, c::16]
                        S.op("dve", lambda e, dst=dst: e.tensor_tensor(out=dst, in0=dst, in1=psU[k], op=ALU.add), reads=[BpsU[k], Bacc], writes=[Bacc])
            S.op("act", lambda e: e.copy(out=tden[0:64, :], in_=acc[64:128, 0, :]), reads=[Bacc], writes=[Btden])
            S.op("act", lambda e: e.copy(out=tden[64:128, :], in_=acc[0:64, 1, :]), reads=[Bacc], writes=[Btden])
            S.op("dve", lambda e: e.reciprocal(out=tden, in_=tden), reads=[Btden], writes=[Btden])
            S.op("dve", lambda e, hp=hp: e.tensor_tensor(out=oT[0:64, hp, :], in0=acc[0:64, 0, :], in1=tden[0:64, :], op=ALU.mult),
                 reads=[Bacc, Btden], writes=[BoT])
            S.op("dve", lambda e, hp=hp: e.tensor_tensor(out=oT[64:128, hp, :], in0=acc[64:128, 1, :], in1=tden[64:128, :], op=ALU.mult),
                 reads=[Bacc, Btden], writes=[BoT])
        S.barrier()
        self.out_proj_phase(s, R0, "w_o", li, 8, oT, BoT, V_MIXPOST + 8 * l)
        S.barrier()

    def run_stages(self, stages=None):
        if stages is None:
            stages = [("prep", None)]
            for s in range(self.nseq):
                for l in range(4):
                    stages.append(("ret" if l < 2 else "att", l, s))
                    stages.append(("ffn", l, s))
                    if l == 1:
                        stages.append(("kv", s))
        for st in stages:
            kind = st[0]
            if kind == "prep":
                self.prep(st[1])
            elif kind == "ffn":
                self.ffn(st[1], st[2])
                self.xsrc[st[2]] = self.xdst[st[2]]
            elif kind == "ret":
                self.ret(st[1], st[2])
                self.xsrc[st[2]] = self.xdst[st[2]]
            elif kind == "att":
                self.att(st[1], st[2])
                self.xsrc[st[2]] = self.xdst[st[2]]
            elif kind == "kv":
                self.kv(st[1])

    def finish(self):
        S = self.S
        S.barrier()
        return self.nc


def _kxm(w):
    K, F = w.shape
    return np.ascontiguousarray(w.reshape(K // P, P, F).transpose(1, 0, 2))


def host_weights(inp):
    out = {}
    wi = np.asarray(inp["ret_w_in"], np.float32)
    cols = []
    for h in range(RH):
        cols += list(range(h * 256, (h + 1) * 256))
        cols += list(range(1024 + h * 256, 1024 + (h + 1) * 256))
        cols += list(range(2048 + h * 512, 2048 + (h + 1) * 512))
        cols += list(range(4096 + h * 512, 4096 + (h + 1) * 512))
    cols = np.array(cols)
    out["w_ret_in"] = np.stack([_kxm(wi[l][:, cols]).reshape(P, KC, 4, 1536).transpose(0, 2, 1, 3) for l in range(2)])
    wo = np.asarray(inp["ret_w_out"], np.float32)
    out["w_ret_out"] = np.stack([_kxm(wo[l]).reshape(P, 16, KC, P).transpose(0, 2, 1, 3) for l in range(2)])
    wkv = np.asarray(inp["att_w_kv"], np.float32)
    out["w_k"] = _kxm(wkv[:, :3072]).reshape(P, KC, 24, P).transpose(0, 2, 1, 3)
    out["w_v"] = _kxm(wkv[:, 3072:]).reshape(P, KC, 6, 512).transpose(0, 2, 1, 3)
    wq = np.asarray(inp["att_w_q"], np.float32)
    out["w_q"] = np.stack([_kxm(wq[l]).reshape(P, KC, 3, 8, P).transpose(0, 3, 1, 2, 4).reshape(P, 8, KC, 384) for l in range(2)])
    wao = np.asarray(inp["att_w_o"], np.float32)
    out["w_o"] = np.stack([_kxm(wao[l]).reshape(P, KC, KC, P).transpose(0, 2, 1, 3) for l in range(2)])
    wu = np.asarray(inp["ffn_w_up"], np.float32)
    out["w_up"] = np.stack([np.concatenate([_kxm(wu[l][:, :DFF]).reshape(P, KC, NJ, P), _kxm(wu[l][:, DFF:]).reshape(P, KC, NJ, P)], axis=3)
                            .transpose(0, 2, 1, 3) for l in range(4)])
    wd = np.asarray(inp["ffn_w_down"], np.float32)
    out["w_down"] = np.stack([_kxm(wd[l]).reshape(P, NJ, KC, P).transpose(0, 2, 1, 3) for l in range(4)])
    out = {k: np.ascontiguousarray(v, dtype=np.float32) for k, v in out.items()}

    def pv(v):
        v = np.asarray(v, np.float32)
        return v.reshape(-1, P).T

    vecs = np.zeros((P, NV), np.float32)
    for l in range(4):
        vecs[:, V_MIXPRE + 8 * l:V_MIXPRE + 8 * l + 8] = pv(inp["norm_mix_pre"][l])
        vecs[:, V_MIXPOST + 8 * l:V_MIXPOST + 8 * l + 8] = pv(inp["norm_mix_post"][l])
        vecs[:, V_FFNPRE + 8 * l:V_FFNPRE + 8 * l + 8] = pv(inp["norm_ffn_pre"][l])
        vecs[:, V_FFNPOST + 8 * l:V_FFNPOST + 8 * l + 8] = pv(inp["norm_ffn_post"][l])
        for tap in range(3):
            vecs[:, V_CW + (l * 3 + tap) * NJ:V_CW + (l * 3 + tap + 1) * NJ] = pv(inp["ffn_conv_w"][l][tap])
        vecs[:, V_CB + l * NJ:V_CB + (l + 1) * NJ] = pv(inp["ffn_conv_b"][l])
    vecs[:, V_KVN:V_KVN + 8] = pv(inp["kv_norm"])
    for l in range(2):
        vecs[:, V_GN + 16 * l:V_GN + 16 * l + 16] = pv(inp["ret_gn_gain"][l])
    out["vecs"] = vecs
    return out


def host_consts():
    c = {}
    pos = np.arange(SEQ, dtype=np.float32)
    inv = (1.0 / (np.float32(10000.0) ** np.linspace(0.0, 1.0, 128, dtype=np.float32))).astype(np.float32)
    ang = (pos[None, :] * inv[:, None]).astype(np.float32)
    c["rcs"] = np.stack([np.cos(ang), np.sin(ang)], axis=1).astype(np.float32)
    invf = (np.float32(500000.0) ** (-np.arange(0, 16, 2, dtype=np.float32) / np.float32(16))).astype(np.float32)
    acs = np.zeros((P, 2, SEQ), np.float32)
    pi = np.zeros((P, P), np.float32)
    for p in range(P):
        hd = p % 64
        if hd < 8:
            a = (pos * invf[hd]).astype(np.float32)
            acs[p, 0] = np.cos(a)
            acs[p, 1] = -np.sin(a)
            pi[p + 8, p] = 1.0
        elif hd < 16:
            a = (pos * invf[hd - 8]).astype(np.float32)
            acs[p, 0] = np.cos(a)
            acs[p, 1] = np.sin(a)
            pi[p - 8, p] = 1.0
        else:
            acs[p, 0] = 1.0
    c["acs"] = acs
    cm = np.zeros((P, NCM), np.float32)
    cm[:, C_PI:C_PI + P] = pi
    cm[:, C_ID:C_ID + P] = np.eye(P, dtype=np.float32)
    n = np.arange(P, dtype=np.float64)
    for h in range(RH):
        lg = math.log(1.0 - 2.0 ** (-5.0 - h))
        dtm = np.where(n[None, :] >= n[:, None], np.exp(-(n[:, None] + 1.0) * lg) / 16.0, 0.0)
        cm[:, C_DT + h * P:C_DT + (h + 1) * P] = dtm
        cm[:, C_ZETA + h] = np.exp((127.0 - n) * lg) / 16.0
        cm[:, C_EPS + h] = GN_EPS * np.exp(-2.0 * (n + 1.0) * lg)
    kj = np.arange(P)
    cm[:, C_MPREV:C_MPREV + P] = (kj[:, None] >= kj[None, :]).astype(np.float32)
    cm[:, C_MCUR:C_MCUR + P] = (kj[:, None] <= kj[None, :]).astype(np.float32)
    c["cmat"] = cm
    return c


def build_program(nseq=NSEQ, stages=None):
    pg = Prog(nseq)
    pg.run_stages(stages)
    return pg.finish()


def kernel(**inputs):
    x = np.asarray(inputs["x"], np.float32)
    hw = host_weights(inputs)
    hc = host_consts()
    nc = build_program(NSEQ)
    in_maps = []
    for c in range(NCORES):
        m = dict(hw)
        m.update(hc)
        m["xT"] = np.ascontiguousarray(x[c * NSEQ:(c + 1) * NSEQ].transpose(0, 2, 1))
        in_maps.append(m)
    res = run_bass_kernel_spmd(nc, in_maps, core_ids=list(range(NCORES)))
    outs = [np.asarray(r["out"], np.float32).transpose(0, 2, 1) for r in res.results]
    return np.ascontiguousarray(np.concatenate(outs, axis=0))
```

```python
import math
import numpy as np
import ml_dtypes
import concourse.bass as bass
import concourse.mybir as mybir
from concourse.bass_utils import run_bass_kernel_spmd

F32 = mybir.dt.float32
BF16 = mybir.dt.bfloat16
AF = mybir.ActivationFunctionType
ALU = mybir.AluOpType

P = 128
SEQ = 2048
DM = 1024
KC = 8
NCORES = 8
BATCH = 32
NSEQ = BATCH // NCORES
DFF = 2816
NJ = DFF // P
RMS_EPS = 1e-6
GN_EPS = 1e-6
RH = 4
DILS = (1, 4, 16)
SB_BASE = 16640
FORCE_INC = False
SB_TOP = 229344

V_MIXPRE = 0
V_MIXPOST = 32
V_FFNPRE = 64
V_FFNPOST = 96
V_KVN = 128
V_GN = 136
V_CW = 168
V_CB = V_CW + 4 * 3 * NJ
NV = V_CB + 4 * NJ
C_PI = 0
C_ID = 128
C_DT = 256
C_MPREV = 768
C_MCUR = 896
C_ZETA = 1024
C_EPS = 1028
NCM = 1032


class Buf:
    __slots__ = ("name", "lw", "rd")

    def __init__(self, name=""):
        self.name = name
        self.lw = None
        self.rd = {}


class Chan:
    def __init__(self, nc, name):
        self.sem = nc.alloc_semaphore(name=name)
        self.count = 0
        self.name = name


class Sched:
    def __init__(self, nc):
        self.nc = nc
        self.engs = {"pe": nc.tensor, "act": nc.scalar, "dve": nc.vector, "pool": nc.gpsimd, "sp": nc.sync}
        self.chan = {k: Chan(nc, "c_" + k) for k in self.engs}
        self.waited = {k: {} for k in self.engs}
        self.dchans = []
        self.ninst = 0

    def dchan(self, name):
        c = Chan(self.nc, name)
        self.dchans.append(c)
        return c

    def _wait(self, e, c, v):
        if v <= 0:
            return
        w = self.waited[e]
        if w.get(c, 0) >= v:
            return
        self.engs[e].wait_ge(c.sem, v)
        self.ninst += 1
        w[c] = v

    def _deps(self, e, reads, writes):
        need = {}
        for b in reads:
            if b.lw is not None:
                c, v = b.lw
                if need.get(c, 0) < v:
                    need[c] = v
        for b in writes:
            if b.lw is not None:
                c, v = b.lw
                if need.get(c, 0) < v:
                    need[c] = v
            for c, v in b.rd.items():
                if need.get(c, 0) < v:
                    need[c] = v
        own = self.chan.get(e)
        for c, v in need.items():
            if c is own and v > c.count:
                continue
            self._wait(e, c, v)

    def _record(self, c, v, reads, writes):
        for b in reads:
            if b.rd.get(c, 0) < v:
                b.rd[c] = v
        for b in writes:
            b.lw = (c, v)
            b.rd = {}

    def op(self, e, fn, reads=(), writes=(), inc=True):
        self._deps(e, reads, writes)
        ins = fn(self.engs[e])
        self.ninst += 1
        c = self.chan[e]
        if FORCE_INC:
            inc = True
        if inc:
            c.count += 1
            ins.then_inc(c.sem, 1)
            v = c.count
        else:
            v = c.count + 1
        self._record(c, v, reads, writes)
        return ins

    def dma(self, ch, out, in_, reads=(), writes=(), e="sp"):
        self._deps(e, reads, writes)
        ins = self.engs[e].dma_start(out=out, in_=in_)
        self.ninst += 1
        ch.count += 16
        ins.then_inc(ch.sem, 16)
        self._record(ch, ch.count, reads, writes)
        return ins

    def barrier(self):
        chans = list(self.chan.values()) + self.dchans
        for e in self.engs:
            for c in chans:
                if c is self.chan[e]:
                    continue
                self._wait(e, c, c.count)


class SbAlloc:
    def __init__(self, nc, base, top=SB_TOP):
        self.nc = nc
        self.off = base
        self.top = top
        self.n = 0

    def __call__(self, shape, dtype, name="t"):
        nb = 1
        for s in shape[1:]:
            nb *= s
        nb *= 2 if dtype == BF16 else 4
        nb = (nb + 63) // 64 * 64
        assert self.off + nb <= self.top, f"SBUF overflow {name} {self.off}+{nb}>{self.top}"
        self.n += 1
        t = self.nc.alloc_sbuf_tensor_at(f"{name}{self.n}", list(shape), dtype, offset=self.off)
        self.off += nb
        return t.ap()

    def fork(self):
        return SbAlloc(self.nc, self.off, self.top)


class Prog:
    def __init__(self, nseq=NSEQ):
        self.nseq = nseq
        nc = self.nc = bass.Bass("TRN2", target_bir_lowering=False)
        S = self.S = Sched(nc)
        dt = nc.dram_tensor

        def ext(name, shape, dtype=F32):
            return dt(name, list(shape), dtype, kind="ExternalInput").ap()

        def scr(name, shape, dtype=BF16):
            return dt(name, list(shape), dtype, kind="Internal").ap()

        self.xin = ext("xT", [nseq, DM, SEQ])
        self.out = dt("out", [nseq, DM, SEQ], F32, kind="ExternalOutput").ap()
        self.wshapes = {
            "w_ret_in": [2, P, 4, KC, 1536],
            "w_ret_out": [2, P, KC, 16, P],
            "w_k": [P, 24, KC, P],
            "w_v": [P, 6, KC, 512],
            "w_q": [2, P, 8, KC, 384],
            "w_o": [2, P, KC, KC, P],
            "w_up": [4, P, NJ, KC, 256],
            "w_down": [4, P, KC, NJ, P],
        }
        self.wf = {k: ext(k, v) for k, v in self.wshapes.items()}
        self.wb = {k: scr(k + "_b", v) for k, v in self.wshapes.items()}
        self.vecs_d = ext("vecs", [P, NV])
        self.cmat_d = ext("cmat", [P, NCM])
        self.rcs_d = ext("rcs", [P, 2, SEQ])
        self.acs_d = ext("acs", [P, 2, SEQ])
        self.kt_d = scr("kt_s", [3, 8, P, SEQ])
        self.va_d = scr("va_s", [3, P, 8, 16 * 192])
        self.Bx = [[Buf(f"x{s}_{t}") for t in range(4)] for s in range(nseq)]
        self.Bwb = {k: Buf(k) for k in self.wshapes}
        self.Bkt = Buf("kt")
        self.Bva = Buf("va")
        self.xsrc = [self.xin[s] for s in range(nseq)]
        self.xdst = [self.out[s] for s in range(nseq)]
        self.ld = S.dchan("ld")
        self.ldw = [S.dchan(f"ldw{i}") for i in range(4)]
        self.st = S.dchan("st")
        self.ps = [nc.alloc_psum_tensor(f"ps{i}", [P, 512], F32).ap() for i in range(8)]
        self.Bps = [Buf(f"ps{i}") for i in range(8)]
        A = self.A0 = SbAlloc(nc, SB_BASE)
        self.vecs = A([P, NV], F32, "vecs")
        self.cmat = A([P, NCM], F32, "cmat")
        self.ones_b = A([P, P], BF16, "ones")
        self.ident_b = A([P, P], BF16, "ident")
        self.pi_b = A([P, P], BF16, "pi")
        self.mask_b = A([P, 2, P], BF16, "mask")
        self.Bconst = Buf("const")
        S.dma(self.ld, self.vecs, self.vecs_d, writes=[self.Bconst])
        S.dma(self.ld, self.cmat, self.cmat_d, writes=[self.Bconst])
        S.op("pool", lambda e: e.memset(self.ones_b, 1.0), writes=[self.Bconst])
        S.op("dve", lambda e: e.tensor_copy(out=self.ident_b, in_=self.cmat[:, C_ID:C_ID + P]), reads=[self.Bconst], writes=[self.Bconst])
        S.op("dve", lambda e: e.tensor_copy(out=self.pi_b, in_=self.cmat[:, C_PI:C_PI + P]), reads=[self.Bconst], writes=[self.Bconst])
        S.op("dve", lambda e: e.tensor_copy(out=self.mask_b, in_=self.cmat[:, C_MPREV:C_MPREV + 2 * P].rearrange("p (a b) -> p a b", a=2)),
             reads=[self.Bconst], writes=[self.Bconst])
        S.barrier()

    def prep(self, names=None):
        S, nc = self.S, self.nc
        A = self.A0.fork()
        NB = 3
        CH = 4096
        fin = [A([P, CH], F32, "pin") for _ in range(NB)]
        fout = [A([P, CH], BF16, "pout") for _ in range(NB)]
        Bin = [Buf() for _ in range(NB)]
        Bout = [Buf() for _ in range(NB)]
        k = 0
        engs = ["dve", "act"]
        for name in (names or list(self.wshapes)):
            shp = self.wshapes[name]
            nl = shp[0] if shp[0] != P else 1
            for l in range(nl):
                src = self.wf[name][l] if shp[0] != P else self.wf[name]
                dst = self.wb[name][l] if shp[0] != P else self.wb[name]
                nd = len(src.shape)
                letters = "abcd"[: nd - 1]
                pat = "p " + " ".join(letters) + " -> p (" + " ".join(letters) + ")"
                src2 = src.rearrange(pat)
                dst2 = dst.rearrange(pat)
                n = src2.shape[1]
                for c0 in range(0, n, CH):
                    w = min(CH, n - c0)
                    i = k % NB
                    S.dma(self.ld, fin[i][:, :w], src2[:, c0:c0 + w], writes=[Bin[i]])
                    if name == "w_ret_out":
                        per = 16 * P
                        for q0 in range(0, w, P):
                            ec = ((c0 + q0) % per) // P
                            col = V_GN + l * 16 + ec
                            S.op("dve", lambda e, i=i, q0=q0, col=col: e.tensor_scalar(
                                out=fout[i][:, q0:q0 + P], in0=fin[i][:, q0:q0 + P], scalar1=self.vecs[:, col:col + 1],
                                scalar2=None, op0=ALU.mult), reads=[Bin[i], self.Bconst], writes=[Bout[i]])
                    else:
                        en = engs[k % 2]
                        if en == "act":
                            S.op("act", lambda e, i=i, w=w: e.copy(out=fout[i][:, :w], in_=fin[i][:, :w]), reads=[Bin[i]], writes=[Bout[i]])
                        else:
                            S.op(en, lambda e, i=i, w=w: e.tensor_copy(out=fout[i][:, :w], in_=fin[i][:, :w]), reads=[Bin[i]], writes=[Bout[i]])
                    S.dma(self.st, dst2[:, c0:c0 + w], fout[i][:, :w], reads=[Bout[i]], writes=[self.Bwb[name]])
                    k += 1
        S.barrier()

    def x_tile(self, ap_seq, tt):
        return ap_seq[:, tt * 512:(tt + 1) * 512].rearrange("(kc p) t -> p kc t", p=P)

    def rstd_from(self, src, sq, psb, rstd, Bsrc, Bsq, Brstd):
        S = self.S
        lvl = int(getattr(self, "dbglvl", 9))
        if lvl < 2:
            return
        S.op("act", lambda e: e.activation(out=sq, in_=src, func=AF.Square), reads=[Bsrc], writes=[Bsq])
        if lvl < 3:
            return
        for kc in range(KC):
            S.op("pe", lambda e, kc=kc: e.matmul(self.ps[psb], lhsT=self.ones_b, rhs=sq[:, kc, :], start=(kc == 0), stop=(kc == KC - 1)),
                 reads=[Bsq, self.Bconst], writes=[self.Bps[psb]], inc=(kc == KC - 1))
        if lvl < 4:
            return
        S.op("act", lambda e: e.activation(out=rstd, in_=self.ps[psb], func=AF.Sqrt, bias=RMS_EPS, scale=1.0 / DM),
             reads=[self.Bps[psb]], writes=[Brstd])
        if lvl < 5:
            return
        S.op("dve", lambda e: e.reciprocal(out=rstd, in_=rstd), reads=[Brstd], writes=[Brstd])

    def phase_hT(self, s, A, hT, BhT, gcol):
        S = self.S
        xt = [A([P, KC, 512], F32, "xt") for _ in range(2)]
        sq = A([P, KC, 512], BF16, "sq")
        rs = [A([P, 512], F32, "rs") for _ in range(2)]
        Bxt = [Buf() for _ in range(2)]
        Bsq = Buf()
        Brs = [Buf() for _ in range(2)]
        for tt in range(4):
            i = tt % 2
            S.dma(self.ld, xt[i], self.x_tile(self.xsrc[s], tt), reads=[self.Bx[s][tt]], writes=[Bxt[i]])
            self.rstd_from(xt[i], sq, tt % 2, rs[i], Bxt[i], Bsq, Brs[i])
            if int(getattr(self, "dbglvl", 9)) < 6:
                continue
            for kc in range(KC):
                S.op("dve", lambda e, kc=kc, i=i, tt=tt: e.scalar_tensor_tensor(
                    out=hT[:, kc, tt * 512:(tt + 1) * 512], in0=xt[i][:, kc, :], scalar=self.vecs[:, gcol + kc:gcol + kc + 1],
                    in1=rs[i], op0=ALU.mult, op1=ALU.mult), reads=[Bxt[i], Brs[i], self.Bconst], writes=[BhT[tt]])

    def post_norm_residual(self, s, tt, fT, BfT, xt, Bxt, sq, Bsq, rs, Brs, gcol, psb):
        S = self.S
        self.rstd_from(fT, sq, psb, rs, BfT, Bsq, Brs)
        for kc in range(KC):
            S.op("dve", lambda e, kc=kc: e.tensor_tensor(out=fT[:, kc, :], in0=fT[:, kc, :], in1=rs, op=ALU.mult),
                 reads=[BfT, Brs], writes=[BfT])
        for kc in range(KC):
            S.op("dve", lambda e, kc=kc: e.scalar_tensor_tensor(
                out=xt[:, kc, :], in0=fT[:, kc, :], scalar=self.vecs[:, gcol + kc:gcol + kc + 1], in1=xt[:, kc, :],
                op0=ALU.mult, op1=ALU.add), reads=[BfT, Bxt, self.Bconst], writes=[Bxt])
        S.dma(self.st, self.x_tile(self.xdst[s], tt), xt, reads=[Bxt], writes=[self.Bx[s][tt]])

    def out_proj_phase(self, s, A, wname, l, nk, actT, BactT, gcol):
        S = self.S
        xt = [A([P, KC, 512], F32, "xt") for _ in range(2)]
        fT = A([P, KC, 512], F32, "fT")
        sq = A([P, KC, 512], BF16, "sq")
        rs = A([P, 512], F32, "rs")
        NW = 3
        wd = [A([P, nk, P], BF16, "wd") for _ in range(NW)]
        Bxt = [Buf() for _ in range(2)]
        BfT, Bsq, Brs = Buf(), Buf(), Buf()
        Bwd = [Buf() for _ in range(NW)]
        wsrc = self.wb[wname][l]
        seq = [(tt, dc) for tt in range(4) for dc in range(KC)]

        def loadw(n):
            tt, dc = seq[n]
            S.dma(self.ldw[n % NW], wd[n % NW], wsrc[:, dc], reads=[self.Bwb[wname]], writes=[Bwd[n % NW]])

        loadw(0)
        loadw(1)
        S.dma(self.ld, xt[0], self.x_tile(self.xsrc[s], 0), reads=[self.Bx[s][0]], writes=[Bxt[0]])
        for n, (tt, dc) in enumerate(seq):
            if n + 2 < len(seq):
                loadw(n + 2)
            if dc == 0 and tt + 1 < 4:
                S.dma(self.ld, xt[(tt + 1) % 2], self.x_tile(self.xsrc[s], tt + 1), reads=[self.Bx[s][tt + 1]], writes=[Bxt[(tt + 1) % 2]])
            pb = 2 + (n % 2)
            w = wd[n % NW]
            for j in range(nk):
                S.op("pe", lambda e, j=j, w=w, pb=pb, tt=tt: e.matmul(self.ps[pb], lhsT=w[:, j, :], rhs=actT[:, j, tt * 512:(tt + 1) * 512],
                                                               start=(j == 0), stop=(j == nk - 1)),
                     reads=[Bwd[n % NW], BactT], writes=[self.Bps[pb]], inc=(j == nk - 1))
            S.op("act", lambda e, dc=dc, pb=pb: e.copy(out=fT[:, dc, :], in_=self.ps[pb]), reads=[self.Bps[pb]], writes=[BfT])
            if dc == KC - 1:
                self.post_norm_residual(s, tt, fT, BfT, xt[tt % 2], Bxt[tt % 2], sq, Bsq, rs, Brs, gcol, tt % 2)

    def ffn(self, l, s):
        S = self.S
        A = self.A0.fork()
        aT = A([P, NJ, SEQ], BF16, "aT")
        BaT = Buf()
        R = A.fork()
        hT = R([P, KC, SEQ], BF16, "hT")
        BhT = [Buf() for _ in range(4)]
        self.phase_hT(s, self.A0.fork(), hT, BhT, V_FFNPRE + 8 * l)
        S.barrier()
        if getattr(self, "dbg", "") == "A":
            return
        NW = 3
        wu = [R([P, KC, 256], BF16, "wu") for _ in range(NW)]
        Bwu = [Buf() for _ in range(NW)]
        gb = R([P, 2 + SEQ], F32, "gb")
        Bgb = [Buf() for _ in range(4)]
        ct = [R([P, 512], F32, "ct") for _ in range(2)]
        cg = [R([P, 512], F32, "cg") for _ in range(2)]
        Bct = [Buf() for _ in range(2)]
        Bcg = [Buf() for _ in range(2)]
        vb = [R([P, 512], BF16, "vb") for _ in range(2)]
        Bvb = [Buf() for _ in range(2)]
        S.op("pool", lambda e: e.memset(gb[:, 0:2], 0.0), writes=[Bgb[0]])
        wsrc = self.wb["w_up"][l]

        def loadw(j):
            S.dma(self.ldw[j % NW], wu[j % NW], wsrc[:, j], reads=[self.Bwb["w_up"]], writes=[Bwu[j % NW]])

        loadw(0)
        loadw(1)
        cwb = V_CW + l * 3 * NJ
        cbb = V_CB + l * NJ
        n = 0
        for j in range(NJ):
            if j + 2 < NJ:
                loadw(j + 2)
            w = wu[j % NW]
            for tt in range(4):
                pg, pv = 4 + 2 * (n % 2), 5 + 2 * (n % 2)
                i = n % 2
                cols = slice(tt * 512, (tt + 1) * 512)
                for half, pb in ((0, pg), (1, pv)):
                    for kc in range(KC):
                        S.op("pe", lambda e, kc=kc, pb=pb, half=half, w=w, cols=cols: e.matmul(
                            self.ps[pb], lhsT=w[:, kc, half * P:(half + 1) * P], rhs=hT[:, kc, cols], start=(kc == 0), stop=(kc == KC - 1)),
                            reads=[Bwu[j % NW], BhT[tt]], writes=[self.Bps[pb]], inc=(kc == KC - 1))
                S.op("act", lambda e, pg=pg, tt=tt: e.copy(out=gb[:, 2 + tt * 512:2 + (tt + 1) * 512], in_=self.ps[pg]),
                     reads=[self.Bps[pg]], writes=[Bgb[tt]])
                S.op("act", lambda e, pv=pv, i=i: e.copy(out=vb[i], in_=self.ps[pv]), reads=[self.Bps[pv]], writes=[Bvb[i]])
                rb = [Bgb[tt]] + ([Bgb[tt - 1]] if tt > 0 else [])
                S.op("dve", lambda e, tt=tt, i=i, j=j: e.tensor_scalar(
                    out=ct[i], in0=gb[:, 2 + tt * 512:2 + (tt + 1) * 512], scalar1=self.vecs[:, cwb + 2 * NJ + j:cwb + 2 * NJ + j + 1],
                    scalar2=self.vecs[:, cbb + j:cbb + j + 1], op0=ALU.mult, op1=ALU.add), reads=rb + [self.Bconst], writes=[Bct[i]])
                for tap in (1, 0):
                    sh = 2 - tap
                    S.op("dve", lambda e, tt=tt, i=i, j=j, tap=tap, sh=sh: e.scalar_tensor_tensor(
                        out=ct[i], in0=gb[:, 2 - sh + tt * 512:2 - sh + (tt + 1) * 512],
                        scalar=self.vecs[:, cwb + tap * NJ + j:cwb + tap * NJ + j + 1], in1=ct[i], op0=ALU.mult, op1=ALU.add),
                        reads=rb + [Bct[i], self.Bconst], writes=[Bct[i]])
                S.op("act", lambda e, i=i: e.activation(out=cg[i], in_=ct[i], func=AF.Gelu_apprx_tanh), reads=[Bct[i]], writes=[Bcg[i]])
                S.op("dve", lambda e, i=i, j=j, cols=cols: e.tensor_tensor(out=aT[:, j, cols], in0=cg[i], in1=vb[i], op=ALU.mult),
                     reads=[Bcg[i], Bvb[i]], writes=[BaT])
                n += 1
        S.barrier()
        if getattr(self, "dbg", "") == "B":
            return
        self.out_proj_phase(s, A.fork(), "w_down", l, NJ, aT, BaT, V_FFNPOST + 8 * l)
        S.barrier()


    def ret(self, l, s):
        S = self.S
        A = self.A0.fork()
        yT = A([P, 16, SEQ], BF16, "yT")
        ByT = Buf()
        R = A.fork()
        hT = R([P, KC, SEQ], BF16, "hT")
        BhT = [Buf() for _ in range(4)]
        self.phase_hT(s, self.A0.fork(), hT, BhT, V_MIXPRE + 8 * l)
        S.barrier()
        R2 = R.fork()
        cs = R([P, 2, SEQ], F32, "cs")
        Bcs = Buf()
        S.dma(self.ld, cs, self.rcs_d, writes=[Bcs])
        wsl = R([P, KC, 1536], BF16, "wsl")
        Bw = Buf()
        qk = [R([P, 4, 512], BF16, "qk") for _ in range(2)]
        vv = [R([P, 4, 512], BF16, "vv") for _ in range(2)]
        sg = [R([P, 4, 512], BF16, "sg") for _ in range(2)]
        Bqk = [[Buf(), Buf()] for _ in range(2)]
        Bvv = [[Buf() for _ in range(4)] for _ in range(2)]
        Bsg = [[Buf() for _ in range(4)] for _ in range(2)]
        tm = [R([P, 512], F32, "tm") for _ in range(4)]
        Btm = [Buf() for _ in range(4)]
        stf = R([P, 2, 512], F32, "stf")
        stb = R([P, 2, 512], BF16, "stb")
        Bstf, Bstb = Buf(), Buf()
        sT = [R([P, P], BF16, "sT") for _ in range(2)]
        kz = [R([P, 256], BF16, "kz") for _ in range(2)]
        yn = [R([P, 512], F32, "yn") for _ in range(2)]
        gt = [R([P, 512], BF16, "gt") for _ in range(2)]
        bst = [R([P, 6], F32, "bst") for _ in range(2)]
        mv = [R([P, 2], F32, "mv") for _ in range(2)]
        rg = [R([P, 2], F32, "rg") for _ in range(2)]
        BsT = [Buf() for _ in range(2)]
        Bkz = [Buf() for _ in range(2)]
        Byn = [Buf() for _ in range(2)]
        Bgt = [Buf() for _ in range(2)]
        Bsm = [Buf() for _ in range(2)]
        ps, Bps = self.ps, self.Bps
        psK = ps[3].bitcast(BF16)[:, 0:256]
        psT = ps[3].bitcast(BF16)[:, 512:1024]
        BpsK, BpsT = Buf(), Buf()
        proj_banks = [0, 1, 7]
        pbn = [0]
        wsrc = self.wb["w_ret_in"][l]
        units = [(h, st) for h in range(RH) for st in range(4)]

        def nextbank():
            b = proj_banks[pbn[0] % 3]
            pbn[0] += 1
            return b

        def load_w(h):
            S.dma(self.ldw[0], wsl, wsrc[:, h], reads=[self.Bwb["w_ret_in"]], writes=[Bw])

        def qk_pair(u, which):
            h, st = units[u]
            i = u % 2
            cols = slice(st * 512, (st + 1) * 512)
            banks = []
            for half in range(2):
                fi = which * 2 + half
                b = nextbank()
                banks.append(b)
                for kc in range(KC):
                    S.op("pe", lambda e, kc=kc, b=b, fi=fi: e.matmul(ps[b], lhsT=wsl[:, kc, fi * P:(fi + 1) * P], rhs=hT[:, kc, cols],
                                                                 start=(kc == 0), stop=(kc == KC - 1)),
                         reads=[Bw, BhT[st]], writes=[Bps[b]], inc=(kc == KC - 1))
            b0, b1 = banks
            cosv, sinv = cs[:, 0, cols], cs[:, 1, cols]
            o = which * 2
            S.op("dve", lambda e: e.tensor_tensor(out=tm[0], in0=ps[b0], in1=cosv, op=ALU.mult), reads=[Bps[b0], Bcs], writes=[Btm[0]])
            S.op("dve", lambda e: e.tensor_tensor(out=tm[1], in0=ps[b1], in1=sinv, op=ALU.mult), reads=[Bps[b1], Bcs], writes=[Btm[1]])
            S.op("dve", lambda e: e.tensor_tensor(out=tm[2], in0=ps[b1], in1=cosv, op=ALU.mult), reads=[Bps[b1], Bcs], writes=[Btm[2]])
            S.op("dve", lambda e: e.tensor_tensor(out=tm[3], in0=ps[b0], in1=sinv, op=ALU.mult), reads=[Bps[b0], Bcs], writes=[Btm[3]])
            S.op("dve", lambda e: e.tensor_tensor(out=qk[i][:, o, :], in0=tm[0], in1=tm[1], op=ALU.subtract),
                 reads=[Btm[0], Btm[1]], writes=[Bqk[i][which]])
            S.op("dve", lambda e: e.tensor_tensor(out=qk[i][:, o + 1, :], in0=tm[2], in1=tm[3], op=ALU.add),
                 reads=[Btm[2], Btm[3]], writes=[Bqk[i][which]])

        def vg_part(u, c):
            h, st = units[u]
            i = u % 2
            t0 = st * 512 + c * P
            for which in range(2):
                b = nextbank()
                for kc in range(KC):
                    S.op("pe", lambda e, kc=kc, b=b, which=which: e.matmul(
                        ps[b], lhsT=hT[:, kc, t0:t0 + P], rhs=wsl[:, kc, 512 + which * 512:1024 + which * 512],
                        start=(kc == 0), stop=(kc == KC - 1)), reads=[Bw, BhT[st]], writes=[Bps[b]], inc=(kc == KC - 1))
                if which == 0:
                    S.op("act", lambda e, b=b: e.copy(out=vv[i][:, c, :], in_=ps[b]), reads=[Bps[b]], writes=[Bvv[i][c]])
                else:
                    S.op("act", lambda e, b=b: e.activation(out=sg[i][:, c, :], in_=ps[b], func=AF.Silu), reads=[Bps[b]], writes=[Bsg[i][c]])

        def proj_parts(u):
            return [lambda: (qk_pair(u, 0), vg_part(u, 0)), lambda: (qk_pair(u, 1), vg_part(u, 1)),
                    lambda: vg_part(u, 2), lambda: vg_part(u, 3)]

        def front(u, c):
            h, st = units[u]
            i = u % 2
            k = c % 2
            cc = slice(c * P, (c + 1) * P)
            for j in range(2):
                S.op("pe", lambda e, j=j: e.matmul(ps[2][:, 0:P], lhsT=qk[i][:, 2 + j, cc], rhs=qk[i][:, j, cc], start=(j == 0), stop=(j == 1)),
                     reads=[Bqk[i][0], Bqk[i][1]], writes=[Bps[2]])
            for j in range(2):
                S.op("pe", lambda e, j=j: e.transpose(psK[:, j * P:(j + 1) * P], qk[i][:, 2 + j, cc], self.ident_b),
                     reads=[Bqk[i][1], self.Bconst], writes=[BpsK])
            S.op("dve", lambda e: e.tensor_tensor(out=sT[k], in0=ps[2][:, 0:P], in1=self.cmat[:, C_DT + h * P:C_DT + (h + 1) * P], op=ALU.mult),
                 reads=[Bps[2], self.Bconst], writes=[BsT[k]])
            S.op("act", lambda e: e.activation(out=kz[k], in_=psK, func=AF.Identity, scale=self.cmat[:, C_ZETA + h:C_ZETA + h + 1]),
                 reads=[BpsK, self.Bconst], writes=[Bkz[k]])

        def back(u, c):
            h, st = units[u]
            i = u % 2
            k = c % 2
            cg = st * 4 + c
            cc = slice(c * P, (c + 1) * P)
            lg = math.log(1.0 - 2.0 ** (-5.0 - h))
            gC = math.exp(128.0 * lg)
            S.op("pe", lambda e: e.matmul(ps[4], lhsT=sT[k], rhs=vv[i][:, c, :], start=True, stop=(cg == 0)),
                 reads=[BsT[k], Bvv[i][c]], writes=[Bps[4]])
            if cg > 0:
                for j in range(2):
                    S.op("pe", lambda e, j=j: e.matmul(ps[4], lhsT=qk[i][:, j, cc], rhs=stb[:, j, :], start=False, stop=(j == 1)),
                         reads=[Bqk[i][0], Bstb], writes=[Bps[4]])
            if cg < 15:
                for j in range(2):
                    S.op("pe", lambda e, j=j: e.matmul(ps[5 + j], lhsT=kz[k][:, j * P:(j + 1) * P], rhs=vv[i][:, c, :], start=True, stop=True),
                         reads=[Bkz[k], Bvv[i][c]], writes=[Bps[5 + j]])
                for j in range(2):
                    if cg == 0:
                        S.op("dve", lambda e, j=j: e.tensor_copy(out=stf[:, j, :], in_=ps[5 + j]), reads=[Bps[5 + j]], writes=[Bstf])
                    else:
                        S.op("dve", lambda e, j=j: e.scalar_tensor_tensor(out=stf[:, j, :], in0=stf[:, j, :], scalar=gC, in1=ps[5 + j],
                                                                         op0=ALU.mult, op1=ALU.add), reads=[Bps[5 + j], Bstf], writes=[Bstf])
                S.op("act", lambda e: e.copy(out=stb, in_=stf), reads=[Bstf], writes=[Bstb])
            S.op("dve", lambda e: e.bn_stats(out=bst[k], in_=ps[4]), reads=[Bps[4]], writes=[Bsm[k]])
            S.op("dve", lambda e: e.bn_aggr(out=mv[k], in_=bst[k]), reads=[Bsm[k]], writes=[Bsm[k]])
            S.op("act", lambda e: e.activation(out=rg[k][:, 0:1], in_=mv[k][:, 1:2], func=AF.Sqrt, bias=self.cmat[:, C_EPS + h:C_EPS + h + 1], scale=1.0),
                 reads=[Bsm[k], self.Bconst], writes=[Bsm[k]])
            S.op("dve", lambda e: e.reciprocal(out=rg[k][:, 0:1], in_=rg[k][:, 0:1]), reads=[Bsm[k]], writes=[Bsm[k]])
            S.op("dve", lambda e: e.scalar_tensor_tensor(out=rg[k][:, 1:2], in0=mv[k][:, 0:1], scalar=-1.0, in1=rg[k][:, 0:1], op0=ALU.mult, op1=ALU.mult),
                 reads=[Bsm[k]], writes=[Bsm[k]])
            S.op("act", lambda e: e.activation(out=yn[k], in_=ps[4], func=AF.Identity, bias=rg[k][:, 1:2], scale=rg[k][:, 0:1]),
                 reads=[Bps[4], Bsm[k]], writes=[Byn[k]])
            S.op("dve", lambda e: e.tensor_tensor(out=gt[k], in0=yn[k], in1=sg[i][:, c, :], op=ALU.mult),
                 reads=[Byn[k], Bsg[i][c]], writes=[Bgt[k]])

        def ytr(u, c):
            h, st = units[u]
            k = c % 2
            cg = st * 4 + c
            for jj in range(4):
                S.op("pe", lambda e, jj=jj: e.transpose(psT[:, jj * P:(jj + 1) * P], gt[k][:, jj * P:(jj + 1) * P], self.ident_b),
                     reads=[Bgt[k], self.Bconst], writes=[BpsT])
            S.op("act", lambda e: e.copy(out=yT[:, 4 * h:4 * h + 4, cg * P:(cg + 1) * P], in_=psT.rearrange("p (a b) -> p a b", a=4)),
                 reads=[BpsT], writes=[ByT])

        load_w(0)
        for f in proj_parts(0):
            f()
        pending = None
        for u in range(len(units)):
            nxt = [None] * 4
            if u + 1 < len(units):
                if units[u + 1][0] != units[u][0]:
                    load_w(units[u + 1][0])
                nxt = proj_parts(u + 1)
            for c in range(4):
                front(u, c)
                if nxt[c] is not None:
                    nxt[c]()
                back(u, c)
                if pending is not None:
                    ytr(*pending)
                pending = (u, c)
        ytr(*pending)
        S.barrier()
        self.out_proj_phase(s, R2, "w_ret_out", l, 16, yT, ByT, V_MIXPOST + 8 * l)
        S.barrier()


    @staticmethod
    def gcols(ap2d, g, u):
        if g == 0:
            return ap2d[:, u * 512:(u + 1) * 512]
        if g == 1:
            return ap2d[:, u::4]
        return ap2d.rearrange("p (i r) -> p r i", r=16)[:, 4 * u:4 * u + 4, :]

    @staticmethod
    def gview(ap512, g):
        return ap512 if g < 2 else ap512.rearrange("p (r i) -> p r i", r=4)

    @staticmethod
    def chunk_tokens(ap2d, g, c):
        if g == 0:
            return ap2d[..., c * P:(c + 1) * P] if False else ap2d[:, c * P:(c + 1) * P]
        if g == 1:
            st = 512 * (c % 4) + c // 4
            return ap2d[:, st:st + 509:4]
        return ap2d[:, c::16]

    def rot_proj(self, g, u, w_lhsT, hT, BhT, Bw, cs, Bcs, qraw, Bqraw, t1, t2, Bt, out512, Bout, pb, pb2):
        S, ps, Bps = self.S, self.ps, self.Bps
        for kc in range(KC):
            S.op("pe", lambda e, kc=kc: e.matmul(self.gview(ps[pb], g), lhsT=w_lhsT[:, kc, :], rhs=self.gcols(hT[:, kc, :], g, u),
                                               start=(kc == 0), stop=(kc == KC - 1)), reads=[Bw] + BhT, writes=[Bps[pb]], inc=(kc == KC - 1))
        S.op("act", lambda e: e.copy(out=qraw, in_=ps[pb]), reads=[Bps[pb]], writes=[Bqraw])
        S.op("pe", lambda e: e.matmul(ps[pb2], lhsT=self.pi_b, rhs=qraw, start=True, stop=True), reads=[Bqraw, self.Bconst], writes=[Bps[pb2]])
        S.op("dve", lambda e: e.tensor_tensor(out=self.gview(t1, g), in0=self.gview(ps[pb2], g), in1=self.gcols(cs[:, 1, :], g, u), op=ALU.mult),
             reads=[Bps[pb2], Bcs], writes=[Bt[0]])
        S.op("dve", lambda e: e.tensor_tensor(out=self.gview(t2, g), in0=self.gview(ps[pb], g), in1=self.gcols(cs[:, 0, :], g, u), op=ALU.mult),
             reads=[Bps[pb], Bcs], writes=[Bt[1]])
        if isinstance(out512, tuple):
            for rows, o in out512:
                S.op("dve", lambda e, rows=rows, o=o: e.tensor_tensor(out=o, in0=t1[rows, :], in1=t2[rows, :], op=ALU.add),
                     reads=[Bt[0], Bt[1]], writes=[Bout])
        else:
            S.op("dve", lambda e: e.tensor_tensor(out=out512, in0=t1, in1=t2, op=ALU.add), reads=[Bt[0], Bt[1]], writes=[Bout])

    def kv(self, s):
        S = self.S
        A = self.A0.fork()
        hT = A([P, KC, SEQ], BF16, "hT")
        BhT = [Buf() for _ in range(4)]
        self.phase_hT(s, A.fork(), hT, BhT, V_KVN)
        S.barrier()
        R = A.fork()
        cs = R([P, 2, SEQ], F32, "cs")
        Bcs = Buf()
        S.dma(self.ld, cs, self.acs_d, writes=[Bcs])
        wk = [R([P, KC, P], BF16, "wk") for _ in range(3)]
        Bwk = [Buf() for _ in range(3)]
        qraw = [R([P, 512], BF16, "qraw") for _ in range(2)]
        t1 = [R([P, 512], F32, "t1") for _ in range(2)]
        t2 = [R([P, 512], F32, "t2") for _ in range(2)]
        Bqraw = [Buf() for _ in range(2)]
        Bt = [[Buf(), Buf()] for _ in range(2)]
        kst = [R([P, SEQ], BF16, "kst") for _ in range(2)]
        Bkst = [Buf() for _ in range(2)]
        wv = R([P, 2, KC, 512], BF16, "wv")
        Bwv = Buf()
        stg = R([P, 8, 16, 192], BF16, "stg")
        Bstg = Buf()
        S.op("dve", lambda e: e.memset(stg.rearrange("p a c x -> p (a c x)"), 1.0), writes=[Bstg])
        wsrc = self.wb["w_k"]

        def loadk(gp):
            S.dma(self.ldw[gp % 3], wk[gp % 3], wsrc[:, gp], reads=[self.Bwb["w_k"]], writes=[Bwk[gp % 3]])

        loadk(0)
        loadk(1)
        n = 0
        for gp in range(24):
            g, hp = gp // 8, gp % 8
            if gp + 2 < 24:
                loadk(gp + 2)
            ks = kst[gp % 2]
            for u in range(4):
                i = n % 2
                self.rot_proj(g, u, wk[gp % 3], hT, BhT, Bwk[gp % 3], cs, Bcs, qraw[i], Bqraw[i], t1[i], t2[i], Bt[i],
                              ks[:, u * 512:(u + 1) * 512], Bkst[gp % 2], 0 + i, 2 + i)
                n += 1
            S.dma(self.st, self.kt_d[g, hp], ks, reads=[Bkst[gp % 2]], writes=[self.Bkt])
        ps, Bps = self.ps, self.Bps
        n = 0
        for g in range(3):
            S.dma(self.ldw[3], wv, self.wb["w_v"][:, 2 * g:2 * g + 2], reads=[self.Bwb["w_v"]], writes=[Bwv])
            for c in range(16):
                for half in range(2):
                    pb = 4 + (n % 4)
                    n += 1
                    for kc in range(KC):
                        S.op("pe", lambda e, kc=kc, pb=pb, half=half: e.matmul(ps[pb], lhsT=self.chunk_tokens(hT[:, kc, :], g, c), rhs=wv[:, half, kc, :],
                                                                         start=(kc == 0), stop=(kc == KC - 1)), reads=[Bwv] + BhT, writes=[Bps[pb]], inc=(kc == KC - 1))
                    pv = ps[pb].rearrange("p (a h d) -> p a h d", a=4, h=2)
                    for hh in range(2):
                        o = stg[:, half * 4:half * 4 + 4, c, hh * 128:hh * 128 + 64]
                        if hh == 0:
                            S.op("act", lambda e, o=o, pv=pv: e.copy(out=o, in_=pv[:, :, 0, :]), reads=[Bps[pb]], writes=[Bstg])
                        else:
                            S.op("dve", lambda e, o=o, pv=pv: e.tensor_copy(out=o, in_=pv[:, :, 1, :]), reads=[Bps[pb]], writes=[Bstg])
            S.dma(self.st, self.va_d[g], stg.rearrange("p a c x -> p a (c x)"), reads=[Bstg], writes=[self.Bva])
        S.barrier()

    def att(self, l, s):
        S = self.S
        li = l - 2
        A = self.A0.fork()
        oT = A([P, 8, SEQ], BF16, "oT")
        BoT = Buf()
        R = A.fork()
        R0 = R.fork()
        hT = R([P, KC, SEQ], BF16, "hT")
        BhT = [Buf() for _ in range(4)]
        self.phase_hT(s, R.fork(), hT, BhT, V_MIXPRE + 8 * l)
        S.barrier()
        cs = R([P, 2, SEQ], F32, "cs")
        Bcs = Buf()
        S.dma(self.ld, cs, self.acs_d, writes=[Bcs])
        wq = [R([P, KC, 384], BF16, "wq") for _ in range(2)]
        Bwq = [Buf() for _ in range(2)]
        kt = R([P, 3, SEQ], BF16, "kt")
        vt = R([P, 3, 16 * 192], BF16, "vt")
        Bkt, Bvt = Buf(), Buf()
        qT = R([P, 3, 2, SEQ], BF16, "qT")
        BqT = [Buf() for _ in range(3)]
        S.op("dve", lambda e: e.memset(qT.rearrange("p g h t -> p (g h t)"), 0.0), writes=BqT)
        acc = R([P, 2, SEQ], F32, "acc")
        Bacc = Buf()
        qraw = [R([P, 512], BF16, "qraw") for _ in range(2)]
        t1 = [R([P, 512], F32, "t1") for _ in range(2)]
        t2 = [R([P, 512], F32, "t2") for _ in range(2)]
        Bqraw = [Buf() for _ in range(2)]
        Bt = [[Buf(), Buf()] for _ in range(2)]
        PT = [R([P, 2, 2, P], BF16, "PT") for _ in range(2)]
        BpP = [Buf() for _ in range(2)]
        BPT = [Buf() for _ in range(2)]
        tden = R([P, SEQ], F32, "tden")
        Btden = Buf()
        ps, Bps = self.ps, self.Bps
        psTb = ps[6].bitcast(BF16)
        psT = [psTb[:, k * 512:(k + 1) * 512].rearrange("p (h c q) -> p h c q", h=2, c=2) for k in range(2)]
        psU = [ps[7][:, k * 256:(k + 1) * 256].rearrange("p (h q) -> p h q", h=2) for k in range(2)]
        BpsT = [Buf() for _ in range(2)]
        BpsU = [Buf() for _ in range(2)]
        wsrc = self.wb["w_q"][li]

        def loadq(hp):
            S.dma(self.ldw[hp % 2], wq[hp % 2], wsrc[:, hp], reads=[self.Bwb["w_q"]], writes=[Bwq[hp % 2]])

        loadq(0)
        n = 0
        m = 0
        for hp in range(8):
            if hp + 1 < 8:
                loadq(hp + 1)
            S.dma(self.ldw[2], kt, self.kt_d[:, hp].rearrange("g p t -> p g t"), reads=[self.Bkt], writes=[Bkt])
            S.dma(self.ldw[3], vt, self.va_d[:, :, hp].rearrange("g p x -> p g x"), reads=[self.Bva], writes=[Bvt])
            w = wq[hp % 2]
            for g in range(3):
                for u in range(4):
                    i = n % 2
                    self.rot_proj(g, u, w[:, :, g * P:(g + 1) * P], hT, BhT, Bwq[hp % 2], cs, Bcs, qraw[i], Bqraw[i], t1[i], t2[i], Bt[i],
                                  ((slice(0, 64), qT[0:64, g, 0, u * 512:(u + 1) * 512]), (slice(64, 128), qT[64:128, g, 1, u * 512:(u + 1) * 512])),
                                  BqT[g], 0 + i, 2 + i)
                    n += 1
            for g in range(3):
                for c in range(16):
                    has_prev = (c > 0) if g == 0 else ((c % 4) > 0 if g == 1 else False)
                    nkc = 2 if has_prev else 1
                    nk = nkc * P
                    k0 = (c - 1) * P if has_prev else c * P
                    k = m % 2
                    m += 1
                    pS = 4 + k
                    pSv = ps[pS].rearrange("p (c h q) -> p c h q", c=2, h=2)
                    for kc in range(nkc):
                        S.op("pe", lambda e, kc=kc: e.matmul(pSv[:, kc], lhsT=kt[:, g, k0 + kc * P:k0 + (kc + 1) * P], rhs=qT[:, g, :, c * P:(c + 1) * P],
                                                          start=True, stop=True), reads=[BqT[g], Bkt], writes=[Bps[pS]])
                    S.op("act", lambda e: e.activation(out=PT[k][:, 0:nkc], in_=pSv[:, 0:nkc], func=AF.Exp, scale=0.125),
                         reads=[Bps[pS]], writes=[BPT[k]])
                    msk = self.mask_b[:, 2 - nkc:2, :].unsqueeze(2).broadcast_to([P, nkc, 2, P])
                    S.op("dve", lambda e, msk=msk: e.tensor_tensor(out=PT[k][:, 0:nkc], in0=PT[k][:, 0:nkc], in1=msk, op=ALU.mult),
                         reads=[BPT[k], self.Bconst], writes=[BPT[k]])
                    for hh in range(2):
                        for kc in range(nkc):
                            ch = (c - 1 + kc) if has_prev else c
                            S.op("pe", lambda e, hh=hh, kc=kc, ch=ch: e.matmul(psU[k][:, hh, :], lhsT=vt[:, g, ch * 192 + hh * 64:ch * 192 + hh * 64 + P],
                                                                            rhs=PT[k][:, kc, hh, :], start=(kc == 0), stop=(kc == nkc - 1)),
                                 reads=[Bvt, BPT[k]], writes=[BpsU[k]], inc=(kc == nkc - 1))
                    if g == 0:
                        S.op("act", lambda e: e.copy(out=acc[:, :, c * P:(c + 1) * P], in_=psU[k]), reads=[BpsU[k]], writes=[Bacc])
                    else:
                        if g == 1:
                            st = 512 * (c % 4) + c // 4
                            dst = acc[:, :, st:st + 509:4]
                        else:
                            dst = acc[:, :, c::16]
                        S.op("dve", lambda e, dst=dst: e.tensor_tensor(out=dst, in0=dst, in1=psU[k], op=ALU.add), reads=[BpsU[k], Bacc], writes=[Bacc])
            S.op("act", lambda e: e.copy(out=tden[0:64, :], in_=acc[64:128, 0, :]), reads=[Bacc], writes=[Btden])
            S.op("act", lambda e: e.copy(out=tden[64:128, :], in_=acc[0:64, 1, :]), reads=[Bacc], writes=[Btden])
            S.op("dve", lambda e: e.reciprocal(out=tden, in_=tden), reads=[Btden], writes=[Btden])
            S.op("dve", lambda e, hp=hp: e.tensor_tensor(out=oT[0:64, hp, :], in0=acc[0:64, 0, :], in1=tden[0:64, :], op=ALU.mult),
                 reads=[Bacc, Btden], writes=[BoT])
            S.op("dve", lambda e, hp=hp: e.tensor_tensor(out=oT[64:128, hp, :], in0=acc[64:128, 1, :], in1=tden[64:128, :], op=ALU.mult),
                 reads=[Bacc, Btden], writes=[BoT])
        S.barrier()
        self.out_proj_phase(s, R0, "w_o", li, 8, oT, BoT, V_MIXPOST + 8 * l)
        S.barrier()

    def run_stages(self, stages=None):
        if stages is None:
            stages = [("prep", None)]
            for s in range(self.nseq):
                for l in range(4):
                    stages.append(("ret" if l < 2 else "att", l, s))
                    stages.append(("ffn", l, s))
                    if l == 1:
                        stages.append(("kv", s))
        for st in stages:
            kind = st[0]
            if kind == "prep":
                self.prep(st[1])
            elif kind == "ffn":
                self.ffn(st[1], st[2])
                self.xsrc[st[2]] = self.xdst[st[2]]
            elif kind == "ret":
                self.ret(st[1], st[2])
                self.xsrc[st[2]] = self.xdst[st[2]]
            elif kind == "att":
                self.att(st[1], st[2])
                self.xsrc[st[2]] = self.xdst[st[2]]
            elif kind == "kv":
                self.kv(st[1])

    def finish(self):
        S = self.S
        S.barrier()
        return self.nc


def _kxm(w):
    K, F = w.shape
    return np.ascontiguousarray(w.reshape(K // P, P, F).transpose(1, 0, 2))


def host_weights(inp):
    out = {}
    wi = np.asarray(inp["ret_w_in"], np.float32)
    cols = []
    for h in range(RH):
        cols += list(range(h * 256, (h + 1) * 256))
        cols += list(range(1024 + h * 256, 1024 + (h + 1) * 256))
        cols += list(range(2048 + h * 512, 2048 + (h + 1) * 512))
        cols += list(range(4096 + h * 512, 4096 + (h + 1) * 512))
    cols = np.array(cols)
    out["w_ret_in"] = np.stack([_kxm(wi[l][:, cols]).reshape(P, KC, 4, 1536).transpose(0, 2, 1, 3) for l in range(2)])
    wo = np.asarray(inp["ret_w_out"], np.float32)
    out["w_ret_out"] = np.stack([_kxm(wo[l]).reshape(P, 16, KC, P).transpose(0, 2, 1, 3) for l in range(2)])
    wkv = np.asarray(inp["att_w_kv"], np.float32)
    out["w_k"] = _kxm(wkv[:, :3072]).reshape(P, KC, 24, P).transpose(0, 2, 1, 3)
    out["w_v"] = _kxm(wkv[:, 3072:]).reshape(P, KC, 6, 512).transpose(0, 2, 1, 3)
    wq = np.asarray(inp["att_w_q"], np.float32)
    out["w_q"] = np.stack([_kxm(wq[l]).reshape(P, KC, 3, 8, P).transpose(0, 3, 1, 2, 4).reshape(P, 8, KC, 384) for l in range(2)])
    wao = np.asarray(inp["att_w_o"], np.float32)
    out["w_o"] = np.stack([_kxm(wao[l]).reshape(P, KC, KC, P).transpose(0, 2, 1, 3) for l in range(2)])
    wu = np.asarray(inp["ffn_w_up"], np.float32)
    out["w_up"] = np.stack([np.concatenate([_kxm(wu[l][:, :DFF]).reshape(P, KC, NJ, P), _kxm(wu[l][:, DFF:]).reshape(P, KC, NJ, P)], axis=3)
                            .transpose(0, 2, 1, 3) for l in range(4)])
    wd = np.asarray(inp["ffn_w_down"], np.float32)
    out["w_down"] = np.stack([_kxm(wd[l]).reshape(P, NJ, KC, P).transpose(0, 2, 1, 3) for l in range(4)])
    out = {k: np.ascontiguousarray(v, dtype=np.float32) for k, v in out.items()}

    def pv(v):
        v = np.asarray(v, np.float32)
        return v.reshape(-1, P).T

    vecs = np.zeros((P, NV), np.float32)
    for l in range(4):
        vecs[:, V_MIXPRE + 8 * l:V_MIXPRE + 8 * l + 8] = pv(inp["norm_mix_pre"][l])
        vecs[:, V_MIXPOST + 8 * l:V_MIXPOST + 8 * l + 8] = pv(inp["norm_mix_post"][l])
        vecs[:, V_FFNPRE + 8 * l:V_FFNPRE + 8 * l + 8] = pv(inp["norm_ffn_pre"][l])
        vecs[:, V_FFNPOST + 8 * l:V_FFNPOST + 8 * l + 8] = pv(inp["norm_ffn_post"][l])
        for tap in range(3):
            vecs[:, V_CW + (l * 3 + tap) * NJ:V_CW + (l * 3 + tap + 1) * NJ] = pv(inp["ffn_conv_w"][l][tap])
        vecs[:, V_CB + l * NJ:V_CB + (l + 1) * NJ] = pv(inp["ffn_conv_b"][l])
    vecs[:, V_KVN:V_KVN + 8] = pv(inp["kv_norm"])
    for l in range(2):
        vecs[:, V_GN + 16 * l:V_GN + 16 * l + 16] = pv(inp["ret_gn_gain"][l])
    out["vecs"] = vecs
    return out


def host_consts():
    c = {}
    pos = np.arange(SEQ, dtype=np.float32)
    inv = (1.0 / (np.float32(10000.0) ** np.linspace(0.0, 1.0, 128, dtype=np.float32))).astype(np.float32)
    ang = (pos[None, :] * inv[:, None]).astype(np.float32)
    c["rcs"] = np.stack([np.cos(ang), np.sin(ang)], axis=1).astype(np.float32)
    invf = (np.float32(500000.0) ** (-np.arange(0, 16, 2, dtype=np.float32) / np.float32(16))).astype(np.float32)
    acs = np.zeros((P, 2, SEQ), np.float32)
    pi = np.zeros((P, P), np.float32)
    for p in range(P):
        hd = p % 64
        if hd < 8:
            a = (pos * invf[hd]).astype(np.float32)
            acs[p, 0] = np.cos(a)
            acs[p, 1] = -np.sin(a)
            pi[p + 8, p] = 1.0
        elif hd < 16:
            a = (pos * invf[hd - 8]).astype(np.float32)
            acs[p, 0] = np.cos(a)
            acs[p, 1] = np.sin(a)
            pi[p - 8, p] = 1.0
        else:
            acs[p, 0] = 1.0
    c["acs"] = acs
    cm = np.zeros((P, NCM), np.float32)
    cm[:, C_PI:C_PI + P] = pi
    cm[:, C_ID:C_ID + P] = np.eye(P, dtype=np.float32)
    n = np.arange(P, dtype=np.float64)
    for h in range(RH):
        lg = math.log(1.0 - 2.0 ** (-5.0 - h))
        dtm = np.where(n[None, :] >= n[:, None], np.exp(-(n[:, None] + 1.0) * lg) / 16.0, 0.0)
        cm[:, C_DT + h * P:C_DT + (h + 1) * P] = dtm
        cm[:, C_ZETA + h] = np.exp((127.0 - n) * lg) / 16.0
        cm[:, C_EPS + h] = GN_EPS * np.exp(-2.0 * (n + 1.0) * lg)
    kj = np.arange(P)
    cm[:, C_MPREV:C_MPREV + P] = (kj[:, None] >= kj[None, :]).astype(np.float32)
    cm[:, C_MCUR:C_MCUR + P] = (kj[:, None] <= kj[None, :]).astype(np.float32)
    c["cmat"] = cm
    return c


def build_program(nseq=NSEQ, stages=None):
    pg = Prog(nseq)
    pg.run_stages(stages)
    return pg.finish()


def kernel(**inputs):
    x = np.asarray(inputs["x"], np.float32)
    hw = host_weights(inputs)
    hc = host_consts()
    nc = build_program(NSEQ)
    in_maps = []
    for c in range(NCORES):
        m = dict(hw)
        m.update(hc)
        m["xT"] = np.ascontiguousarray(x[c * NSEQ:(c + 1) * NSEQ].transpose(0, 2, 1))
        in_maps.append(m)
    res = run_bass_kernel_spmd(nc, in_maps, core_ids=list(range(NCORES)))
    outs = [np.asarray(r["out"], np.float32).transpose(0, 2, 1) for r in res.results]
    return np.ascontiguousarray(np.concatenate(outs, axis=0))
```

```python
import math
import numpy as np
import ml_dtypes
import concourse.bass as bass
import concourse.mybir as mybir
from concourse.bass_utils import run_bass_kernel_spmd

F32 = mybir.dt.float32
BF16 = mybir.dt.bfloat16
AF = mybir.ActivationFunctionType
ALU = mybir.AluOpType

P = 128
SEQ = 2048
DM = 1024
KC = 8
NCORES = 8
BATCH = 32
NSEQ = BATCH // NCORES
DFF = 2816
NJ = DFF // P
RMS_EPS = 1e-6
GN_EPS = 1e-6
RH = 4
DILS = (1, 4, 16)
SB_BASE = 16640
FORCE_INC = False
SB_TOP = 229344

V_MIXPRE = 0
V_MIXPOST = 32
V_FFNPRE = 64
V_FFNPOST = 96
V_KVN = 128
V_GN = 136
V_CW = 168
V_CB = V_CW + 4 * 3 * NJ
NV = V_CB + 4 * NJ
C_PI = 0
C_ID = 128
C_DT = 256
C_MPREV = 768
C_MCUR = 896
C_ZETA = 1024
C_EPS = 1028
NCM = 1032


class Buf:
    __slots__ = ("name", "lw", "rd")

    def __init__(self, name=""):
        self.name = name
        self.lw = None
        self.rd = {}


class Chan:
    def __init__(self, nc, name):
        self.sem = nc.alloc_semaphore(name=name)
        self.count = 0
        self.name = name


class Sched:
    def __init__(self, nc):
        self.nc = nc
        self.engs = {"pe": nc.tensor, "act": nc.scalar, "dve": nc.vector, "pool": nc.gpsimd, "sp": nc.sync}
        self.chan = {k: Chan(nc, "c_" + k) for k in self.engs}
        self.waited = {k: {} for k in self.engs}
        self.dchans = []
        self.ninst = 0

    def dchan(self, name):
        c = Chan(self.nc, name)
        self.dchans.append(c)
        return c

    def _wait(self, e, c, v):
        if v <= 0:
            return
        w = self.waited[e]
        if w.get(c, 0) >= v:
            return
        self.engs[e].wait_ge(c.sem, v)
        self.ninst += 1
        w[c] = v

    def _deps(self, e, reads, writes):
        need = {}
        for b in reads:
            if b.lw is not None:
                c, v = b.lw
                if need.get(c, 0) < v:
                    need[c] = v
        for b in writes:
            if b.lw is not None:
                c, v = b.lw
                if need.get(c, 0) < v:
                    need[c] = v
            for c, v in b.rd.items():
                if need.get(c, 0) < v:
                    need[c] = v
        own = self.chan.get(e)
        for c, v in need.items():
            if c is own and v > c.count:
                continue
            self._wait(e, c, v)

    def _record(self, c, v, reads, writes):
        for b in reads:
            if b.rd.get(c, 0) < v:
                b.rd[c] = v
        for b in writes:
            b.lw = (c, v)
            b.rd = {}

    def op(self, e, fn, reads=(), writes=(), inc=True):
        self._deps(e, reads, writes)
        ins = fn(self.engs[e])
        self.ninst += 1
        c = self.chan[e]
        if FORCE_INC:
            inc = True
        if inc:
            c.count += 1
            ins.then_inc(c.sem, 1)
            v = c.count
        else:
            v = c.count + 1
        self._record(c, v, reads, writes)
        return ins

    def dma(self, ch, out, in_, reads=(), writes=(), e="sp"):
        self._deps(e, reads, writes)
        ins = self.engs[e].dma_start(out=out, in_=in_)
        self.ninst += 1
        ch.count += 16
        ins.then_inc(ch.sem, 16)
        self._record(ch, ch.count, reads, writes)
        return ins

    def barrier(self):
        chans = list(self.chan.values()) + self.dchans
        for e in self.engs:
            for c in chans:
                if c is self.chan[e]:
                    continue
                self._wait(e, c, c.count)


class SbAlloc:
    def __init__(self, nc, base, top=SB_TOP):
        self.nc = nc
        self.off = base
        self.top = top
        self.n = 0

    def __call__(self, shape, dtype, name="t"):
        nb = 1
        for s in shape[1:]:
            nb *= s
        nb *= 2 if dtype == BF16 else 4
        nb = (nb + 63) // 64 * 64
        assert self.off + nb <= self.top, f"SBUF overflow {name} {self.off}+{nb}>{self.top}"
        self.n += 1
        t = self.nc.alloc_sbuf_tensor_at(f"{name}{self.n}", list(shape), dtype, offset=self.off)
        self.off += nb
        return t.ap()

    def fork(self):
        return SbAlloc(self.nc, self.off, self.top)


class Prog:
    def __init__(self, nseq=NSEQ):
        self.nseq = nseq
        nc = self.nc = bass.Bass("TRN2", target_bir_lowering=False)
        S = self.S = Sched(nc)
        dt = nc.dram_tensor

        def ext(name, shape, dtype=F32):
            return dt(name, list(shape), dtype, kind="ExternalInput").ap()

        def scr(name, shape, dtype=BF16):
            return dt(name, list(shape), dtype, kind="Internal").ap()

        self.xin = ext("xT", [nseq, DM, SEQ])
        self.out = dt("out", [nseq, DM, SEQ], F32, kind="ExternalOutput").ap()
        self.wshapes = {
            "w_ret_in": [2, P, 4, KC, 1536],
            "w_ret_out": [2, P, KC, 16, P],
            "w_k": [P, 24, KC, P],
            "w_v": [P, 6, KC, 512],
            "w_q": [2, P, 8, KC, 384],
            "w_o": [2, P, KC, KC, P],
            "w_up": [4, P, NJ, KC, 256],
            "w_down": [4, P, KC, NJ, P],
        }
        self.wf = {k: ext(k, v) for k, v in self.wshapes.items()}
        self.wb = {k: scr(k + "_b", v) for k, v in self.wshapes.items()}
        self.vecs_d = ext("vecs", [P, NV])
        self.cmat_d = ext("cmat", [P, NCM])
        self.rcs_d = ext("rcs", [P, 2, SEQ])
        self.acs_d = ext("acs", [P, 2, SEQ])
        self.kt_d = scr("kt_s", [3, 8, P, SEQ])
        self.va_d = scr("va_s", [3, P, 8, 16 * 192])
        self.Bx = [[Buf(f"x{s}_{t}") for t in range(4)] for s in range(nseq)]
        self.Bwb = {k: Buf(k) for k in self.wshapes}
        self.Bkt = Buf("kt")
        self.Bva = Buf("va")
        self.xsrc = [self.xin[s] for s in range(nseq)]
        self.xdst = [self.out[s] for s in range(nseq)]
        self.ld = S.dchan("ld")
        self.ldw = [S.dchan(f"ldw{i}") for i in range(4)]
        self.st = S.dchan("st")
        self.ps = [nc.alloc_psum_tensor(f"ps{i}", [P, 512], F32).ap() for i in range(8)]
        self.Bps = [Buf(f"ps{i}") for i in range(8)]
        A = self.A0 = SbAlloc(nc, SB_BASE)
        self.vecs = A([P, NV], F32, "vecs")
        self.cmat = A([P, NCM], F32, "cmat")
        self.ones_b = A([P, P], BF16, "ones")
        self.ident_b = A([P, P], BF16, "ident")
        self.pi_b = A([P, P], BF16, "pi")
        self.mask_b = A([P, 2, P], BF16, "mask")
        self.Bconst = Buf("const")
        S.dma(self.ld, self.vecs, self.vecs_d, writes=[self.Bconst])
        S.dma(self.ld, self.cmat, self.cmat_d, writes=[self.Bconst])
        S.op("pool", lambda e: e.memset(self.ones_b, 1.0), writes=[self.Bconst])
        S.op("dve", lambda e: e.tensor_copy(out=self.ident_b, in_=self.cmat[:, C_ID:C_ID + P]), reads=[self.Bconst], writes=[self.Bconst])
        S.op("dve", lambda e: e.tensor_copy(out=self.pi_b, in_=self.cmat[:, C_PI:C_PI + P]), reads=[self.Bconst], writes=[self.Bconst])
        S.op("dve", lambda e: e.tensor_copy(out=self.mask_b, in_=self.cmat[:, C_MPREV:C_MPREV + 2 * P].rearrange("p (a b) -> p a b", a=2)),
             reads=[self.Bconst], writes=[self.Bconst])
        S.barrier()

    def prep(self, names=None):
        S, nc = self.S, self.nc
        A = self.A0.fork()
        NB = 3
        CH = 4096
        fin = [A([P, CH], F32, "pin") for _ in range(NB)]
        fout = [A([P, CH], BF16, "pout") for _ in range(NB)]
        Bin = [Buf() for _ in range(NB)]
        Bout = [Buf() for _ in range(NB)]
        engs = ["dve", "act"]
        pieces = []
        for name in (names or list(self.wshapes)):
            shp = self.wshapes[name]
            nl = shp[0] if shp[0] != P else 1
            for l in range(nl):
                src = self.wf[name][l] if shp[0] != P else self.wf[name]
                dst = self.wb[name][l] if shp[0] != P else self.wb[name]
                nd = len(src.shape)
                letters = "abcd"[: nd - 1]
                pat = "p " + " ".join(letters) + " -> p (" + " ".join(letters) + ")"
                src2 = src.rearrange(pat)
                dst2 = dst.rearrange(pat)
                n = src2.shape[1]
                for c0 in range(0, n, CH):
                    pieces.append((name, l, src2, dst2, c0, min(CH, n - c0)))

        def load(k):
            name, l, src2, dst2, c0, w = pieces[k]
            S.dma(self.ld, fin[k % NB][:, :w], src2[:, c0:c0 + w], writes=[Bin[k % NB]])

        for k in range(min(2, len(pieces))):
            load(k)
        for k, (name, l, src2, dst2, c0, w) in enumerate(pieces):
            i = k % NB
            if name == "w_ret_out":
                per = 16 * P
                for q0 in range(0, w, P):
                    ec = ((c0 + q0) % per) // P
                    col = V_GN + l * 16 + ec
                    S.op("dve", lambda e, i=i, q0=q0, col=col: e.tensor_scalar(
                        out=fout[i][:, q0:q0 + P], in0=fin[i][:, q0:q0 + P], scalar1=self.vecs[:, col:col + 1],
                        scalar2=None, op0=ALU.mult), reads=[Bin[i], self.Bconst], writes=[Bout[i]])
            else:
                en = engs[k % 2]
                if en == "act":
                    S.op("act", lambda e, i=i, w=w: e.copy(out=fout[i][:, :w], in_=fin[i][:, :w]), reads=[Bin[i]], writes=[Bout[i]])
                else:
                    S.op(en, lambda e, i=i, w=w: e.tensor_copy(out=fout[i][:, :w], in_=fin[i][:, :w]), reads=[Bin[i]], writes=[Bout[i]])
            if k + 2 < len(pieces):
                load(k + 2)
            S.dma(self.st, dst2[:, c0:c0 + w], fout[i][:, :w], reads=[Bout[i]], writes=[self.Bwb[name]])
        S.barrier()

    def x_tile(self, ap_seq, tt):
        return ap_seq[:, tt * 512:(tt + 1) * 512].rearrange("(kc p) t -> p kc t", p=P)

    def rstd_from(self, src, sq, psb, rstd, Bsrc, Bsq, Brstd):
        S = self.S
        lvl = int(getattr(self, "dbglvl", 9))
        if lvl < 2:
            return
        S.op("act", lambda e: e.activation(out=sq, in_=src, func=AF.Square), reads=[Bsrc], writes=[Bsq])
        if lvl < 3:
            return
        for kc in range(KC):
            S.op("pe", lambda e, kc=kc: e.matmul(self.ps[psb], lhsT=self.ones_b, rhs=sq[:, kc, :], start=(kc == 0), stop=(kc == KC - 1)),
                 reads=[Bsq, self.Bconst], writes=[self.Bps[psb]], inc=(kc == KC - 1))
        if lvl < 4:
            return
        S.op("act", lambda e: e.activation(out=rstd, in_=self.ps[psb], func=AF.Sqrt, bias=RMS_EPS, scale=1.0 / DM),
             reads=[self.Bps[psb]], writes=[Brstd])
        if lvl < 5:
            return
        S.op("dve", lambda e: e.reciprocal(out=rstd, in_=rstd), reads=[Brstd], writes=[Brstd])

    def phase_hT(self, s, A, hT, BhT, gcol):
        S = self.S
        xt = [A([P, KC, 512], F32, "xt") for _ in range(2)]
        sq = A([P, KC, 512], BF16, "sq")
        rs = [A([P, 512], F32, "rs") for _ in range(2)]
        Bxt = [Buf() for _ in range(2)]
        Bsq = Buf()
        Brs = [Buf() for _ in range(2)]
        for tt in range(4):
            i = tt % 2
            S.dma(self.ld, xt[i], self.x_tile(self.xsrc[s], tt), reads=[self.Bx[s][tt]], writes=[Bxt[i]])
            self.rstd_from(xt[i], sq, tt % 2, rs[i], Bxt[i], Bsq, Brs[i])
            if int(getattr(self, "dbglvl", 9)) < 6:
                continue
            for kc in range(KC):
                S.op("dve", lambda e, kc=kc, i=i, tt=tt: e.scalar_tensor_tensor(
                    out=hT[:, kc, tt * 512:(tt + 1) * 512], in0=xt[i][:, kc, :], scalar=self.vecs[:, gcol + kc:gcol + kc + 1],
                    in1=rs[i], op0=ALU.mult, op1=ALU.mult), reads=[Bxt[i], Brs[i], self.Bconst], writes=[BhT[tt]])

    def post_norm_residual(self, s, tt, fT, BfT, xt, Bxt, sq, Bsq, rs, Brs, gcol, psb):
        S = self.S
        self.rstd_from(fT, sq, psb, rs, BfT, Bsq, Brs)
        for kc in range(KC):
            S.op("dve", lambda e, kc=kc: e.tensor_tensor(out=fT[:, kc, :], in0=fT[:, kc, :], in1=rs, op=ALU.mult),
                 reads=[BfT, Brs], writes=[BfT])
        for kc in range(KC):
            S.op("dve", lambda e, kc=kc: e.scalar_tensor_tensor(
                out=xt[:, kc, :], in0=fT[:, kc, :], scalar=self.vecs[:, gcol + kc:gcol + kc + 1], in1=xt[:, kc, :],
                op0=ALU.mult, op1=ALU.add), reads=[BfT, Bxt, self.Bconst], writes=[Bxt])
        S.dma(self.st, self.x_tile(self.xdst[s], tt), xt, reads=[Bxt], writes=[self.Bx[s][tt]], e="pool")

    def out_proj_phase(self, s, A, wname, l, nk, actT, BactT, gcol):
        S = self.S
        xt = [A([P, KC, 512], F32, "xt") for _ in range(2)]
        fT = A([P, KC, 512], F32, "fT")
        sq = A([P, KC, 512], BF16, "sq")
        rs = A([P, 512], F32, "rs")
        NW = 3
        wd = [A([P, nk, P], BF16, "wd") for _ in range(NW)]
        Bxt = [Buf() for _ in range(2)]
        BfT, Bsq, Brs = Buf(), Buf(), Buf()
        Bwd = [Buf() for _ in range(NW)]
        wsrc = self.wb[wname][l]
        seq = [(tt, dc) for tt in range(4) for dc in range(KC)]

        def loadw(n):
            tt, dc = seq[n]
            S.dma(self.ldw[n % NW], wd[n % NW], wsrc[:, dc], reads=[self.Bwb[wname]], writes=[Bwd[n % NW]])

        loadw(0)
        loadw(1)
        S.dma(self.ld, xt[0], self.x_tile(self.xsrc[s], 0), reads=[self.Bx[s][0]], writes=[Bxt[0]])
        for n, (tt, dc) in enumerate(seq):
            if n + 2 < len(seq):
                loadw(n + 2)
            if dc == 0 and tt + 1 < 4:
                S.dma(self.ld, xt[(tt + 1) % 2], self.x_tile(self.xsrc[s], tt + 1), reads=[self.Bx[s][tt + 1]], writes=[Bxt[(tt + 1) % 2]])
            pb = 2 + (n % 2)
            w = wd[n % NW]
            for j in range(nk):
                S.op("pe", lambda e, j=j, w=w, pb=pb, tt=tt: e.matmul(self.ps[pb], lhsT=w[:, j, :], rhs=actT[:, j, tt * 512:(tt + 1) * 512],
                                                               start=(j == 0), stop=(j == nk - 1)),
                     reads=[Bwd[n % NW], BactT], writes=[self.Bps[pb]], inc=(j == nk - 1))
            S.op("act", lambda e, dc=dc, pb=pb: e.copy(out=fT[:, dc, :], in_=self.ps[pb]), reads=[self.Bps[pb]], writes=[BfT])
            if dc == KC - 1:
                self.post_norm_residual(s, tt, fT, BfT, xt[tt % 2], Bxt[tt % 2], sq, Bsq, rs, Brs, gcol, tt % 2)

    def ffn(self, l, s):
        S = self.S
        A = self.A0.fork()
        aT = A([P, NJ, SEQ], BF16, "aT")
        BaT = Buf()
        R = A.fork()
        hT = R([P, KC, SEQ], BF16, "hT")
        BhT = [Buf() for _ in range(4)]
        self.phase_hT(s, self.A0.fork(), hT, BhT, V_FFNPRE + 8 * l)
        S.barrier()
        if getattr(self, "dbg", "") == "A":
            return
        NW = 3
        wu = [R([P, KC, 256], BF16, "wu") for _ in range(NW)]
        Bwu = [Buf() for _ in range(NW)]
        gb = R([P, 2 + SEQ], F32, "gb")
        Bgb = [Buf() for _ in range(4)]
        ct = [R([P, 512], F32, "ct") for _ in range(2)]
        cg = [R([P, 512], F32, "cg") for _ in range(2)]
        Bct = [Buf() for _ in range(2)]
        Bcg = [Buf() for _ in range(2)]
        vb = [R([P, 512], BF16, "vb") for _ in range(2)]
        Bvb = [Buf() for _ in range(2)]
        S.op("pool", lambda e: e.memset(gb[:, 0:2], 0.0), writes=[Bgb[0]])
        wsrc = self.wb["w_up"][l]

        def loadw(j):
            S.dma(self.ldw[j % NW], wu[j % NW], wsrc[:, j], reads=[self.Bwb["w_up"]], writes=[Bwu[j % NW]])

        loadw(0)
        loadw(1)
        cwb = V_CW + l * 3 * NJ
        cbb = V_CB + l * NJ
        n = 0
        for j in range(NJ):
            if j + 2 < NJ:
                loadw(j + 2)
            w = wu[j % NW]
            for tt in range(4):
                pg, pv = 4 + 2 * (n % 2), 5 + 2 * (n % 2)
                i = n % 2
                cols = slice(tt * 512, (tt + 1) * 512)
                for half, pb in ((0, pg), (1, pv)):
                    for kc in range(KC):
                        S.op("pe", lambda e, kc=kc, pb=pb, half=half, w=w, cols=cols: e.matmul(
                            self.ps[pb], lhsT=w[:, kc, half * P:(half + 1) * P], rhs=hT[:, kc, cols], start=(kc == 0), stop=(kc == KC - 1)),
                            reads=[Bwu[j % NW], BhT[tt]], writes=[self.Bps[pb]], inc=(kc == KC - 1))
                S.op("act", lambda e, pg=pg, tt=tt: e.copy(out=gb[:, 2 + tt * 512:2 + (tt + 1) * 512], in_=self.ps[pg]),
                     reads=[self.Bps[pg]], writes=[Bgb[tt]])
                S.op("act", lambda e, pv=pv, i=i: e.copy(out=vb[i], in_=self.ps[pv]), reads=[self.Bps[pv]], writes=[Bvb[i]])
                rb = [Bgb[tt]] + ([Bgb[tt - 1]] if tt > 0 else [])
                S.op("dve", lambda e, tt=tt, i=i, j=j: e.tensor_scalar(
                    out=ct[i], in0=gb[:, 2 + tt * 512:2 + (tt + 1) * 512], scalar1=self.vecs[:, cwb + 2 * NJ + j:cwb + 2 * NJ + j + 1],
                    scalar2=self.vecs[:, cbb + j:cbb + j + 1], op0=ALU.mult, op1=ALU.add), reads=rb + [self.Bconst], writes=[Bct[i]])
                for tap in (1, 0):
                    sh = 2 - tap
                    S.op("dve", lambda e, tt=tt, i=i, j=j, tap=tap, sh=sh: e.scalar_tensor_tensor(
                        out=ct[i], in0=gb[:, 2 - sh + tt * 512:2 - sh + (tt + 1) * 512],
                        scalar=self.vecs[:, cwb + tap * NJ + j:cwb + tap * NJ + j + 1], in1=ct[i], op0=ALU.mult, op1=ALU.add),
                        reads=rb + [Bct[i], self.Bconst], writes=[Bct[i]])
                S.op("act", lambda e, i=i: e.activation(out=cg[i], in_=ct[i], func=AF.Gelu_apprx_tanh), reads=[Bct[i]], writes=[Bcg[i]])
                S.op("dve", lambda e, i=i, j=j, cols=cols: e.tensor_tensor(out=aT[:, j, cols], in0=cg[i], in1=vb[i], op=ALU.mult),
                     reads=[Bcg[i], Bvb[i]], writes=[BaT])
                n += 1
        S.barrier()
        if getattr(self, "dbg", "") == "B":
            return
        self.out_proj_phase(s, A.fork(), "w_down", l, NJ, aT, BaT, V_FFNPOST + 8 * l)
        S.barrier()


    def ret(self, l, s):
        S = self.S
        A = self.A0.fork()
        yT = A([P, 16, SEQ], BF16, "yT")
        ByT = Buf()
        R = A.fork()
        hT = R([P, KC, SEQ], BF16, "hT")
        BhT = [Buf() for _ in range(4)]
        self.phase_hT(s, self.A0.fork(), hT, BhT, V_MIXPRE + 8 * l)
        S.barrier()
        R2 = R.fork()
        cs = R([P, 2, SEQ], F32, "cs")
        Bcs = Buf()
        S.dma(self.ld, cs, self.rcs_d, writes=[Bcs])
        wsl = R([P, KC, 1536], BF16, "wsl")
        Bw = Buf()
        qk = [R([P, 4, 512], BF16, "qk") for _ in range(2)]
        vv = [R([P, 4, 512], BF16, "vv") for _ in range(2)]
        sg = [R([P, 4, 512], BF16, "sg") for _ in range(2)]
        Bqk = [[Buf(), Buf()] for _ in range(2)]
        Bvv = [[Buf() for _ in range(4)] for _ in range(2)]
        Bsg = [[Buf() for _ in range(4)] for _ in range(2)]
        tm = [R([P, 512], F32, "tm") for _ in range(4)]
        Btm = [Buf() for _ in range(4)]
        stf = R([P, 2, 512], F32, "stf")
        stb = R([P, 2, 512], BF16, "stb")
        Bstf, Bstb = Buf(), Buf()
        sT = [R([P, P], BF16, "sT") for _ in range(2)]
        kz = [R([P, 256], BF16, "kz") for _ in range(2)]
        yn = [R([P, 512], F32, "yn") for _ in range(2)]
        gt = [R([P, 512], BF16, "gt") for _ in range(2)]
        bst = [R([P, 6], F32, "bst") for _ in range(2)]
        mv = [R([P, 2], F32, "mv") for _ in range(2)]
        rg = [R([P, 2], F32, "rg") for _ in range(2)]
        BsT = [Buf() for _ in range(2)]
        Bkz = [Buf() for _ in range(2)]
        Byn = [Buf() for _ in range(2)]
        Bgt = [Buf() for _ in range(2)]
        Bsm = [Buf() for _ in range(2)]
        ps, Bps = self.ps, self.Bps
        psK = ps[3].bitcast(BF16)[:, 0:256]
        psT = ps[3].bitcast(BF16)[:, 512:1024]
        BpsK, BpsT = Buf(), Buf()
        proj_banks = [0, 1, 7]
        pbn = [0]
        wsrc = self.wb["w_ret_in"][l]
        units = [(h, st) for h in range(RH) for st in range(4)]

        def nextbank():
            b = proj_banks[pbn[0] % 3]
            pbn[0] += 1
            return b

        def load_w(h):
            S.dma(self.ldw[0], wsl, wsrc[:, h], reads=[self.Bwb["w_ret_in"]], writes=[Bw])

        def qk_pair(u, which):
            h, st = units[u]
            i = u % 2
            cols = slice(st * 512, (st + 1) * 512)
            banks = []
            for half in range(2):
                fi = which * 2 + half
                b = nextbank()
                banks.append(b)
                for kc in range(KC):
                    S.op("pe", lambda e, kc=kc, b=b, fi=fi: e.matmul(ps[b], lhsT=wsl[:, kc, fi * P:(fi + 1) * P], rhs=hT[:, kc, cols],
                                                                 start=(kc == 0), stop=(kc == KC - 1)),
                         reads=[Bw, BhT[st]], writes=[Bps[b]], inc=(kc == KC - 1))
            b0, b1 = banks
            cosv, sinv = cs[:, 0, cols], cs[:, 1, cols]
            o = which * 2
            S.op("dve", lambda e: e.tensor_tensor(out=tm[0], in0=ps[b0], in1=cosv, op=ALU.mult), reads=[Bps[b0], Bcs], writes=[Btm[0]])
            S.op("dve", lambda e: e.tensor_tensor(out=tm[1], in0=ps[b1], in1=sinv, op=ALU.mult), reads=[Bps[b1], Bcs], writes=[Btm[1]])
            S.op("dve", lambda e: e.tensor_tensor(out=tm[2], in0=ps[b1], in1=cosv, op=ALU.mult), reads=[Bps[b1], Bcs], writes=[Btm[2]])
            S.op("dve", lambda e: e.tensor_tensor(out=tm[3], in0=ps[b0], in1=sinv, op=ALU.mult), reads=[Bps[b0], Bcs], writes=[Btm[3]])
            S.op("dve", lambda e: e.tensor_tensor(out=qk[i][:, o, :], in0=tm[0], in1=tm[1], op=ALU.subtract),
                 reads=[Btm[0], Btm[1]], writes=[Bqk[i][which]])
            S.op("dve", lambda e: e.tensor_tensor(out=qk[i][:, o + 1, :], in0=tm[2], in1=tm[3], op=ALU.add),
                 reads=[Btm[2], Btm[3]], writes=[Bqk[i][which]])

        def vg_part(u, c):
            h, st = units[u]
            i = u % 2
            t0 = st * 512 + c * P
            for which in range(2):
                b = nextbank()
                for kc in range(KC):
                    S.op("pe", lambda e, kc=kc, b=b, which=which: e.matmul(
                        ps[b], lhsT=hT[:, kc, t0:t0 + P], rhs=wsl[:, kc, 512 + which * 512:1024 + which * 512],
                        start=(kc == 0), stop=(kc == KC - 1)), reads=[Bw, BhT[st]], writes=[Bps[b]], inc=(kc == KC - 1))
                if which == 0:
                    S.op("act", lambda e, b=b: e.copy(out=vv[i][:, c, :], in_=ps[b]), reads=[Bps[b]], writes=[Bvv[i][c]])
                else:
                    S.op("act", lambda e, b=b: e.activation(out=sg[i][:, c, :], in_=ps[b], func=AF.Silu), reads=[Bps[b]], writes=[Bsg[i][c]])

        def proj_parts(u):
            return [lambda: (qk_pair(u, 0), vg_part(u, 0)), lambda: (qk_pair(u, 1), vg_part(u, 1)),
                    lambda: vg_part(u, 2), lambda: vg_part(u, 3)]

        def front(u, c):
            h, st = units[u]
            i = u % 2
            k = c % 2
            cc = slice(c * P, (c + 1) * P)
            for j in range(2):
                S.op("pe", lambda e, j=j: e.matmul(ps[2][:, 0:P], lhsT=qk[i][:, 2 + j, cc], rhs=qk[i][:, j, cc], start=(j == 0), stop=(j == 1)),
                     reads=[Bqk[i][0], Bqk[i][1]], writes=[Bps[2]])
            for j in range(2):
                S.op("pe", lambda e, j=j: e.transpose(psK[:, j * P:(j + 1) * P], qk[i][:, 2 + j, cc], self.ident_b),
                     reads=[Bqk[i][1], self.Bconst], writes=[BpsK])
            S.op("dve", lambda e: e.tensor_tensor(out=sT[k], in0=ps[2][:, 0:P], in1=self.cmat[:, C_DT + h * P:C_DT + (h + 1) * P], op=ALU.mult),
                 reads=[Bps[2], self.Bconst], writes=[BsT[k]])
            S.op("act", lambda e: e.activation(out=kz[k], in_=psK, func=AF.Identity, scale=self.cmat[:, C_ZETA + h:C_ZETA + h + 1]),
                 reads=[BpsK, self.Bconst], writes=[Bkz[k]])

        def back(u, c):
            h, st = units[u]
            i = u % 2
            k = c % 2
            cg = st * 4 + c
            cc = slice(c * P, (c + 1) * P)
            lg = math.log(1.0 - 2.0 ** (-5.0 - h))
            gC = math.exp(128.0 * lg)
            S.op("pe", lambda e: e.matmul(ps[4], lhsT=sT[k], rhs=vv[i][:, c, :], start=True, stop=(cg == 0)),
                 reads=[BsT[k], Bvv[i][c]], writes=[Bps[4]])
            if cg > 0:
                for j in range(2):
                    S.op("pe", lambda e, j=j: e.matmul(ps[4], lhsT=qk[i][:, j, cc], rhs=stb[:, j, :], start=False, stop=(j == 1)),
                         reads=[Bqk[i][0], Bstb], writes=[Bps[4]])
            if cg < 15:
                for j in range(2):
                    S.op("pe", lambda e, j=j: e.matmul(ps[5 + j], lhsT=kz[k][:, j * P:(j + 1) * P], rhs=vv[i][:, c, :], start=True, stop=True),
                         reads=[Bkz[k], Bvv[i][c]], writes=[Bps[5 + j]])
                for j in range(2):
                    if cg == 0:
                        S.op("dve", lambda e, j=j: e.tensor_copy(out=stf[:, j, :], in_=ps[5 + j]), reads=[Bps[5 + j]], writes=[Bstf])
                    else:
                        S.op("dve", lambda e, j=j: e.scalar_tensor_tensor(out=stf[:, j, :], in0=stf[:, j, :], scalar=gC, in1=ps[5 + j],
                                                                         op0=ALU.mult, op1=ALU.add), reads=[Bps[5 + j], Bstf], writes=[Bstf])
                S.op("act", lambda e: e.copy(out=stb, in_=stf), reads=[Bstf], writes=[Bstb])
            S.op("dve", lambda e: e.bn_stats(out=bst[k], in_=ps[4]), reads=[Bps[4]], writes=[Bsm[k]])
            S.op("dve", lambda e: e.bn_aggr(out=mv[k], in_=bst[k]), reads=[Bsm[k]], writes=[Bsm[k]])
            S.op("act", lambda e: e.activation(out=rg[k][:, 0:1], in_=mv[k][:, 1:2], func=AF.Sqrt, bias=self.cmat[:, C_EPS + h:C_EPS + h + 1], scale=1.0),
                 reads=[Bsm[k], self.Bconst], writes=[Bsm[k]])
            S.op("dve", lambda e: e.reciprocal(out=rg[k][:, 0:1], in_=rg[k][:, 0:1]), reads=[Bsm[k]], writes=[Bsm[k]])
            S.op("dve", lambda e: e.scalar_tensor_tensor(out=rg[k][:, 1:2], in0=mv[k][:, 0:1], scalar=-1.0, in1=rg[k][:, 0:1], op0=ALU.mult, op1=ALU.mult),
                 reads=[Bsm[k]], writes=[Bsm[k]])
            S.op("act", lambda e: e.activation(out=yn[k], in_=ps[4], func=AF.Identity, bias=rg[k][:, 1:2], scale=rg[k][:, 0:1]),
                 reads=[Bps[4], Bsm[k]], writes=[Byn[k]])
            S.op("dve", lambda e: e.tensor_tensor(out=gt[k], in0=yn[k], in1=sg[i][:, c, :], op=ALU.mult),
                 reads=[Byn[k], Bsg[i][c]], writes=[Bgt[k]])

        def ytr(u, c):
            h, st = units[u]
            k = c % 2
            cg = st * 4 + c
            for jj in range(4):
                S.op("pe", lambda e, jj=jj: e.transpose(psT[:, jj * P:(jj + 1) * P], gt[k][:, jj * P:(jj + 1) * P], self.ident_b),
                     reads=[Bgt[k], self.Bconst], writes=[BpsT])
            S.op("act", lambda e: e.copy(out=yT[:, 4 * h:4 * h + 4, cg * P:(cg + 1) * P], in_=psT.rearrange("p (a b) -> p a b", a=4)),
                 reads=[BpsT], writes=[ByT])

        load_w(0)
        for f in proj_parts(0):
            f()
        pending = None
        for u in range(len(units)):
            nxt = [None] * 4
            if u + 1 < len(units):
                if units[u + 1][0] != units[u][0]:
                    load_w(units[u + 1][0])
                nxt = proj_parts(u + 1)
            for c in range(4):
                front(u, c)
                if nxt[c] is not None:
                    nxt[c]()
                back(u, c)
                if pending is not None:
                    ytr(*pending)
                pending = (u, c)
        ytr(*pending)
        S.barrier()
        self.out_proj_phase(s, R2, "w_ret_out", l, 16, yT, ByT, V_MIXPOST + 8 * l)
        S.barrier()


    @staticmethod
    def gcols(ap2d, g, u):
        if g == 0:
            return ap2d[:, u * 512:(u + 1) * 512]
        if g == 1:
            return ap2d[:, u::4]
        return ap2d.rearrange("p (i r) -> p r i", r=16)[:, 4 * u:4 * u + 4, :]

    @staticmethod
    def gview(ap512, g):
        return ap512 if g < 2 else ap512.rearrange("p (r i) -> p r i", r=4)

    @staticmethod
    def chunk_tokens(ap2d, g, c):
        if g == 0:
            return ap2d[..., c * P:(c + 1) * P] if False else ap2d[:, c * P:(c + 1) * P]
        if g == 1:
            st = 512 * (c % 4) + c // 4
            return ap2d[:, st:st + 509:4]
        return ap2d[:, c::16]

    def rot_proj(self, g, u, w_lhsT, hT, BhT, Bw, cs, Bcs, qraw, Bqraw, t1, t2, Bt, out512, Bout, pb, pb2):
        S, ps, Bps = self.S, self.ps, self.Bps
        for kc in range(KC):
            S.op("pe", lambda e, kc=kc: e.matmul(self.gview(ps[pb], g), lhsT=w_lhsT[:, kc, :], rhs=self.gcols(hT[:, kc, :], g, u),
                                               start=(kc == 0), stop=(kc == KC - 1)), reads=[Bw] + BhT, writes=[Bps[pb]], inc=(kc == KC - 1))
        S.op("act", lambda e: e.copy(out=qraw, in_=ps[pb]), reads=[Bps[pb]], writes=[Bqraw])
        S.op("pe", lambda e: e.matmul(ps[pb2], lhsT=self.pi_b, rhs=qraw, start=True, stop=True), reads=[Bqraw, self.Bconst], writes=[Bps[pb2]])
        S.op("dve", lambda e: e.tensor_tensor(out=self.gview(t1, g), in0=self.gview(ps[pb2], g), in1=self.gcols(cs[:, 1, :], g, u), op=ALU.mult),
             reads=[Bps[pb2], Bcs], writes=[Bt[0]])
        S.op("dve", lambda e: e.tensor_tensor(out=self.gview(t2, g), in0=self.gview(ps[pb], g), in1=self.gcols(cs[:, 0, :], g, u), op=ALU.mult),
             reads=[Bps[pb], Bcs], writes=[Bt[1]])
        if isinstance(out512, tuple):
            for rows, o in out512:
                S.op("dve", lambda e, rows=rows, o=o: e.tensor_tensor(out=o, in0=t1[rows, :], in1=t2[rows, :], op=ALU.add),
                     reads=[Bt[0], Bt[1]], writes=[Bout])
        else:
            S.op("dve", lambda e: e.tensor_tensor(out=out512, in0=t1, in1=t2, op=ALU.add), reads=[Bt[0], Bt[1]], writes=[Bout])

    def kv(self, s):
        S = self.S
        A = self.A0.fork()
        hT = A([P, KC, SEQ], BF16, "hT")
        BhT = [Buf() for _ in range(4)]
        self.phase_hT(s, A.fork(), hT, BhT, V_KVN)
        S.barrier()
        R = A.fork()
        cs = R([P, 2, SEQ], F32, "cs")
        Bcs = Buf()
        S.dma(self.ld, cs, self.acs_d, writes=[Bcs])
        wk = [R([P, KC, P], BF16, "wk") for _ in range(3)]
        Bwk = [Buf() for _ in range(3)]
        qraw = [R([P, 512], BF16, "qraw") for _ in range(2)]
        t1 = [R([P, 512], F32, "t1") for _ in range(2)]
        t2 = [R([P, 512], F32, "t2") for _ in range(2)]
        Bqraw = [Buf() for _ in range(2)]
        Bt = [[Buf(), Buf()] for _ in range(2)]
        kst = [R([P, SEQ], BF16, "kst") for _ in range(2)]
        Bkst = [Buf() for _ in range(2)]
        wv = R([P, 2, KC, 512], BF16, "wv")
        Bwv = Buf()
        stg = R([P, 8, 16, 192], BF16, "stg")
        Bstg = Buf()
        S.op("dve", lambda e: e.memset(stg.rearrange("p a c x -> p (a c x)"), 1.0), writes=[Bstg])
        wsrc = self.wb["w_k"]

        def loadk(gp):
            S.dma(self.ldw[gp % 3], wk[gp % 3], wsrc[:, gp], reads=[self.Bwb["w_k"]], writes=[Bwk[gp % 3]])

        loadk(0)
        loadk(1)
        n = 0
        for gp in range(24):
            g, hp = gp // 8, gp % 8
            if gp + 2 < 24:
                loadk(gp + 2)
            ks = kst[gp % 2]
            for u in range(4):
                i = n % 2
                self.rot_proj(g, u, wk[gp % 3], hT, BhT, Bwk[gp % 3], cs, Bcs, qraw[i], Bqraw[i], t1[i], t2[i], Bt[i],
                              ks[:, u * 512:(u + 1) * 512], Bkst[gp % 2], 0 + i, 2 + i)
                n += 1
            S.dma(self.st, self.kt_d[g, hp], ks, reads=[Bkst[gp % 2]], writes=[self.Bkt])
        ps, Bps = self.ps, self.Bps
        n = 0
        for g in range(3):
            S.dma(self.ldw[3], wv, self.wb["w_v"][:, 2 * g:2 * g + 2], reads=[self.Bwb["w_v"]], writes=[Bwv])
            for c in range(16):
                for half in range(2):
                    pb = 4 + (n % 4)
                    n += 1
                    for kc in range(KC):
                        S.op("pe", lambda e, kc=kc, pb=pb, half=half: e.matmul(ps[pb], lhsT=self.chunk_tokens(hT[:, kc, :], g, c), rhs=wv[:, half, kc, :],
                                                                         start=(kc == 0), stop=(kc == KC - 1)), reads=[Bwv] + BhT, writes=[Bps[pb]], inc=(kc == KC - 1))
                    pv = ps[pb].rearrange("p (a h d) -> p a h d", a=4, h=2)
                    for hh in range(2):
                        o = stg[:, half * 4:half * 4 + 4, c, hh * 128:hh * 128 + 64]
                        if hh == 0:
                            S.op("act", lambda e, o=o, pv=pv: e.copy(out=o, in_=pv[:, :, 0, :]), reads=[Bps[pb]], writes=[Bstg])
                        else:
                            S.op("dve", lambda e, o=o, pv=pv: e.tensor_copy(out=o, in_=pv[:, :, 1, :]), reads=[Bps[pb]], writes=[Bstg])
            S.dma(self.st, self.va_d[g], stg.rearrange("p a c x -> p a (c x)"), reads=[Bstg], writes=[self.Bva])
        S.barrier()

    def att(self, l, s):
        S = self.S
        li = l - 2
        A = self.A0.fork()
        oT = A([P, 8, SEQ], BF16, "oT")
        BoT = Buf()
        R = A.fork()
        R0 = R.fork()
        hT = R([P, KC, SEQ], BF16, "hT")
        BhT = [Buf() for _ in range(4)]
        self.phase_hT(s, R.fork(), hT, BhT, V_MIXPRE + 8 * l)
        S.barrier()
        cs = R([P, 2, SEQ], F32, "cs")
        Bcs = Buf()
        S.dma(self.ld, cs, self.acs_d, writes=[Bcs])
        wq = [R([P, KC, 384], BF16, "wq") for _ in range(2)]
        Bwq = [Buf() for _ in range(2)]
        kt = R([P, 3, SEQ], BF16, "kt")
        vt = R([P, 3, 16 * 192], BF16, "vt")
        Bkt, Bvt = Buf(), Buf()
        qT = R([P, 3, 2, SEQ], BF16, "qT")
        BqT = [Buf() for _ in range(3)]
        S.op("dve", lambda e: e.memset(qT.rearrange("p g h t -> p (g h t)"), 0.0), writes=BqT)
        acc = R([P, 2, SEQ], F32, "acc")
        Bacc = Buf()
        qraw = [R([P, 512], BF16, "qraw") for _ in range(2)]
        t1 = [R([P, 512], F32, "t1") for _ in range(2)]
        t2 = [R([P, 512], F32, "t2") for _ in range(2)]
        Bqraw = [Buf() for _ in range(2)]
        Bt = [[Buf(), Buf()] for _ in range(2)]
        PT = [R([P, 2, 2, P], BF16, "PT") for _ in range(2)]
        BpP = [Buf() for _ in range(2)]
        BPT = [Buf() for _ in range(2)]
        tden = R([P, SEQ], F32, "tden")
        Btden = Buf()
        ps, Bps = self.ps, self.Bps
        psTb = ps[6].bitcast(BF16)
        psT = [psTb[:, k * 512:(k + 1) * 512].rearrange("p (h c q) -> p h c q", h=2, c=2) for k in range(2)]
        psU = [ps[7][:, k * 256:(k + 1) * 256].rearrange("p (h q) -> p h q", h=2) for k in range(2)]
        BpsT = [Buf() for _ in range(2)]
        BpsU = [Buf() for _ in range(2)]
        wsrc = self.wb["w_q"][li]

        def loadq(hp):
            S.dma(self.ldw[hp % 2], wq[hp % 2], wsrc[:, hp], reads=[self.Bwb["w_q"]], writes=[Bwq[hp % 2]])

        loadq(0)
        n = 0
        m = 0
        for hp in range(8):
            if hp + 1 < 8:
                loadq(hp + 1)
            S.dma(self.ldw[2], kt, self.kt_d[:, hp].rearrange("g p t -> p g t"), reads=[self.Bkt], writes=[Bkt])
            S.dma(self.ldw[3], vt, self.va_d[:, :, hp].rearrange("g p x -> p g x"), reads=[self.Bva], writes=[Bvt])
            w = wq[hp % 2]
            for g in range(3):
                for u in range(4):
                    i = n % 2
                    self.rot_proj(g, u, w[:, :, g * P:(g + 1) * P], hT, BhT, Bwq[hp % 2], cs, Bcs, qraw[i], Bqraw[i], t1[i], t2[i], Bt[i],
                                  ((slice(0, 64), qT[0:64, g, 0, u * 512:(u + 1) * 512]), (slice(64, 128), qT[64:128, g, 1, u * 512:(u + 1) * 512])),
                                  BqT[g], 0 + i, 2 + i)
                    n += 1
            for g in range(3):
                for c in range(16):
                    has_prev = (c > 0) if g == 0 else ((c % 4) > 0 if g == 1 else False)
                    nkc = 2 if has_prev else 1
                    nk = nkc * P
                    k0 = (c - 1) * P if has_prev else c * P
                    k = m % 2
                    m += 1
                    pS = 4 + k
                    pSv = ps[pS].rearrange("p (c h q) -> p c h q", c=2, h=2)
                    for kc in range(nkc):
                        S.op("pe", lambda e, kc=kc: e.matmul(pSv[:, kc], lhsT=kt[:, g, k0 + kc * P:k0 + (kc + 1) * P], rhs=qT[:, g, :, c * P:(c + 1) * P],
                                                          start=True, stop=True), reads=[BqT[g], Bkt], writes=[Bps[pS]])
                    S.op("act", lambda e: e.activation(out=PT[k][:, 0:nkc], in_=pSv[:, 0:nkc], func=AF.Exp, scale=0.125),
                         reads=[Bps[pS]], writes=[BPT[k]])
                    msk = self.mask_b[:, 2 - nkc:2, :].unsqueeze(2).broadcast_to([P, nkc, 2, P])
                    S.op("dve", lambda e, msk=msk: e.tensor_tensor(out=PT[k][:, 0:nkc], in0=PT[k][:, 0:nkc], in1=msk, op=ALU.mult),
                         reads=[BPT[k], self.Bconst], writes=[BPT[k]])
                    for hh in range(2):
                        for kc in range(nkc):
                            ch = (c - 1 + kc) if has_prev else c
                            S.op("pe", lambda e, hh=hh, kc=kc, ch=ch: e.matmul(psU[k][:, hh, :], lhsT=vt[:, g, ch * 192 + hh * 64:ch * 192 + hh * 64 + P],
                                                                            rhs=PT[k][:, kc, hh, :], start=(kc == 0), stop=(kc == nkc - 1)),
                                 reads=[Bvt, BPT[k]], writes=[BpsU[k]], inc=(kc == nkc - 1))
                    if g == 0:
                        S.op("act", lambda e: e.copy(out=acc[:, :, c * P:(c + 1) * P], in_=psU[k]), reads=[BpsU[k]], writes=[Bacc])
                    else:
                        if g == 1:
                            st = 512 * (c % 4) + c // 4
                            dst = acc[:, :, st:st + 509:4]
                        else:
                            dst = acc[:, :, c::16]
                        S.op("dve", lambda e, dst=dst: e.tensor_tensor(out=dst, in0=dst, in1=psU[k], op=ALU.add), reads=[BpsU[k], Bacc], writes=[Bacc])
            S.op("act", lambda e: e.copy(out=tden[0:64, :], in_=acc[64:128, 0, :]), reads=[Bacc], writes=[Btden])
            S.op("act", lambda e: e.copy(out=tden[64:128, :], in_=acc[0:64, 1, :]), reads=[Bacc], writes=[Btden])
            S.op("dve", lambda e: e.reciprocal(out=tden, in_=tden), reads=[Btden], writes=[Btden])
            S.op("dve", lambda e, hp=hp: e.tensor_tensor(out=oT[0:64, hp, :], in0=acc[0:64, 0, :], in1=tden[0:64, :], op=ALU.mult),
                 reads=[Bacc, Btden], writes=[BoT])
            S.op("dve", lambda e, hp=hp: e.tensor_tensor(out=oT[64:128, hp, :], in0=acc[64:128, 1, :], in1=tden[64:128, :], op=ALU.mult),
                 reads=[Bacc, Btden], writes=[BoT])
        S.barrier()
        self.out_proj_phase(s, R0, "w_o", li, 8, oT, BoT, V_MIXPOST + 8 * l)
        S.barrier()

    def run_stages(self, stages=None):
        if stages is None:
            stages = [("prep", None)]
            for s in range(self.nseq):
                for l in range(4):
                    stages.append(("ret" if l < 2 else "att", l, s))
                    stages.append(("ffn", l, s))
                    if l == 1:
                        stages.append(("kv", s))
        for st in stages:
            kind = st[0]
            if kind == "prep":
                self.prep(st[1])
            elif kind == "ffn":
                self.ffn(st[1], st[2])
                self.xsrc[st[2]] = self.xdst[st[2]]
            elif kind == "ret":
                self.ret(st[1], st[2])
                self.xsrc[st[2]] = self.xdst[st[2]]
            elif kind == "att":
                self.att(st[1], st[2])
                self.xsrc[st[2]] = self.xdst[st[2]]
            elif kind == "kv":
                self.kv(st[1])

    def finish(self):
        S = self.S
        S.barrier()
        return self.nc


def _kxm(w):
    K, F = w.shape
    return np.ascontiguousarray(w.reshape(K // P, P, F).transpose(1, 0, 2))


def host_weights(inp):
    out = {}
    wi = np.asarray(inp["ret_w_in"], np.float32)
    cols = []
    for h in range(RH):
        cols += list(range(h * 256, (h + 1) * 256))
        cols += list(range(1024 + h * 256, 1024 + (h + 1) * 256))
        cols += list(range(2048 + h * 512, 2048 + (h + 1) * 512))
        cols += list(range(4096 + h * 512, 4096 + (h + 1) * 512))
    cols = np.array(cols)
    out["w_ret_in"] = np.stack([_kxm(wi[l][:, cols]).reshape(P, KC, 4, 1536).transpose(0, 2, 1, 3) for l in range(2)])
    wo = np.asarray(inp["ret_w_out"], np.float32)
    out["w_ret_out"] = np.stack([_kxm(wo[l]).reshape(P, 16, KC, P).transpose(0, 2, 1, 3) for l in range(2)])
    wkv = np.asarray(inp["att_w_kv"], np.float32)
    out["w_k"] = _kxm(wkv[:, :3072]).reshape(P, KC, 24, P).transpose(0, 2, 1, 3)
    out["w_v"] = _kxm(wkv[:, 3072:]).reshape(P, KC, 6, 512).transpose(0, 2, 1, 3)
    wq = np.asarray(inp["att_w_q"], np.float32)
    out["w_q"] = np.stack([_kxm(wq[l]).reshape(P, KC, 3, 8, P).transpose(0, 3, 1, 2, 4).reshape(P, 8, KC, 384) for l in range(2)])
    wao = np.asarray(inp["att_w_o"], np.float32)
    out["w_o"] = np.stack([_kxm(wao[l]).reshape(P, KC, KC, P).transpose(0, 2, 1, 3) for l in range(2)])
    wu = np.asarray(inp["ffn_w_up"], np.float32)
    out["w_up"] = np.stack([np.concatenate([_kxm(wu[l][:, :DFF]).reshape(P, KC, NJ, P), _kxm(wu[l][:, DFF:]).reshape(P, KC, NJ, P)], axis=3)
                            .transpose(0, 2, 1, 3) for l in range(4)])
    wd = np.asarray(inp["ffn_w_down"], np.float32)
    out["w_down"] = np.stack([_kxm(wd[l]).reshape(P, NJ, KC, P).transpose(0, 2, 1, 3) for l in range(4)])
    out = {k: np.ascontiguousarray(v, dtype=np.float32) for k, v in out.items()}

    def pv(v):
        v = np.asarray(v, np.float32)
        return v.reshape(-1, P).T

    vecs = np.zeros((P, NV), np.float32)
    for l in range(4):
        vecs[:, V_MIXPRE + 8 * l:V_MIXPRE + 8 * l + 8] = pv(inp["norm_mix_pre"][l])
        vecs[:, V_MIXPOST + 8 * l:V_MIXPOST + 8 * l + 8] = pv(inp["norm_mix_post"][l])
        vecs[:, V_FFNPRE + 8 * l:V_FFNPRE + 8 * l + 8] = pv(inp["norm_ffn_pre"][l])
        vecs[:, V_FFNPOST + 8 * l:V_FFNPOST + 8 * l + 8] = pv(inp["norm_ffn_post"][l])
        for tap in range(3):
            vecs[:, V_CW + (l * 3 + tap) * NJ:V_CW + (l * 3 + tap + 1) * NJ] = pv(inp["ffn_conv_w"][l][tap])
        vecs[:, V_CB + l * NJ:V_CB + (l + 1) * NJ] = pv(inp["ffn_conv_b"][l])
    vecs[:, V_KVN:V_KVN + 8] = pv(inp["kv_norm"])
    for l in range(2):
        vecs[:, V_GN + 16 * l:V_GN + 16 * l + 16] = pv(inp["ret_gn_gain"][l])
    out["vecs"] = vecs
    return out


def host_consts():
    c = {}
    pos = np.arange(SEQ, dtype=np.float32)
    inv = (1.0 / (np.float32(10000.0) ** np.linspace(0.0, 1.0, 128, dtype=np.float32))).astype(np.float32)
    ang = (pos[None, :] * inv[:, None]).astype(np.float32)
    c["rcs"] = np.stack([np.cos(ang), np.sin(ang)], axis=1).astype(np.float32)
    invf = (np.float32(500000.0) ** (-np.arange(0, 16, 2, dtype=np.float32) / np.float32(16))).astype(np.float32)
    acs = np.zeros((P, 2, SEQ), np.float32)
    pi = np.zeros((P, P), np.float32)
    for p in range(P):
        hd = p % 64
        if hd < 8:
            a = (pos * invf[hd]).astype(np.float32)
            acs[p, 0] = np.cos(a)
            acs[p, 1] = -np.sin(a)
            pi[p + 8, p] = 1.0
        elif hd < 16:
            a = (pos * invf[hd - 8]).astype(np.float32)
            acs[p, 0] = np.cos(a)
            acs[p, 1] = np.sin(a)
            pi[p - 8, p] = 1.0
        else:
            acs[p, 0] = 1.0
    c["acs"] = acs
    cm = np.zeros((P, NCM), np.float32)
    cm[:, C_PI:C_PI + P] = pi
    cm[:, C_ID:C_ID + P] = np.eye(P, dtype=np.float32)
    n = np.arange(P, dtype=np.float64)
    for h in range(RH):
        lg = math.log(1.0 - 2.0 ** (-5.0 - h))
        dtm = np.where(n[None, :] >= n[:, None], np.exp(-(n[:, None] + 1.0) * lg) / 16.0, 0.0)
        cm[:, C_DT + h * P:C_DT + (h + 1) * P] = dtm
        cm[:, C_ZETA + h] = np.exp((127.0 - n) * lg) / 16.0
        cm[:, C_EPS + h] = GN_EPS * np.exp(-2.0 * (n + 1.0) * lg)
    kj = np.arange(P)
    cm[:, C_MPREV:C_MPREV + P] = (kj[:, None] >= kj[None, :]).astype(np.float32)
    cm[:, C_MCUR:C_MCUR + P] = (kj[:, None] <= kj[None, :]).astype(np.float32)
    c["cmat"] = cm
    return c


def build_program(nseq=NSEQ, stages=None):
    pg = Prog(nseq)
    pg.run_stages(stages)
    return pg.finish()


def kernel(**inputs):
    x = np.asarray(inputs["x"], np.float32)
    hw = host_weights(inputs)
    hc = host_consts()
    nc = build_program(NSEQ)
    in_maps = []
    for c in range(NCORES):
        m = dict(hw)
        m.update(hc)
        m["xT"] = np.ascontiguousarray(x[c * NSEQ:(c + 1) * NSEQ].transpose(0, 2, 1))
        in_maps.append(m)
    res = run_bass_kernel_spmd(nc, in_maps, core_ids=list(range(NCORES)))
    outs = [np.asarray(r["out"], np.float32).transpose(0, 2, 1) for r in res.results]
    return np.ascontiguousarray(np.concatenate(outs, axis=0))
```
